# Optimizing a Trainium2 kernel written in Bass

```python
import jax
import jax.numpy as jnp
from jax import lax
import numpy as np

D_MODEL = 2048
BATCH = 4
SEQ = 4096
DEPTH = 2

CTX_LEN = 256
GRID_W = 64
NORM_EPS = 1e-6

N_HEADS = 16
N_KV_HEADS = 4
HEAD_DIM = D_MODEL // 32
WINDOW = 128
ATTN_BLOCK = 128
ROPE_BASE = 10000.0

FOURIER_GROUPS = 4
FOURIER_GROUP_DIM = D_MODEL // 8

MLSTM_HEADS = 8
MLSTM_DK = D_MODEL // 32
MLSTM_DV = D_MODEL // 16
MLSTM_CHUNK = 64

N_BRANCHES = 3

N_GROUPS = 4
EXPERTS_PER_GROUP = 8
N_EXPERTS = N_GROUPS * EXPERTS_PER_GROUP
TOP_K = 2
EXPERT_FF = D_MODEL // 2
MOE_BLOCK = 128

ATTN_Q = N_HEADS * HEAD_DIM
ATTN_KV = N_KV_HEADS * HEAD_DIM
ML_QK = MLSTM_HEADS * MLSTM_DK
ML_V = MLSTM_HEADS * MLSTM_DV
ML_GATES = 4 * MLSTM_HEADS
FOURIER_W = FOURIER_GROUPS * FOURIER_GROUP_DIM
PROJ_SPLITS = (ATTN_Q, ATTN_KV, ATTN_KV, ML_QK, ML_QK, ML_V, ML_V, ML_GATES, FOURIER_W, N_BRANCHES * D_MODEL)
PROJ_DIM = sum(PROJ_SPLITS)

kernel_name = 'hybrid_gated_flow_block'


def rmsnorm(x, g):
    xf = x.astype(jnp.float32)
    y = xf * lax.rsqrt(jnp.mean(xf * xf, axis=-1, keepdims=True) + NORM_EPS)
    return (y * g.astype(jnp.float32)).astype(x.dtype)


def split_cols(a):
    idx = [int(i) for i in np.cumsum(PROJ_SPLITS)[:-1]]
    return jnp.split(a, idx, axis=-1)


def grid_positions(n_tok):
    n_rows = n_tok // GRID_W
    row = jnp.repeat(jnp.arange(n_rows, dtype=jnp.float32), GRID_W)
    col = jnp.tile(jnp.arange(GRID_W, dtype=jnp.float32), n_rows)
    return row, col


def axial_rope(x, row, col):
    n_freq = HEAD_DIM // 4
    inv = ROPE_BASE ** (-jnp.arange(n_freq, dtype=jnp.float32) / n_freq)
    ang = jnp.concatenate([row[:, None] * inv, col[:, None] * inv], axis=-1)
    cos = jnp.cos(ang)[None, :, None, :]
    sin = jnp.sin(ang)[None, :, None, :]
    xf = x.astype(jnp.float32)
    x1, x2 = xf[..., 0::2], xf[..., 1::2]
    out = jnp.stack([x1 * cos - x2 * sin, x1 * sin + x2 * cos], axis=-1).reshape(x.shape)
    return out.astype(x.dtype)


def window_attention(q, k, v, kc, vc, sink):
    B, T = q.shape[:2]
    Lc = kc.shape[1]
    G = N_HEADS // N_KV_HEADS
    nb = T // ATTN_BLOCK
    scale = HEAD_DIM ** -0.5
    qb = q.reshape(B, nb, ATTN_BLOCK, N_KV_HEADS, G, HEAD_DIM)

    def band(a):
        ab = a.reshape(B, nb, ATTN_BLOCK, N_KV_HEADS, HEAD_DIM)
        pad = jnp.zeros_like(ab[:, :1])
        ap = jnp.concatenate([pad, ab, pad], axis=1)
        return jnp.concatenate([ap[:, :-2], ap[:, 1:-1], ap[:, 2:]], axis=2)

    kw, vw = band(k), band(v)
    blk_start = jnp.arange(nb)[:, None, None] * ATTN_BLOCK
    qpos = blk_start + jnp.arange(ATTN_BLOCK)[None, :, None]
    kpos = blk_start + jnp.arange(3 * ATTN_BLOCK)[None, None, :] - ATTN_BLOCK
    mask = (jnp.abs(kpos - qpos) <= WINDOW) & (kpos >= 0) & (kpos < T)
    s_loc = jnp.einsum('bnqhgd,bnkhd->bhgnqk', qb, kw).astype(jnp.float32) * scale
    s_loc = jnp.where(mask, s_loc, -jnp.inf)
    s_ctx = jnp.einsum('bnqhgd,bchd->bhgnqc', qb, kc).astype(jnp.float32) * scale
    s_sink = jnp.broadcast_to(sink.astype(jnp.float32).reshape(1, N_KV_HEADS, G, 1, 1, 1), s_ctx.shape[:-1] + (1,))
    p = jax.nn.softmax(jnp.concatenate([s_loc, s_ctx, s_sink], axis=-1), axis=-1).astype(v.dtype)
    nk = 3 * ATTN_BLOCK
    o = (jnp.einsum('bhgnqk,bnkhd->bnqhgd', p[..., :nk], vw)
         + jnp.einsum('bhgnqc,bchd->bnqhgd', p[..., nk:nk + Lc], vc))
    return o.reshape(B, T, ATTN_Q)


def context_attention(qc, kc, vc, sink):
    B, Lc = qc.shape[:2]
    G = N_HEADS // N_KV_HEADS
    qg = qc.reshape(B, Lc, N_KV_HEADS, G, HEAD_DIM)
    s = jnp.einsum('bqhgd,bkhd->bhgqk', qg, kc).astype(jnp.float32) * HEAD_DIM ** -0.5
    s_sink = jnp.broadcast_to(sink.astype(jnp.float32).reshape(1, N_KV_HEADS, G, 1, 1), s.shape[:-1] + (1,))
    p = jax.nn.softmax(jnp.concatenate([s, s_sink], axis=-1), axis=-1)[..., :Lc].astype(vc.dtype)
    o = jnp.einsum('bhgqk,bkhd->bqhgd', p, vc)
    return o.reshape(B, Lc, ATTN_Q)


def fourier_mix(u):
    B, T = u.shape[:2]
    ug = u.reshape(B, T, FOURIER_GROUPS, FOURIER_GROUP_DIM).astype(jnp.float32)
    f = jnp.fft.fft2(ug, axes=(1, 3), norm='ortho').real
    return f.reshape(B, T, FOURIER_W).astype(u.dtype)


def mlstm_inputs(mq, mk, mv, mg, gate_b):
    B, T = mq.shape[:2]
    f32 = jnp.float32
    q = mq.reshape(B, T, MLSTM_HEADS, MLSTM_DK).transpose(0, 2, 1, 3).astype(f32) * MLSTM_DK ** -0.5
    k = mk.reshape(B, T, MLSTM_HEADS, MLSTM_DK).transpose(0, 2, 1, 3).astype(f32)
    v = mv.reshape(B, T, MLSTM_HEADS, MLSTM_DV).transpose(0, 2, 1, 3).astype(f32)
    g = (mg.reshape(B, T, 2, 2, MLSTM_HEADS).astype(f32) + gate_b.astype(f32)).transpose(2, 3, 0, 4, 1)
    ig = g[:, 0]
    lf = jax.nn.log_sigmoid(g[:, 1])
    return q, k, v, ig, lf


def mlstm_zero_state(B):
    return (jnp.zeros((B, MLSTM_HEADS, MLSTM_DK, MLSTM_DV), jnp.float32),
            jnp.zeros((B, MLSTM_HEADS, MLSTM_DK), jnp.float32),
            jnp.zeros((B, MLSTM_HEADS), jnp.float32))


def mlstm_chunk_scan(q, k, v, ig, lf, state):
    B, H, T, _ = q.shape
    L = MLSTM_CHUNK
    nc = T // L
    tril = jnp.tril(jnp.ones((L, L), bool))

    def to_chunks(a):
        return jnp.moveaxis(a.reshape(B, H, nc, L, *a.shape[3:]), 2, 0)

    def step(carry, inp):
        C, n, m = carry
        qc, kc, vc, ic, fc = inp
        b = jnp.cumsum(fc, axis=-1)
        log_d = jnp.where(tril, b[..., :, None] - b[..., None, :] + ic[..., None, :], -jnp.inf)
        inter = b + m[..., None]
        m_t = jnp.maximum(inter, jnp.max(log_d, axis=-1))
        s = jnp.einsum('bhtd,bhsd->bhts', qc, kc) * jnp.exp(log_d - m_t[..., None])
        w_inter = jnp.exp(inter - m_t)
        num = jnp.einsum('bhts,bhsv->bhtv', s, vc) + w_inter[..., None] * jnp.einsum('bhtd,bhdv->bhtv', qc, C)
        den = jnp.sum(s, axis=-1) + w_inter * jnp.einsum('bhtd,bhd->bht', qc, n)
        h = num / jnp.maximum(jnp.abs(den), jnp.exp(-m_t))[..., None]
        b_last = b[..., -1]
        log_w = b_last[..., None] - b + ic
        m_new = jnp.maximum(b_last + m, jnp.max(log_w, axis=-1))
        w = jnp.exp(log_w - m_new[..., None])
        decay = jnp.exp(b_last + m - m_new)
        C_new = decay[..., None, None] * C + jnp.einsum('bhs,bhsd,bhsv->bhdv', w, kc, vc)
        n_new = decay[..., None] * n + jnp.einsum('bhs,bhsd->bhd', w, kc)
        return (C_new, n_new, m_new), h

    final, hs = lax.scan(step, state, (to_chunks(q), to_chunks(k), to_chunks(v), to_chunks(ig), to_chunks(lf)))
    return jnp.moveaxis(hs, 0, 2).reshape(B, H, T, MLSTM_DV), final


def mlstm_output(h_sum, mo, norm_g):
    B, H, T, DV = h_sum.shape
    hh = h_sum.transpose(0, 2, 1, 3)
    hn = hh * lax.rsqrt(jnp.mean(hh * hh, axis=-1, keepdims=True) + NORM_EPS)
    hn = hn.reshape(B, T, ML_V) * norm_g.astype(jnp.float32)
    return (jax.nn.sigmoid(mo.astype(jnp.float32)) * hn).astype(mo.dtype)


def merge_branches(gp, a, f, m, b_gate, w_br_attn, w_br_four, w_br_mlstm, w_out):
    B, T = gp.shape[:2]
    g = jax.nn.sigmoid(gp.reshape(B, T, N_BRANCHES, D_MODEL) + b_gate)
    y = (g[:, :, 0] * (a @ w_br_attn) + g[:, :, 1] * (f @ w_br_four) + g[:, :, 2] * (m @ w_br_mlstm))
    return y @ w_out


def mixer(h, hc, need_ctx, w_in, q_norm_g, k_norm_g, attn_sink, ml_gate_b, ml_norm_g,
          w_br_attn, w_br_four, w_br_mlstm, b_gate, w_out):
    B, T = h.shape[:2]
    Lc = hc.shape[1]
    q, k, v, mq, mk, mv, mo, mg, fu, gp = split_cols(h @ w_in)
    qc, kc, vc, mqc, mkc, mvc, moc, mgc, fuc, gpc = split_cols(hc @ w_in)

    row, col = grid_positions(T)
    q_l = axial_rope(rmsnorm(q.reshape(B, T, N_HEADS, HEAD_DIM), q_norm_g), row, col)
    k_l = axial_rope(rmsnorm(k.reshape(B, T, N_KV_HEADS, HEAD_DIM), k_norm_g), row, col)
    v_l = v.reshape(B, T, N_KV_HEADS, HEAD_DIM)
    k_c = rmsnorm(kc.reshape(B, Lc, N_KV_HEADS, HEAD_DIM), k_norm_g)
    v_c = vc.reshape(B, Lc, N_KV_HEADS, HEAD_DIM)
    a_lat = window_attention(q_l, k_l, v_l, k_c, v_c, attn_sink)

    f_lat = fourier_mix(fu)

    flip = lambda a: jnp.flip(a, axis=2)
    lq, lk, lv, lig, llf = mlstm_inputs(mq, mk, mv, mg, ml_gate_b)
    cq, ck, cv, cig, clf = mlstm_inputs(mqc, mkc, mvc, mgc, ml_gate_b)
    st0 = mlstm_zero_state(B)
    h_cf, st_f = mlstm_chunk_scan(cq, ck, cv, cig[0], clf[0], st0)
    h_cb, st_b = mlstm_chunk_scan(flip(cq), flip(ck), flip(cv), flip(cig[1]), flip(clf[1]), st0)
    h_lf, _ = mlstm_chunk_scan(lq, lk, lv, lig[0], llf[0], st_f)
    h_lb, _ = mlstm_chunk_scan(flip(lq), flip(lk), flip(lv), flip(lig[1]), flip(llf[1]), st_b)
    m_lat = mlstm_output(h_lf + flip(h_lb), mo, ml_norm_g)

    y = merge_branches(gp, a_lat, f_lat, m_lat, b_gate, w_br_attn, w_br_four, w_br_mlstm, w_out)
    if not need_ctx:
        return y, None
    q_c = rmsnorm(qc.reshape(B, Lc, N_HEADS, HEAD_DIM), q_norm_g)
    a_c = context_attention(q_c, k_c, v_c, attn_sink)
    f_c = fourier_mix(fuc)
    m_c = mlstm_output(h_cf + flip(h_cb), moc, ml_norm_g)
    yc = merge_branches(gpc, a_c, f_c, m_c, b_gate, w_br_attn, w_br_four, w_br_mlstm, w_out)
    return y, yc


def grouped_expert_ffn(xt, eid, wts, w1, w3, w2):
    N, D = xt.shape
    M = N * TOP_K
    flat_e = eid.reshape(M).astype(jnp.int32)
    flat_tok = jnp.repeat(jnp.arange(N, dtype=jnp.int32), TOP_K)
    flat_w = wts.reshape(M)
    order = jnp.argsort(flat_e)
    se, stok, sw = flat_e[order], flat_tok[order], flat_w[order]
    counts = jnp.bincount(flat_e, length=N_EXPERTS).astype(jnp.int32)
    padded = (counts + MOE_BLOCK - 1) // MOE_BLOCK * MOE_BLOCK
    start = jnp.cumsum(counts) - counts
    pend = jnp.cumsum(padded)
    pstart = pend - padded
    dest = pstart[se] + jnp.arange(M, dtype=jnp.int32) - start[se]
    n_blocks = -(-M // MOE_BLOCK) + N_EXPERTS
    buf = jnp.zeros((n_blocks * MOE_BLOCK, D), xt.dtype).at[dest].set(xt[stok])
    block_e = jnp.minimum(jnp.searchsorted(pend, jnp.arange(n_blocks, dtype=jnp.int32) * MOE_BLOCK, side='right'),
                          N_EXPERTS - 1)

    def expert_block(args):
        xb, e = args
        return (jax.nn.silu(xb @ w1[e]) * (xb @ w3[e])) @ w2[e]

    yb = lax.map(expert_block, (buf.reshape(n_blocks, MOE_BLOCK, D), block_e))
    y_sorted = yb.reshape(n_blocks * MOE_BLOCK, D)[dest]
    return jnp.zeros((N, D), xt.dtype).at[stok].add(sw[:, None].astype(xt.dtype) * y_sorted)


def hierarchical_moe(h, w_grp, b_grp, w_exp_router, b_exp_router, w1, w3, w2):
    B, T, D = h.shape
    xt = h.reshape(B * T, D)
    glog = (xt @ w_grp).astype(jnp.float32) + b_grp.astype(jnp.float32)
    gprob = jax.nn.softmax(glog, axis=-1)
    gsel = jnp.argmax(glog, axis=-1)
    pg = jnp.take_along_axis(gprob, gsel[:, None], axis=1)
    elog_all = jnp.einsum('nd,dge->nge', xt, w_exp_router).astype(jnp.float32) + b_exp_router.astype(jnp.float32)
    elog = jnp.take_along_axis(elog_all, gsel[:, None, None], axis=1)[:, 0]
    top_v, top_i = lax.top_k(elog, TOP_K)
    wts = pg * jax.nn.softmax(top_v, axis=-1)
    eid = gsel[:, None] * EXPERTS_PER_GROUP + top_i
    return grouped_expert_ffn(xt, eid, wts, w1, w3, w2).reshape(B, T, D)


def setup_inputs(seed: int = 0) -> dict:
    key = jax.random.key(seed)
    ks = jax.random.split(key, 26)
    L, D = DEPTH, D_MODEL

    def nrm(k, shape, s):
        return jax.random.normal(k, shape, jnp.float32) * s

    return {
        'x': nrm(ks[0], (BATCH, SEQ, D), 1.0),
        'c': nrm(ks[1], (BATCH, D), 1.0),
        'ctx': nrm(ks[2], (BATCH, CTX_LEN, D), 1.0),
        'c_ctx': nrm(ks[3], (D,), 1.0),
        'w_mod': nrm(ks[4], (L, D, 6 * D), 0.5 * D ** -0.5),
        'b_mod': nrm(ks[5], (L, 6 * D), 0.02),
        'norm1_g': 1.0 + nrm(ks[6], (L, D), 0.02),
        'norm2_g': 1.0 + nrm(ks[7], (L, D), 0.02),
        'w_in': nrm(ks[8], (L, D, PROJ_DIM), D ** -0.5),
        'q_norm_g': 1.0 + nrm(ks[9], (L, HEAD_DIM), 0.02),
        'k_norm_g': 1.0 + nrm(ks[10], (L, HEAD_DIM), 0.02),
        'attn_sink': nrm(ks[11], (L, N_HEADS), 0.5),
        'ml_gate_b': jnp.array([0.0, 3.0], jnp.float32).reshape(1, 1, 2, 1) + nrm(ks[12], (L, 2, 2, MLSTM_HEADS), 0.1),
        'ml_norm_g': 1.0 + nrm(ks[13], (L, ML_V), 0.02),
        'w_br_attn': nrm(ks[14], (L, ATTN_Q, D), ATTN_Q ** -0.5),
        'w_br_four': nrm(ks[15], (L, FOURIER_W, D), FOURIER_W ** -0.5),
        'w_br_mlstm': nrm(ks[16], (L, ML_V, D), ML_V ** -0.5),
        'b_gate': nrm(ks[17], (L, N_BRANCHES, D), 0.02),
        'w_out': nrm(ks[18], (L, D, D), D ** -0.5),
        'w_grp': nrm(ks[19], (L, D, N_GROUPS), D ** -0.5),
        'b_grp': nrm(ks[20], (L, N_GROUPS), 0.01),
        'w_exp_router': nrm(ks[21], (L, D, N_GROUPS, EXPERTS_PER_GROUP), D ** -0.5),
        'b_exp_router': nrm(ks[22], (L, N_GROUPS, EXPERTS_PER_GROUP), 0.01),
        'w1': nrm(ks[23], (L, N_EXPERTS, D, EXPERT_FF), D ** -0.5),
        'w3': nrm(ks[24], (L, N_EXPERTS, D, EXPERT_FF), D ** -0.5),
        'w2': nrm(ks[25], (L, N_EXPERTS, EXPERT_FF, D), EXPERT_FF ** -0.5),
    }


def reference(x, c, ctx, c_ctx, w_mod, b_mod, norm1_g, norm2_g, w_in, q_norm_g, k_norm_g, attn_sink,
              ml_gate_b, ml_norm_g, w_br_attn, w_br_four, w_br_mlstm, b_gate, w_out, w_grp, b_grp,
              w_exp_router, b_exp_router, w1, w3, w2):
    xc = ctx
    silu_c = jax.nn.silu(c)
    silu_cc = jax.nn.silu(c_ctx)
    for l in range(DEPTH):
        need_ctx = l < DEPTH - 1
        mod = silu_c @ w_mod[l] + b_mod[l]
        mod_c = silu_cc @ w_mod[l] + b_mod[l]
        sh1, sc1, g1, sh2, sc2, g2 = jnp.split(mod[:, None, :], 6, axis=-1)
        csh1, csc1, cg1, csh2, csc2, cg2 = jnp.split(mod_c, 6, axis=-1)
        h = rmsnorm(x, norm1_g[l]) * (1.0 + sc1) + sh1
        hc = rmsnorm(xc, norm1_g[l]) * (1.0 + csc1) + csh1
        y, yc = mixer(h, hc, need_ctx, w_in[l], q_norm_g[l], k_norm_g[l], attn_sink[l], ml_gate_b[l],
                      ml_norm_g[l], w_br_attn[l], w_br_four[l], w_br_mlstm[l], b_gate[l], w_out[l])
        x = x + g1 * y
        h2 = rmsnorm(x, norm2_g[l]) * (1.0 + sc2) + sh2
        x = x + g2 * hierarchical_moe(h2, w_grp[l], b_grp[l], w_exp_router[l], b_exp_router[l], w1[l], w3[l], w2[l])
        if need_ctx:
            xc = xc + cg1 * yc
            hc2 = rmsnorm(xc, norm2_g[l]) * (1.0 + csc2) + csh2
            xc = xc + cg2 * hierarchical_moe(hc2, w_grp[l], b_grp[l], w_exp_router[l], b_exp_router[l],
                                             w1[l], w3[l], w2[l])
    return x
```

```python
import contextlib
import numpy as np
import ml_dtypes
import concourse.bass as bass
import concourse.mybir as mybir
from concourse.bass_utils import run_bass_kernel_spmd

F32 = mybir.dt.float32
BF16 = mybir.dt.bfloat16
I32 = mybir.dt.int32
AF = mybir.ActivationFunctionType
ALU = mybir.AluOpType
AX = mybir.AxisListType
NPBF = ml_dtypes.bfloat16

D = 2048
KC = 16
NCTX = 256
NLAT = 4096
NT = NCTX + NLAT
NB = NT // 128
NE = 32
CAP = 384
NBLK = 132
RB = 256
NB2 = 66
EPS = 1e-6
NFM = 10496
NTM = 1824
BIGNEG = -30000.0


class Tok:
    __slots__ = ("w", "r", "dsem", "persist", "old")

    def __init__(self, persist=False):
        self.w = None
        self.r = []
        self.dsem = None
        self.persist = persist


class DSem:
    __slots__ = ("h", "total", "bg")

    def __init__(self, h):
        self.h = h
        self.total = 0
        self.bg = False


class Sched:
    ENG = ("pe", "act", "dve", "pool", "sp")
    EPOCH = 30000
    DMAX = 60000

    def __init__(self, nc, stack):
        self.nc = nc
        self.stack = stack
        self.e = {"pe": nc.tensor, "act": nc.scalar, "dve": nc.vector, "pool": nc.gpsimd, "sp": nc.sync}
        self.csem, self.ccnt, self.prev = {}, {}, {}
        self.nsem = 0
        for k in self.ENG:
            self.prev[k] = None
            self._new_epoch(k)
        self.seen = {k: {} for k in self.ENG}
        self.free_dsems = []
        self.retired = []
        self.all_dsems = []
        self.phase_toks = []
        self.n_inst = 0
        self.npe = 0

    def _new_epoch(self, k):
        if k in self.csem:
            self.prev[k] = (self.csem[k], self.ccnt[k])
        self.nsem += 1
        self.csem[k] = self.stack.enter_context(self.nc.semaphore(f"cs{self.nsem}"))
        self.ccnt[k] = 0

    def tok(self, persist=False):
        t = Tok(persist)
        if not persist:
            self.phase_toks.append(t)
        return t

    def _dsem(self, tok, q=None):
        if tok.dsem is not None and tok.dsem.total >= self.DMAX and q is not None:
            old = tok.dsem
            self._wait_hv(q, old.h, old.total)
            tok.old = getattr(tok, "old", [])
            tok.dsem = None
            self.retired.append(old)
        if tok.dsem is None:
            while self.free_dsems and self.free_dsems[-1].total >= self.DMAX - 64:
                self.free_dsems.pop()
            if self.free_dsems:
                tok.dsem = self.free_dsems.pop()
            else:
                self.nsem += 1
                tok.dsem = DSem(self.stack.enter_context(self.nc.semaphore(f"ds{self.nsem}")))
                self.all_dsems.append(tok.dsem)
        return tok.dsem

    def _wait_hv(self, eng, h, val):
        if val <= 0:
            return
        sd = self.seen[eng]
        if sd.get(id(h), 0) >= val:
            return
        sd[id(h)] = val
        self.e[eng].wait_ge(h, val)
        self.n_inst += 1

    def _wait(self, eng, dep):
        if dep is None:
            return
        if dep[0] == "c":
            self._wait_hv(eng, dep[2], dep[3])
        else:
            self._wait_hv(eng, dep[1].h, dep[1].total)

    def _deps(self, eng, reads, writes, accum=False):
        for t in reads:
            self._wait(eng, t.w)
        for t in writes:
            if not (accum and eng == "pe" and t.w is not None and t.w[0] == "c" and t.w[1] == "pe"):
                self._wait(eng, t.w)
            for d in t.r:
                self._wait(eng, d)

    def _post(self, dep, reads, writes):
        for t in reads:
            if len(t.r) > 6:
                t.r = t.r[-6:] if False else t.r
            t.r.append(dep)
        for t in writes:
            t.w = dep
            t.r = []

    def threads(self, bodies, ways=2):
        for g0 in range(0, len(bodies), ways):
            grp = bodies[g0:g0 + ways]
            recs = []
            for slot, b in enumerate(grp):
                self.rec = []
                b(slot)
                recs.append(self.rec)
                self.rec = None
            for k in range(max(len(r) for r in recs)):
                for r in recs:
                    if k < len(r):
                        kind, a, kw = r[k]
                        getattr(self, kind)(*a, **kw)

    def op(self, eng, fn, reads=(), writes=(), accum=False):
        if getattr(self, "rec", None) is not None:
            self.rec.append(("op", (eng, fn), dict(reads=list(reads), writes=list(writes), accum=accum)))
            return None
        self._deps(eng, reads, writes, accum)
        ins = fn(self.e[eng])
        if eng == "pe":
            self.npe += 1
        if self.ccnt[eng] >= self.EPOCH:
            self._new_epoch(eng)
        self.ccnt[eng] += 1
        h, val = self.csem[eng], self.ccnt[eng]
        ins.then_inc(h, 1)
        dep = ("c", eng, h, val)
        for t in reads:
            t.r = [d for d in t.r if not (d[0] == "c" and d[1] == eng and d[2] is h)]
        self._post(dep, reads, writes)
        self.n_inst += 1
        return ins

    def dma(self, q, out, in_, reads=(), writes=(), pace=(), **kw):
        if getattr(self, "rec", None) is not None:
            self.rec.append(("dma", (q, out, in_), dict(reads=list(reads), writes=list(writes), pace=list(pace), **kw)))
            return None
        ds = self._dsem(writes[0], q)
        for t in pace:
            self._wait(q, t.w)
        for t in reads:
            self._wait(q, t.w)
        for t in writes:
            if not (t.w is not None and t.w[0] == "d" and t.w[1] is ds):
                self._wait(q, t.w)
            for d in t.r:
                self._wait(q, d)
        ins = self.e[q].dma_start(out=out, in_=in_, **kw)
        ins.then_inc(ds.h, 16)
        ds.total += 16
        dep = ("d", ds)
        for t in reads:
            t.r = [d for d in t.r if not (d[0] == "d" and d[1] is ds)]
        self._post(dep, reads, writes)
        self.n_inst += 1
        return ins

    def indirect(self, out, out_off, in_, in_off, bound, reads=(), writes=()):
        if getattr(self, "rec", None) is not None:
            self.rec.append(("indirect", (out, out_off, in_, in_off, bound), dict(reads=list(reads), writes=list(writes))))
            return None
        ds = self._dsem(writes[0], "pool")
        self._deps("pool", reads, writes)
        if not hasattr(self, "_breg"):
            self._breg = {}
        if bound not in self._breg:
            self._breg[bound] = self.nc.gpsimd.to_reg(bound)
        ins = self.nc.gpsimd.indirect_dma_start(out=out, out_offset=out_off, in_=in_, in_offset=in_off,
                                                bounds_check=self._breg[bound], oob_is_err=False)
        ins.then_inc(ds.h, 16)
        ds.total += 16
        dep = ("d", ds)
        for t in reads:
            t.r = [d for d in t.r if not (d[0] == "d" and d[1] is ds)]
        self._post(dep, reads, writes)
        self.n_inst += 1
        return ins

    def wait_tok(self, eng, tok):
        self._wait(eng, tok.w)
        for d in tok.r:
            self._wait(eng, d)

    def barrier(self):
        for e in self.ENG:
            for o in self.ENG:
                if self.prev[o] is not None:
                    self._wait_hv(e, *self.prev[o])
                self._wait_hv(e, self.csem[o], self.ccnt[o])
            for ds in self.all_dsems:
                if not ds.bg:
                    self._wait_hv(e, ds.h, ds.total)
        for t in self.phase_toks:
            if t.dsem is not None:
                self.free_dsems.append(t.dsem)
                t.dsem = None
            t.w = None
            t.r = []
        self.phase_toks = []


def _host_consts():
    c = {}
    i = np.arange(128)
    c["ident_f"] = np.eye(128, dtype=np.float32)
    c["ones_f"] = np.ones((128, 128), np.float32)
    bo = np.zeros((128, 128), np.float32)
    bo[:64, :64] = 1.0
    bo[64:, 64:] = 1.0
    c["blockones"] = bo
    ps = np.zeros((128, 128), np.float32)
    ps[i ^ 1, i] = 1.0
    c["pswap"] = ps
    c["Mf"] = (i[:, None] <= i[None, :]).astype(np.float32)
    c["Mb"] = (i[:, None] >= i[None, :]).astype(np.float32)
    c["maskf"] = np.where(i[:, None] <= i[None, :], 0.0, BIGNEG).astype(np.float32)
    c["maskb"] = np.where(i[:, None] >= i[None, :], 0.0, BIGNEG).astype(np.float32)
    mL = np.where(i[:, None] >= i[None, :], 0.0, 8 * BIGNEG).astype(np.float32)
    mR = np.where(i[:, None] <= i[None, :], 0.0, 8 * BIGNEG).astype(np.float32)
    c["amaskL"] = np.tile(mL, (1, 4))
    c["amaskR"] = np.tile(mR, (1, 4))
    c["iota32"] = np.tile(np.arange(32, dtype=np.float32)[None, :], (128, 1))
    c["Ltri"] = (i[:, None] < i[None, :]).astype(np.float32)
    c["iota128"] = np.tile(np.arange(128, dtype=np.float32)[None, :], (128, 1))
    c["hpc"] = np.stack([np.arange(128, dtype=np.float32), 128.0 + np.arange(128, dtype=np.float32)], axis=1)
    t = np.arange(NLAT)
    row = (t // 64).astype(np.float64)
    col = (t % 64).astype(np.float64)
    inv = 10000.0 ** (-np.arange(16) / 16.0)
    ang = np.concatenate([row[:, None] * inv, col[:, None] * inv], -1)
    cos = np.cos(ang).astype(np.float32)
    sin = np.sin(ang).astype(np.float32)
    dd = np.arange(128) % 64
    fi = dd // 2
    sgn = np.where(dd % 2 == 0, -1.0, 1.0).astype(np.float32)
    c["ropec"] = np.ascontiguousarray(cos[:, fi].T)
    c["ropes"] = np.ascontiguousarray((sin[:, fi] * sgn[None, :]).T)
    cc = np.arange(256)
    a = 2 * np.pi * np.outer(cc, cc) / 256.0
    c["cosC"] = np.ascontiguousarray(np.cos(a).reshape(2, 128, 256).transpose(1, 0, 2)).astype(NPBF)
    c["sinC"] = np.ascontiguousarray(np.sin(a).reshape(2, 128, 256).transpose(1, 0, 2)).astype(NPBF)
    tt = np.arange(NLAT, dtype=np.int64)
    ph = (np.outer(tt, tt) % NLAT).astype(np.float64) * (2 * np.pi / NLAT)
    ct = (np.cos(ph) / 1024.0).astype(np.float32)
    st = (-np.sin(ph) / 1024.0).astype(np.float32)
    c["cosT"] = np.ascontiguousarray(ct.reshape(32, 128, 16, 256).transpose(2, 1, 0, 3)).astype(NPBF)
    c["sinT"] = np.ascontiguousarray(st.reshape(32, 128, 16, 256).transpose(2, 1, 0, 3)).astype(NPBF)
    tc_ = np.arange(NCTX)
    phc = (np.outer(tc_, tc_) % NCTX).astype(np.float64) * (2 * np.pi / NCTX)
    c["cosTc"] = np.ascontiguousarray((np.cos(phc) / 256.0).reshape(2, 128, 256).transpose(1, 0, 2)).astype(NPBF)
    c["sinTc"] = np.ascontiguousarray((-np.sin(phc) / 256.0).reshape(2, 128, 256).transpose(1, 0, 2)).astype(NPBF)
    return c


def _fm(v):
    v = np.asarray(v, np.float32)
    return np.ascontiguousarray(v.reshape(-1, 128).T)


_DT = {np.dtype(np.float32): F32, np.dtype(NPBF): BF16, np.dtype(np.int32): I32}


class Prog:
    def __init__(self, nc, in_shapes, n_layers=2, stop_after=None, debug=()):
        self.nc = nc
        self.L = n_layers
        self.stop_after = stop_after
        self.debug = set(debug)
        self.I = {}
        for name, (shape, dt) in in_shapes.items():
            self.I[name] = nc.dram_tensor(name, list(shape), _DT[np.dtype(dt)], kind="ExternalInput").ap()
        self.out = nc.dram_tensor("out", [D, NLAT], F32, kind="ExternalOutput").ap()

    def dram(self, name, shape, dt):
        kind = "ExternalOutput" if name in self.debug else "Internal"
        return self.nc.dram_tensor(name, list(shape), dt, kind=kind).ap()

    def sb(self, name, shape, dt=F32):
        self._n += 1
        return self.ph.enter_context(self.nc.sbuf_tensor(f"{name}_{self._n}", list(shape), dt))

    def ps(self, name, shape, dt=F32):
        self._n += 1
        return self.ph.enter_context(self.nc.psum_tensor(f"{name}_{self._n}", list(shape), dt))

    @contextlib.contextmanager
    def phase(self):
        old = getattr(self, "ph", None)
        with contextlib.ExitStack() as st:
            self.ph = st
            yield
            self.S.barrier()
        self.ph = old

    def build(self):
        nc = self.nc
        self._n = 0
        with contextlib.ExitStack() as top:
            self.S = S = Sched(nc, top)
            self.phase_log = []
            self.ph = top
            I = self.I
            self.XT = self.dram("XT", [D, NT], F32)
            self.X1T = self.dram("X1T", [D, NT], F32)
            self.QK32 = self.dram("QK32", [1280, NT], F32)
            self.PFM = self.dram("PFM", [9216, NT], BF16)
            self.PTM = self.dram("PTM", [NT, 1792], BF16)
            self.MG = self.dram("MG", [NT, 32], F32)
            self.QT = self.dram("QT", [1280, NT], BF16)
            self.AT = self.dram("AT", [1024, NT], BF16)
            self.FT = self.dram("FT", [1024, NT], BF16)
            self.MT = self.dram("MT", [1024, NT], BF16)
            self.HD = [self.dram(f"HD{d}", [1024, NT], F32) for d in range(2)]
            self.PP = self.dram("PP", [NT, 1024], BF16)
            self.QQ = self.dram("QQ", [NT, 1024], BF16)
            self.YT = self.dram("YT", [D, NT], BF16)
            self.XS = self.dram("XS", [NBLK * 128, D], BF16)
            self.H2 = self.dram("H2", [NT, D], BF16)
            self.YS = self.dram("YS", [NBLK * 128, D], F32)
            self._try("after_dram")
            self.WB = {k: self.dram(f"{k}B", [NE * 2 * 128, 8192], BF16) for k in ("w1", "w3", "w2")}
            self.tkWB = {k: S.tok(True) for k in ("w1", "w3", "w2")}
            for k in self.tkWB:
                S._dsem(self.tkWB[k]).bg = True
            self.tk = {n: S.tok(True) for n in ("XT", "X1T", "QK32", "PFM", "PTM", "MG", "QT", "AT", "FT", "MT", "HD0", "HD1",
                                                "PP", "QQ", "YT", "XS", "YS", "OUT", "H2")}
            self.C = {}
            self.tC = S.tok(True)
            for n in ("ident_f", "ones_f", "blockones", "pswap", "Mf", "Mb", "maskf", "maskb", "iota32", "Ltri"):
                t = self.sb(n, I[n].shape, F32)
                S.dma("sp", t[:], I[n], writes=[self.tC])
                self.C[n] = t
            for n, src in (("ident_b", "ident_f"), ("ones_b", "ones_f"), ("Ltri_b", "Ltri")):
                t = self.sb(n, [128, 128], BF16)
                S.op("dve", lambda e, t=t, src=src: e.tensor_copy(t[:], self.C[src][:]), reads=[self.tC], writes=[self.tC])
                self.C[n] = t
            self._try("after_consts")
            self.modT = self.sb("modT", [128, 96, 2], F32)
            self.A1 = self.sb("A1", [128, 16, 2], F32)
            self.A2 = self.sb("A2", [128, 16, 2], F32)
            self.tmod = S.tok(True)
            self.DEST = self.sb("DEST", [128, NB, 2], I32)
            self.WT = self.sb("WTS", [128, NB, 2], F32)
            self.troute = S.tok(True)
            self.BE = self.sb("BE", [128, 100], F32)
            self.BIDX = self.sb("BIDX", [128, 100, 2], I32)
            for r in range(16):
                S.dma("sp", self.XT[r * 128:(r + 1) * 128, :], I["xT0"][r * 128:(r + 1) * 128, :], writes=[self.tk["XT"]])
            self._try("before_barrier0")
            S.barrier()
            self._try("after_barrier0")
            for l in range(self.L):
                need_ctx = l < self.L - 1
                last = l == self.L - 1
                for name, fn in (("mod", self.ph_mod), ("proj", self.ph_proj), ("qk", self.ph_qkprep), ("attn", self.ph_attn),
                                 ("four", self.ph_fourier), ("mlstm", self.ph_mlstm), ("mlout", self.ph_mlout),
                                 ("mergea", self.ph_merge_a), ("mergeb", self.ph_merge_b), ("route", self.ph_route),
                                 ("moe", self.ph_moe), ("comb", self.ph_combine)):
                    import os
                    if "ONLY_PHASES" in os.environ and name not in os.environ["ONLY_PHASES"].split(","):
                        continue
                    with self.nc.named_scope(f"L{l}_{name}"):
                        with self.phase():
                            fn(l, need_ctx, last)
                    self.phase_log.append((l, name, self.S.npe))
                    if self.stop_after == (l, name):
                        break
                else:
                    continue
                break
            if self.stop_after is not None:
                S.dma("sp", self.out[0:128, 0:128], self.XT[0:128, 0:128], reads=[self.tk["XT"]], writes=[self.tk["OUT"]])
            for t in self.tk.values():
                S.wait_tok("sp", t)
            S.barrier()
            print("instructions:", S.n_inst, "sems:", S.nsem)
            import os
            if "PHASELOG" in os.environ:
                print("PHASELOG", self.phase_log)

    def _try(self, tag):
        import os
        if "TRYDBG" not in os.environ:
            return
        if not hasattr(self, "_tix"):
            self._tix = self.nc.alloc_sbuf_tensor("tix", [128, 1], I32) if False else None
        try:
            self._n += 1
            with self.nc.sbuf_tensor(f"tix{self._n}", [128, 1], I32) as ix, self.nc.sbuf_tensor(f"tH{self._n}", [128, 2048], BF16) as H:
                self.nc.gpsimd.indirect_dma_start(out=self.XS[:, :], out_offset=bass.IndirectOffsetOnAxis(ap=ix[:, :], axis=0), in_=H[:, :], in_offset=None, bounds_check=NE * CAP - 1, oob_is_err=False)
            print("TRY ok", tag)
        except Exception as e:
            print("TRY FAIL", tag, e)

    def ph_mod(self, l, need_ctx, last):
        S, I = self.S, self.I
        sT = self.sb("sT", [128, 16, 2]); tsT = S.tok()
        S.dma("sp", sT[:], I["cT"], writes=[tsT])
        S.op("act", lambda e: e.activation(sT[:], sT[:], AF.Silu), reads=[tsT], writes=[tsT])
        bm = self.sb("bm", [128, 96]); tbm = S.tok()
        S.dma("sp", bm[:], I[f"bmod{l}"], writes=[tbm])
        W = [self.sb("Wm", [128, 16, 512]) for _ in range(2)]
        tW = [S.tok() for _ in range(2)]
        P = [self.ps("pm", [128, 4, 2]) for _ in range(2)]
        tP = [S.tok() for _ in range(2)]
        wsrc = I[f"w_mod{l}"].rearrange("(k p) n -> p k n", p=128)

        def load(t):
            S.dma("sp", W[t % 2][:], wsrc[:, :, t * 512:(t + 1) * 512], writes=[tW[t % 2]])
        load(0)
        for t in range(24):
            if t + 1 < 24:
                load(t + 1)
            w, p = W[t % 2], P[t % 2]
            for j in range(4):
                for kc in range(16):
                    S.op("pe", lambda e, j=j, kc=kc: e.matmul(p[:, j, :], w[:, kc, j * 128:(j + 1) * 128], sT[:, kc, :],
                                                              start=(kc == 0), stop=(kc == 15)),
                         reads=[tW[t % 2], tsT], writes=[tP[t % 2]], accum=True)
            S.op("dve", lambda e: e.tensor_tensor(self.modT[:, t * 4:(t + 1) * 4, :], p[:],
                                                  bm[:, t * 4:(t + 1) * 4].unsqueeze(2).broadcast_to([128, 4, 2]), op=ALU.add),
                 reads=[tP[t % 2], tbm], writes=[self.tmod])
        ng = self.sb("ng", [128, 2, 16]); tng = S.tok()
        S.dma("sp", ng[:], I[f"ng{l}"], writes=[tng])
        for A, off, gi in ((self.A1, 16, 0), (self.A2, 64, 1)):
            S.op("dve", lambda e, A=A, off=off: e.tensor_scalar(A[:], self.modT[:, off:off + 16, :], 1.0, None, op0=ALU.add),
                 reads=[self.tmod], writes=[self.tmod])
            S.op("dve", lambda e, A=A, gi=gi: e.tensor_tensor(A[:], A[:], ng[:, gi, :].unsqueeze(2).broadcast_to([128, 16, 2]), op=ALU.mult),
                 reads=[self.tmod, tng], writes=[self.tmod])

    def norm_block(self, src, tsrc, c0, n, A, Bofs, j, X, SQ, rs, tmp, pss, toks, out_fn):
        S = self.S
        tX, tSQ, trs, ttmp, tps = toks
        S.dma("sp", X[:, :, 0:n], src.rearrange("(k p) t -> p k t", p=128)[:, :, c0:c0 + n], reads=[tsrc], writes=[tX])
        S.op("act", lambda e: e.activation(SQ[:, :, 0:n], X[:, :, 0:n], AF.Square), reads=[tX], writes=[tSQ])
        for kc in range(16):
            S.op("pe", lambda e, kc=kc: e.matmul(pss[:, 0:n], self.C["ones_f"][:], SQ[:, kc, 0:n], start=(kc == 0), stop=(kc == 15)),
                 reads=[tSQ, self.tC], writes=[tps], accum=True)
        S.op("dve", lambda e: e.tensor_scalar(rs[:, 0:n], pss[:, 0:n], 1.0 / D, EPS, op0=ALU.mult, op1=ALU.add), reads=[tps], writes=[trs])
        S.op("act", lambda e: e.activation(rs[:, 0:n], rs[:, 0:n], AF.Sqrt), reads=[trs], writes=[trs])
        S.op("dve", lambda e: e.reciprocal(rs[:, 0:n], rs[:, 0:n]), reads=[trs], writes=[trs])
        for kc in range(16):
            S.op("dve", lambda e, kc=kc: e.scalar_tensor_tensor(tmp[:, 0:n], X[:, kc, 0:n], A[:, kc, j:j + 1], rs[:, 0:n],
                                                                op0=ALU.mult, op1=ALU.mult),
                 reads=[tX, trs, self.tmod], writes=[ttmp])
            out_fn(kc, tmp[:, 0:n], self.modT[:, Bofs + kc, j:j + 1], ttmp)

    def ph_proj(self, l, need_ctx, last):
        S, I = self.S, self.I
        hT = self.sb("hT", [128, 16, 1024], BF16); thT = S.tok()
        X = self.sb("X", [128, 16, 512]); SQ = self.sb("SQ", [128, 16, 512]); rs = self.sb("rs", [128, 512]); tmp = self.sb("tmp", [128, 512])
        pss = self.ps("pss", [128, 512])
        ntoks = [S.tok() for _ in range(5)]
        W = [self.sb("W", [128, 16, 512], BF16) for _ in range(2)]
        tW = [S.tok() for _ in range(2)]
        PS = [self.ps("pp", [128, 512]) for _ in range(4)]
        tPS = [S.tok() for _ in range(4)]
        OB = [self.sb("ob", [128, 512], BF16) for _ in range(3)]
        OF = [self.sb("of", [128, 512], F32) for _ in range(3)]
        tOB = [S.tok() for _ in range(3)]
        tOF = [S.tok() for _ in range(3)]
        wfm = I[f"w_fm{l}"].rearrange("(k p) n -> p k n", p=128)
        wtm = I[f"w_tm{l}"].rearrange("(k p) n -> p k n", p=128)
        fm_tiles = [(t * 512, 512) for t in range(20)] + [(10240, 256)]
        tm_tiles = [(0, 512), (512, 512), (1024, 512), (1536, 512)]
        cnt = {"ps": 0, "ob": 0, "of": 0, "w": 0}
        import os
        sbs = [(0, 256, 1)] + [(256 + i * 1024, 1024, 0) for i in range(4)]
        sbs = sbs[:int(os.environ.get("PROJ_SB", "5"))]
        for (t0, nt, j) in sbs:
            for s0 in range(0, nt, 512):
                n = min(512, nt - s0)

                def outf(kc, tap, bias, ttmp, s0=s0, n=n):
                    S.op("act", lambda e: e.activation(hT[:, kc, s0:s0 + n], tap, AF.Identity, bias=bias, scale=1.0),
                         reads=[ttmp, self.tmod], writes=[thT])
                self.norm_block(self.XT, self.tk["XT"], t0 + s0, n, self.A1, 0, j, X, SQ, rs, tmp, pss, ntoks, outf)
            tiles = [("fm", c0, w) for c0, w in fm_tiles] + [("tm", c0, w) for c0, w in tm_tiles]
            if "PROJ_T0" in os.environ:
                tiles = tiles[int(os.environ["PROJ_T0"]):int(os.environ["PROJ_T1"])]

            def load(i):
                kind, c0, w = tiles[i]
                src = wfm if kind == "fm" else wtm
                S.dma("pool", W[i % 2][:, :, 0:w], src[:, :, c0:c0 + w], writes=[tW[i % 2]])
            load(0)
            for i, (kind, c0, w) in enumerate(tiles):
                if i + 1 < len(tiles):
                    load(i + 1)
                wt, twt = W[i % 2], tW[i % 2]
                if kind == "fm":
                    for jc in range(w // 128):
                        ci = (c0 + jc * 128) // 128
                        for s0 in range(0, nt, 512):
                            n = min(512, nt - s0)
                            pi = cnt["ps"] % 4; cnt["ps"] += 1
                            for kc in range(16):
                                S.op("pe", lambda e, kc=kc, jc=jc: e.matmul(PS[pi][:, 0:n], wt[:, kc, jc * 128:(jc + 1) * 128], hT[:, kc, s0:s0 + n],
                                                                          start=(kc == 0), stop=(kc == 15)),
                                     reads=[twt, thT], writes=[tPS[pi]], accum=True)
                            if ci < 10:
                                oi = cnt["of"] % 3; cnt["of"] += 1
                                S.op("act", lambda e: e.copy(OF[oi][:, 0:n], PS[pi][:, 0:n]), reads=[tPS[pi]], writes=[tOF[oi]])
                                S.dma("sp", self.QK32[ci * 128:(ci + 1) * 128, t0 + s0:t0 + s0 + n], OF[oi][:, 0:n], reads=[tOF[oi]], writes=[self.tk["QK32"]])
                            else:
                                oi = cnt["ob"] % 3; cnt["ob"] += 1
                                eng = "act" if (cnt["ob"] % 2) else "dve"
                                if 10 <= ci < 14:
                                    S.op("act", lambda e: e.mul(OB[oi][:, 0:n], PS[pi][:, 0:n], 0.125), reads=[tPS[pi]], writes=[tOB[oi]])
                                elif eng == "act":
                                    S.op("act", lambda e: e.copy(OB[oi][:, 0:n], PS[pi][:, 0:n]), reads=[tPS[pi]], writes=[tOB[oi]])
                                else:
                                    S.op("dve", lambda e: e.tensor_copy(OB[oi][:, 0:n], PS[pi][:, 0:n]), reads=[tPS[pi]], writes=[tOB[oi]])
                                r0 = (ci - 10) * 128
                                S.dma("sp", self.PFM[r0:r0 + 128, t0 + s0:t0 + s0 + n], OB[oi][:, 0:n], reads=[tOB[oi]], writes=[self.tk["PFM"]])
                else:
                    for b0 in range(0, nt, 128):
                        pi = cnt["ps"] % 4; cnt["ps"] += 1
                        for kc in range(16):
                            S.op("pe", lambda e, kc=kc: e.matmul(PS[pi][:, 0:w], hT[:, kc, b0:b0 + 128], wt[:, kc, 0:w], start=(kc == 0), stop=(kc == 15)),
                                 reads=[twt, thT], writes=[tPS[pi]], accum=True)
                        oi = cnt["ob"] % 3; cnt["ob"] += 1
                        wv = min(w, 512) if c0 < 1536 else 256
                        S.op("dve", lambda e: e.tensor_copy(OB[oi][:, 0:wv], PS[pi][:, 0:wv]), reads=[tPS[pi]], writes=[tOB[oi]])
                        S.dma("sp", self.PTM[t0 + b0:t0 + b0 + 128, c0:c0 + wv], OB[oi][:, 0:wv], reads=[tOB[oi]], writes=[self.tk["PTM"]])
                        if c0 == 1536:
                            oj = cnt["of"] % 3; cnt["of"] += 1
                            S.op("dve", lambda e: e.tensor_copy(OF[oj][:, 0:32], PS[pi][:, 256:288]), reads=[tPS[pi]], writes=[tOF[oj]])
                            S.dma("sp", self.MG[t0 + b0:t0 + b0 + 128, :], OF[oj][:, 0:32], reads=[tOF[oj]], writes=[self.tk["MG"]])

    def ph_qkprep(self, l, need_ctx, last):
        S, I = self.S, self.I
        g = self.sb("qkg", [128, 2]); tg = S.tok()
        S.dma("sp", g[:], I[f"qkg{l}"], writes=[tg])
        RC = [self.sb("rc", [128, 512]) for _ in range(2)]; RSn = [self.sb("rsn", [128, 512]) for _ in range(2)]; tR = [S.tok() for _ in range(2)]
        mk = lambda nm, dt=F32: [self.sb(nm, [128, 512], dt) for _ in range(4)]
        X, SQ, rs, xn, t1, t2, ob = mk("x"), mk("sq"), mk("rs"), mk("xn"), mk("t1"), mk("t2"), mk("ob", BF16)
        tk = lambda: [S.tok() for _ in range(4)]
        tX, tSQ, trs, txn, tt1, tt2, tob = tk(), tk(), tk(), tk(), tk(), tk(), tk()
        p1 = [self.ps("p1", [128, 512]) for _ in range(4)]; tp1 = tk()
        p2 = [self.ps("p2", [128, 512]) for _ in range(4)]; tp2 = tk()
        bodies = []
        for bidx, (t0, n) in enumerate([(0, 256)] + [(256 + i * 512, 512) for i in range(8)]):
            lat = t0 >= NCTX
            first = True
            for ci in range(10):
                if (not lat) and (not need_ctx) and ci < 8:
                    continue

                def body(sl, t0=t0, n=n, lat=lat, ci=ci, first=first, rp=bidx % 2):
                    x, tx, o, to = X[sl], tX[sl], ob[sl], tob[sl]
                    gi = 0 if ci < 8 else 1
                    if lat and first:
                        S.dma("sp", RC[rp][:, 0:n], I["ropec"][:, t0 - NCTX:t0 - NCTX + n], writes=[tR[rp]])
                        S.dma("sp", RSn[rp][:, 0:n], I["ropes"][:, t0 - NCTX:t0 - NCTX + n], writes=[tR[rp]])
                    S.dma("sp", x[:, 0:n], self.QK32[ci * 128:(ci + 1) * 128, t0:t0 + n], reads=[self.tk["QK32"]], writes=[tx])
                    S.op("act", lambda e: e.activation(SQ[sl][:, 0:n], x[:, 0:n], AF.Square), reads=[tx], writes=[tSQ[sl]])
                    S.op("pe", lambda e: e.matmul(p1[sl][:, 0:n], self.C["blockones"][:], SQ[sl][:, 0:n], start=True, stop=True), reads=[tSQ[sl], self.tC], writes=[tp1[sl]])
                    S.op("dve", lambda e: e.tensor_scalar(rs[sl][:, 0:n], p1[sl][:, 0:n], 1.0 / 64, EPS, op0=ALU.mult, op1=ALU.add), reads=[tp1[sl]], writes=[trs[sl]])
                    S.op("act", lambda e: e.activation(rs[sl][:, 0:n], rs[sl][:, 0:n], AF.Sqrt), reads=[trs[sl]], writes=[trs[sl]])
                    S.op("dve", lambda e: e.reciprocal(rs[sl][:, 0:n], rs[sl][:, 0:n]), reads=[trs[sl]], writes=[trs[sl]])
                    if lat:
                        S.op("dve", lambda e: e.scalar_tensor_tensor(xn[sl][:, 0:n], x[:, 0:n], g[:, gi:gi + 1], rs[sl][:, 0:n], op0=ALU.mult, op1=ALU.mult),
                             reads=[tx, trs[sl], tg], writes=[txn[sl]])
                        S.op("pe", lambda e: e.matmul(p2[sl][:, 0:n], self.C["pswap"][:], xn[sl][:, 0:n], start=True, stop=True), reads=[txn[sl], self.tC], writes=[tp2[sl]])
                        S.op("dve", lambda e: e.tensor_tensor(t1[sl][:, 0:n], xn[sl][:, 0:n], RC[rp][:, 0:n], op=ALU.mult), reads=[txn[sl], tR[rp]], writes=[tt1[sl]])
                        S.op("dve", lambda e: e.tensor_tensor(t2[sl][:, 0:n], p2[sl][:, 0:n], RSn[rp][:, 0:n], op=ALU.mult), reads=[tp2[sl], tR[rp]], writes=[tt2[sl]])
                        S.op("dve", lambda e: e.tensor_tensor(o[:, 0:n], t1[sl][:, 0:n], t2[sl][:, 0:n], op=ALU.add), reads=[tt1[sl], tt2[sl]], writes=[to])
                    else:
                        S.op("dve", lambda e: e.scalar_tensor_tensor(o[:, 0:n], x[:, 0:n], g[:, gi:gi + 1], rs[sl][:, 0:n], op0=ALU.mult, op1=ALU.mult),
                             reads=[tx, trs[sl], tg], writes=[to])
                    S.dma("sp", self.QT[ci * 128:(ci + 1) * 128, t0:t0 + n], o[:, 0:n], reads=[to], writes=[self.tk["QT"]])
                bodies.append(body)
                first = False
        S.threads(bodies, ways=4)

    def conv_next(self, pace_tok, k=1):
        for _ in range(k):
            if not getattr(self, "conv_q", None):
                return
            kind, r = self.conv_q.pop(0)
            self.S.dma("pool", self.WB[kind][r * 64:(r + 1) * 64, :], self.I[f"{kind}r_{self.conv_l}"][r * 64:(r + 1) * 64, :],
                       pace=[pace_tok], writes=[self.tkWB[kind]])

    def ph_attn(self, l, need_ctx, last):
        S, I = self.S, self.I
        self.conv_q = [(k, r) for r in range(128) for k in ("w1", "w3", "w2")]
        self.conv_l = l
        es = self.sb("es", [64, 16]); tes = S.tok()
        S.dma("sp", es[:], I[f"sink{l}"], writes=[tes])
        S.op("act", lambda e: e.activation(es[:], es[:], AF.Exp), reads=[tes], writes=[tes])
        mL = self.sb("mL", [128, 512]); mR = self.sb("mR", [128, 512]); tm = S.tok()
        S.dma("sp", mL[:], I["amaskL"], writes=[tm]); S.dma("sp", mR[:], I["amaskR"], writes=[tm])
        kT = self.sb("kT", [64, NT], BF16); tkT = S.tok()
        Q4 = self.sb("Q4", [64, 4, NT], BF16); tQ4 = S.tok()
        Vt = self.sb("Vt", [128, NB, 64], BF16); tVt = S.tok()
        AO = [self.sb("AO", [64, 4, 128], BF16) for _ in range(2)]; tAO = [S.tok() for _ in range(2)]
        ST = [self.ps("st", [128, 512]) for _ in range(4)]; tST = [S.tok() for _ in range(4)]
        OT = [self.ps("ot", [64, 512]) for _ in range(2)]; tOT = [S.tok() for _ in range(2)]
        DN = [self.ps("dn", [64, 512]) for _ in range(2)]; tDN = [S.tok() for _ in range(2)]
        E = [self.sb("E", [128, 512], BF16) for _ in range(3)]; tE = [S.tok() for _ in range(3)]
        tmpm = [self.sb("tmpm", [128, 512]) for _ in range(2)]; ttm = [S.tok() for _ in range(2)]
        dsum = self.sb("dsum", [64, 4, 128]); tds = S.tok()
        ke = 0; ks = 0; kq = 0
        qblocks = ([0, 1] if need_ctx else []) + list(range(2, NB))
        for hk in range(4):
            S.dma("sp", kT[:], self.QT[1024 + hk * 64:1024 + (hk + 1) * 64, :], reads=[self.tk["QT"]], writes=[tkT])
            S.dma("sp", Q4[:], self.QT[hk * 256:(hk + 1) * 256, :].rearrange("(h d) t -> d h t", d=64), reads=[self.tk["QT"]], writes=[tQ4])
            S.dma("sp", Vt[:], self.PTM[:, 1536 + hk * 64:1536 + (hk + 1) * 64].rearrange("(b p) d -> p b d", p=128),
                  reads=[self.tk["PTM"]], writes=[tVt])
            items = []
            for qi, n in enumerate(qblocks):
                if n < 2:
                    kbs = [(0, None), (1, None)]
                else:
                    kbs = []
                    if n - 1 >= 2:
                        kbs.append((n - 1, mL))
                    kbs.append((n, None))
                    if n + 1 < NB:
                        kbs.append((n + 1, mR))
                    kbs += [(0, None), (1, None)]
                for bi, (kb, msk) in enumerate(kbs):
                    items.append((qi, n, bi, kb, msk, len(kbs)))

            def emit_st(it, si):
                qi, n, bi, kb, msk, nk = it
                S.op("pe", lambda e: e.matmul(ST[si][:], kT[:, kb * 128:(kb + 1) * 128], Q4[:, :, n * 128:(n + 1) * 128], start=True, stop=True),
                     reads=[tkT, tQ4], writes=[tST[si]])
            emit_st(items[0], ks % 4)
            emit_st(items[1], (ks + 1) % 4)
            for ii, it in enumerate(items):
                qi, n, bi, kb, msk, nk = it
                si = ks % 4; ks += 1
                ei = ke % 3; ke += 1
                if ii + 2 < len(items):
                    emit_st(items[ii + 2], (ks + 1) % 4)
                if bi == 0:
                    oi = kq % 2; kq += 1
                if msk is not None:
                    S.op("dve", lambda e: e.tensor_tensor(tmpm[si % 2][:], ST[si][:], msk[:], op=ALU.add), reads=[tST[si], tm], writes=[ttm[si % 2]])
                    S.op("act", lambda e: e.activation(E[ei][:], tmpm[si % 2][:], AF.Exp, scale=0.125), reads=[ttm[si % 2]], writes=[tE[ei]])
                else:
                    S.op("act", lambda e: e.activation(E[ei][:], ST[si][:], AF.Exp, scale=0.125), reads=[tST[si]], writes=[tE[ei]])
                S.op("pe", lambda e: e.matmul(OT[oi][:], Vt[:, kb, :], E[ei][:], start=(bi == 0), stop=(bi == nk - 1)),
                     reads=[tVt, tE[ei]], writes=[tOT[oi]], accum=True)
                S.op("pe", lambda e: e.matmul(DN[oi][:], self.C["ones_b"][:, 0:64], E[ei][:], start=(bi == 0), stop=(bi == nk - 1)),
                     reads=[self.tC, tE[ei]], writes=[tDN[oi]], accum=True)
                if bi == nk - 1:
                    S.op("dve", lambda e: e.tensor_tensor(dsum[:], DN[oi][:].rearrange("p (h t) -> p h t", h=4),
                                                          es[:, hk * 4:(hk + 1) * 4].unsqueeze(2).broadcast_to([64, 4, 128]), op=ALU.add),
                         reads=[tDN[oi], tes], writes=[tds])
                    S.op("dve", lambda e: e.reciprocal(dsum[:], dsum[:]), reads=[tds], writes=[tds])
                    ao = AO[qi % 2]
                    S.op("dve", lambda e: e.tensor_tensor(ao[:], OT[oi][:].rearrange("p (h t) -> p h t", h=4), dsum[:], op=ALU.mult),
                         reads=[tOT[oi], tds], writes=[tAO[qi % 2]])
                    S.dma("sp", self.AT[hk * 256:(hk + 1) * 256, n * 128:(n + 1) * 128].rearrange("(h d) t -> d h t", d=64), ao[:], reads=[tAO[qi % 2]], writes=[self.tk["AT"]])
                    self.conv_next(tAO[qi % 2])

    def ph_fourier(self, l, need_ctx, last):
        S, I = self.S, self.I
        with contextlib.ExitStack() as st1:
            old = self.ph; self.ph = st1
            cC = self.sb("cC", [128, 2, 256], BF16); sC = self.sb("sC", [128, 2, 256], BF16); tcs = S.tok()
            S.dma("sp", cC[:], I["cosC"], writes=[tcs]); S.dma("sp", sC[:], I["sinC"], writes=[tcs])
            U = [self.sb("U", [128, 8, 128], BF16) for _ in range(2)]; tU = [S.tok() for _ in range(2)]
            PPs = [self.ps("ppp", [128, 4, 256]) for _ in range(2)]; tPP = [S.tok() for _ in range(2)]
            ob = [self.sb("ob", [128, 1024], BF16) for _ in range(2)]; tob = [S.tok() for _ in range(2)]
            blocks = ([0, 1] if need_ctx else []) + list(range(2, NB))
            for bi, b in enumerate(blocks):
                u, tu = U[bi % 2], tU[bi % 2]
                S.dma("sp", u[:], self.PFM[2048:3072, b * 128:(b + 1) * 128].rearrange("(k p) t -> p k t", p=128), reads=[self.tk["PFM"]], writes=[tu])
                for wi, (tab, dst, tkn) in enumerate(((cC, self.PP, "PP"), (sC, self.QQ, "QQ"))):
                    pp, tpp = PPs[wi], tPP[wi]
                    for g in range(4):
                        for cc in range(2):
                            S.op("pe", lambda e, g=g, cc=cc: e.matmul(pp[:, g, :], u[:, 2 * g + cc, :], tab[:, cc, :], start=(cc == 0), stop=(cc == 1)),
                                 reads=[tu, tcs], writes=[tpp], accum=True)
                    o, to = ob[wi], tob[wi]
                    if wi == 0:
                        S.op("act", lambda e: e.copy(o[:], pp[:].rearrange("p g c -> p (g c)")), reads=[tpp], writes=[to])
                    else:
                        S.op("dve", lambda e: e.tensor_copy(o[:], pp[:].rearrange("p g c -> p (g c)")), reads=[tpp], writes=[to])
                    S.dma("sp", dst[b * 128:(b + 1) * 128, :], o[:], reads=[to], writes=[self.tk[tkn]])
            S.barrier()
            self.ph = old
        PS = [self.ps("f2", [128, 256]) for _ in range(4)]; tPS = [S.tok() for _ in range(4)]
        FO = [self.sb("fo", [128, 256], BF16) for _ in range(3)]; tFO = [S.tok() for _ in range(3)]
        kp = 0; ko = 0
        if need_ctx:
            Pc = self.sb("Pc", [128, 2, 1024], BF16); Qc = self.sb("Qc", [128, 2, 1024], BF16); tpc = S.tok()
            S.dma("sp", Pc[:], self.PP[0:256, :].rearrange("(b p) c -> p b c", p=128), reads=[self.tk["PP"]], writes=[tpc])
            S.dma("sp", Qc[:], self.QQ[0:256, :].rearrange("(b p) c -> p b c", p=128), reads=[self.tk["QQ"]], writes=[tpc])
            cTc = self.sb("cTc", [128, 2, 256], BF16); sTc = self.sb("sTc", [128, 2, 256], BF16); ttc = S.tok()
            S.dma("sp", cTc[:], I["cosTc"], writes=[ttc]); S.dma("sp", sTc[:], I["sinTc"], writes=[ttc])
            for ch in range(8):
                pi = kp % 4; kp += 1
                oi = ko % 3; ko += 1
                k = 0
                for tb in range(2):
                    for (Pm, Tm) in ((Pc, cTc), (Qc, sTc)):
                        S.op("pe", lambda e, Pm=Pm, Tm=Tm, tb=tb, k=k: e.matmul(PS[pi][:], Pm[:, tb, ch * 128:(ch + 1) * 128], Tm[:, tb, :], start=(k == 0), stop=(k == 3)),
                             reads=[tpc, ttc], writes=[tPS[pi]], accum=True)
                        k += 1
                S.op("act", lambda e: e.copy(FO[oi][:], PS[pi][:]), reads=[tPS[pi]], writes=[tFO[oi]])
                S.dma("sp", self.FT[ch * 128:(ch + 1) * 128, 0:256], FO[oi][:], reads=[tFO[oi]], writes=[self.tk["FT"]])
        Ph = self.sb("Ph", [128, 32, 512], BF16); Qh = self.sb("Qh", [128, 32, 512], BF16); tph = S.tok()
        CT = [self.sb("CT", [128, 32, 256], BF16) for _ in range(2)]; STn = [self.sb("STn", [128, 32, 256], BF16) for _ in range(2)]
        tCT = [S.tok() for _ in range(2)]
        for half in range(2):
            S.dma("sp", Ph[:], self.PP[256:NT, half * 512:(half + 1) * 512].rearrange("(b p) c -> p b c", p=128), reads=[self.tk["PP"]], writes=[tph])
            S.dma("sp", Qh[:], self.QQ[256:NT, half * 512:(half + 1) * 512].rearrange("(b p) c -> p b c", p=128), reads=[self.tk["QQ"]], writes=[tph])

            def load(tq):
                S.dma("sp", CT[tq % 2][:], I["cosT"][tq], writes=[tCT[tq % 2]])
                S.dma("sp", STn[tq % 2][:], I["sinT"][tq], writes=[tCT[tq % 2]])
            load(0)
            for tq in range(16):
                if tq + 1 < 16:
                    load(tq + 1)
                for ch in range(4):
                    pi = kp % 4; kp += 1
                    oi = ko % 3; ko += 1
                    for tb in range(32):
                        S.op("pe", lambda e, tb=tb: e.matmul(PS[pi][:], Ph[:, tb, ch * 128:(ch + 1) * 128], CT[tq % 2][:, tb, :], start=(tb == 0), stop=False),
                             reads=[tph, tCT[tq % 2]], writes=[tPS[pi]], accum=True)
                    for tb in range(32):
                        S.op("pe", lambda e, tb=tb: e.matmul(PS[pi][:], Qh[:, tb, ch * 128:(ch + 1) * 128], STn[tq % 2][:, tb, :], start=False, stop=(tb == 31)),
                             reads=[tph, tCT[tq % 2]], writes=[tPS[pi]], accum=True)
                    if ch % 2:
                        S.op("act", lambda e: e.copy(FO[oi][:], PS[pi][:]), reads=[tPS[pi]], writes=[tFO[oi]])
                    else:
                        S.op("dve", lambda e: e.tensor_copy(FO[oi][:], PS[pi][:]), reads=[tPS[pi]], writes=[tFO[oi]])
                    r0 = half * 512 + ch * 128
                    S.dma("sp", self.FT[r0:r0 + 128, 256 + tq * 256:256 + (tq + 1) * 256], FO[oi][:], reads=[tFO[oi]], writes=[self.tk["FT"]])
                    self.conv_next(tFO[oi])

    def ph_mlstm(self, l, need_ctx, last):
        S, I, C = self.S, self.I, self.C
        gb = self.sb("gb", [128, 32]); tgb = S.tok()
        S.dma("sp", gb[:], I[f"mlgb{l}"], writes=[tgb])
        G = self.sb("G", [128, NB, 32]); tG = S.tok()
        S.dma("sp", G[:], self.MG.rearrange("(b p) c -> p b c", p=128), reads=[self.tk["MG"]], writes=[tG])
        S.op("dve", lambda e: e.tensor_tensor(G[:], G[:], gb[:, :].unsqueeze(1).broadcast_to([128, NB, 32]), op=ALU.add), reads=[tG, tgb], writes=[tG])
        Gv = G[:].rearrange("p b (d i h) -> p b d i h", d=2, i=2)
        LF = self.sb("LF", [128, NB, 2, 8]); tLF = S.tok()
        S.op("act", lambda e: e.activation(LF[:], Gv[:, :, :, 1, :], AF.Exp, scale=-1.0), reads=[tG], writes=[tLF])
        S.op("act", lambda e: e.activation(LF[:], LF[:], AF.Ln, bias=1.0), reads=[tLF], writes=[tLF])
        S.op("dve", lambda e: e.tensor_scalar(LF[:], LF[:], -1.0, None, op0=ALU.mult), reads=[tLF], writes=[tLF])
        two = lambda nm, shp, dt=F32: [self.sb(nm, shp, dt) for _ in range(2)]
        tk = lambda: [S.tok() for _ in range(2)]
        Cst = two("Cst", [64, 4, 129]); tCst = tk()
        Cbf = two("Cbf", [64, 4, 128], BF16); nrep = two("nrep", [64, 4, 128], BF16); tCb = tk()
        qT4 = two("qT4", [64, 4, 128], BF16); kT4 = two("kT4", [64, 4, 128], BF16)
        ktm = two("ktm", [128, 4, 64], BF16); vtm = two("vtm", [128, 4, 128], BF16); tin = tk()
        R = two("R", [128, 4, 128]); tR = tk()
        a4 = two("a4", [128, 4]); ta4 = tk()
        brs = two("brs", [128, 4, 128]); tbrs = tk()
        E1 = two("E1", [128, 4, 128]); tE1 = tk()
        Dm = two("Dm", [128, 4, 128]); tDm = tk()
        eb = two("eb", [128, 4, 128]); teb = tk()
        AT4 = two("AT4", [128, 4, 128], BF16); tAT4 = tk()
        qTs = two("qTs", [64, 4, 128], BF16); tqTs = tk()
        ad = two("ad", [128, 4, 128]); tad = tk()
        ho = two("ho", [128, 4, 128]); tho = tk()
        lw = two("lw", [128, 4]); tlw = tk()
        wk = two("wk", [128, 4, 64], BF16); twk = tk()
        psm_all = self.ps("psm", [128, 16]); tpsm = tk()
        pA = [self.ps("pA", [128, 4, 128]) for _ in range(2)]; tpA = tk()
        pB = [self.ps("pB", [128, 4, 128]) for _ in range(2)]; tpB = tk()
        pC = [self.ps("pC", [128, 4, 128]) for _ in range(2)]; tpC = tk()
        for d in range(2):
            M = C["Mf"] if d == 0 else C["Mb"]
            msk = C["maskf"] if d == 0 else C["maskb"]
            tl = 127 if d == 0 else 0
            order = [0, 1] + list(range(2, NB)) if d == 0 else [1, 0] + list(range(NB - 1, 1, -1))
            for hh in range(2):
                S.op("pool", lambda e, hh=hh: e.memset(Cst[hh][:], 0.0), writes=[tCst[hh]])
                S.op("pool", lambda e, hh=hh: e.memset(Cbf[hh][:], 0.0), writes=[tCb[hh]])
                S.op("pool", lambda e, hh=hh: e.memset(nrep[hh][:], 0.0), writes=[tCb[hh]])
            bodies = []
            for blk in order:
                for hh in range(2):
                    def body(sl, blk=blk, hh=hh, d=d, M=M, msk=msk, tl=tl):
                        assert sl == hh
                        c0 = blk * 128
                        hs = slice(hh * 4, hh * 4 + 4)
                        q4, k4, kt, vt, ti = qT4[sl], kT4[sl], ktm[sl], vtm[sl], tin[sl]
                        psm = psm_all[:, sl * 8:(sl + 1) * 8]
                        S.dma("sp", q4[:], self.PFM[hh * 256:(hh + 1) * 256, c0:c0 + 128].rearrange("(h d) t -> d h t", d=64), reads=[self.tk["PFM"]], writes=[ti])
                        S.dma("sp", k4[:], self.PFM[512 + hh * 256:512 + (hh + 1) * 256, c0:c0 + 128].rearrange("(h d) t -> d h t", d=64), reads=[self.tk["PFM"]], writes=[ti])
                        S.dma("sp", kt[:], self.PTM[c0:c0 + 128, 1024 + hh * 256:1024 + (hh + 1) * 256].rearrange("s (h d) -> s h d", d=64), reads=[self.tk["PTM"]], writes=[ti])
                        S.dma("sp", vt[:], self.PTM[c0:c0 + 128, hh * 512:(hh + 1) * 512].rearrange("s (h d) -> s h d", d=128), reads=[self.tk["PTM"]], writes=[ti])
                        lf4 = LF[:, blk, d, hs]
                        ig4 = Gv[:, blk, d, 0, hs]
                        S.op("pe", lambda e: e.matmul(psm[:, 0:4], M[:], lf4, start=True, stop=True), reads=[tLF, self.tC], writes=[tpsm[sl]])
                        S.op("dve", lambda e: e.tensor_tensor(R[sl][:], lf4.unsqueeze(2).broadcast_to([128, 4, 128]), M[:, :].unsqueeze(1).broadcast_to([128, 4, 128]), op=ALU.mult),
                             reads=[tLF, self.tC], writes=[tR[sl]])
                        S.op("pe", lambda e: e.matmul(pA[sl][:], C["ones_f"][:], R[sl][:], start=True, stop=True), reads=[tR[sl], self.tC], writes=[tpA[sl]])
                        S.op("dve", lambda e: e.tensor_tensor(a4[sl][:], ig4, psm[:, 0:4], op=ALU.subtract), reads=[tG, tpsm[sl]], writes=[ta4[sl]])
                        S.op("dve", lambda e: e.tensor_copy(brs[sl][:], pA[sl][:]), reads=[tpA[sl]], writes=[tbrs[sl]])
                        S.op("dve", lambda e: e.tensor_tensor(E1[sl][:], brs[sl][:], a4[sl][:, :].unsqueeze(2).broadcast_to([128, 4, 128]), op=ALU.add), reads=[tbrs[sl], ta4[sl]], writes=[tE1[sl]])
                        S.op("dve", lambda e: e.tensor_tensor(E1[sl][:], E1[sl][:], msk[:, :].unsqueeze(1).broadcast_to([128, 4, 128]), op=ALU.add), reads=[tE1[sl], self.tC], writes=[tE1[sl]])
                        S.op("act", lambda e: e.activation(Dm[sl][:], E1[sl][:], AF.Exp), reads=[tE1[sl]], writes=[tDm[sl]])
                        S.op("act", lambda e: e.activation(eb[sl][:], brs[sl][:], AF.Exp), reads=[tbrs[sl]], writes=[teb[sl]])
                        for h in range(4):
                            S.op("pe", lambda e, h=h: e.matmul(pA[sl][:, h, :], k4[:, h, :], q4[:, h, :], start=True, stop=True), reads=[ti], writes=[tpA[sl]], accum=(h > 0))
                        S.op("dve", lambda e: e.tensor_tensor(AT4[sl][:], pA[sl][:], Dm[sl][:], op=ALU.mult), reads=[tpA[sl], tDm[sl]], writes=[tAT4[sl]])
                        S.op("dve", lambda e: e.tensor_tensor(qTs[sl][:], q4[:], eb[sl][0:64], op=ALU.mult), reads=[ti, teb[sl]], writes=[tqTs[sl]])
                        for h in range(4):
                            S.op("pe", lambda e, h=h: e.matmul(pB[sl][:, h, :], vt[:, h, :], AT4[sl][:, h, :], start=True, stop=False), reads=[ti, tAT4[sl]], writes=[tpB[sl]], accum=(h > 0))
                            S.op("pe", lambda e, h=h: e.matmul(pB[sl][:, h, :], Cbf[sl][:, h, :], qTs[sl][:, h, :], start=False, stop=True), reads=[tCb[sl], tqTs[sl]], writes=[tpB[sl]], accum=True)
                        S.op("pe", lambda e: e.matmul(pC[sl][:], C["ones_b"][:], AT4[sl][:], start=True, stop=False), reads=[self.tC, tAT4[sl]], writes=[tpC[sl]])
                        for h in range(4):
                            S.op("pe", lambda e, h=h: e.matmul(pC[sl][:, h, :], nrep[sl][:, h, :], qTs[sl][:, h, :], start=False, stop=True, skip_group_check=True),
                                 reads=[tCb[sl], tqTs[sl]], writes=[tpC[sl]], accum=True)
                        S.op("act", lambda e: e.activation(ad[sl][:], pC[sl][:], AF.Abs), reads=[tpC[sl]], writes=[tad[sl]])
                        S.op("dve", lambda e: e.tensor_scalar(ad[sl][:], ad[sl][:], 1.0, None, op0=ALU.max), reads=[tad[sl]], writes=[tad[sl]])
                        S.op("dve", lambda e: e.reciprocal(ad[sl][:], ad[sl][:]), reads=[tad[sl]], writes=[tad[sl]])
                        S.op("dve", lambda e: e.tensor_tensor(ho[sl][:], pB[sl][:], ad[sl][:], op=ALU.mult), reads=[tpB[sl], tad[sl]], writes=[tho[sl]])
                        S.dma("sp", self.HD[d][hh * 512:(hh + 1) * 512, c0:c0 + 128].rearrange("(h p) t -> p h t", p=128), ho[sl][:], reads=[tho[sl]], writes=[self.tk[f"HD{d}"]])
                        self.conv_next(tho[sl])
                        S.op("dve", lambda e: e.tensor_tensor(lw[sl][:], a4[sl][:], brs[sl][:, :, tl], op=ALU.add), reads=[ta4[sl], tbrs[sl]], writes=[tlw[sl]])
                        S.op("act", lambda e: e.activation(lw[sl][:], lw[sl][:], AF.Exp), reads=[tlw[sl]], writes=[tlw[sl]])
                        S.op("dve", lambda e: e.tensor_tensor(wk[sl][:], kt[:], lw[sl][:, :].unsqueeze(2).broadcast_to([128, 4, 64]), op=ALU.mult), reads=[ti, tlw[sl]], writes=[twk[sl]])
                        for h in range(4):
                            S.op("pe", lambda e, h=h: e.matmul(pC[sl][0:64, h, :], wk[sl][:, h, :], vt[:, h, :], start=True, stop=True), reads=[twk[sl], ti], writes=[tpC[sl]], accum=(h > 0))
                            S.op("pe", lambda e, h=h: e.matmul(psm[0:64, 4 + h:5 + h], wk[sl][:, h, :], C["ones_b"][:, 0:1], start=True, stop=True), reads=[twk[sl], self.tC], writes=[tpsm[sl]], accum=(h > 0))
                        S.op("dve", lambda e: e.tensor_tensor(Cst[sl][:], Cst[sl][:], eb[sl][0:64, :, tl:tl + 1].broadcast_to([64, 4, 129]), op=ALU.mult), reads=[tCst[sl], teb[sl]], writes=[tCst[sl]])
                        S.op("dve", lambda e: e.tensor_tensor(Cst[sl][:, :, 0:128], Cst[sl][:, :, 0:128], pC[sl][0:64], op=ALU.add), reads=[tCst[sl], tpC[sl]], writes=[tCst[sl]])
                        S.op("dve", lambda e: e.tensor_tensor(Cst[sl][:, :, 128], Cst[sl][:, :, 128], psm[0:64, 4:8], op=ALU.add), reads=[tCst[sl], tpsm[sl]], writes=[tCst[sl]])
                        S.op("act", lambda e: e.copy(Cbf[sl][:], Cst[sl][:, :, 0:128]), reads=[tCst[sl]], writes=[tCb[sl]])
                        S.op("dve", lambda e: e.tensor_copy(nrep[sl][:], Cst[sl][:, :, 128:129].broadcast_to([64, 4, 128])), reads=[tCst[sl]], writes=[tCb[sl]])
                    bodies.append(body)
            S.threads(bodies)

    def ph_mlout(self, l, need_ctx, last):
        S, I, C = self.S, self.I, self.C
        ng = self.sb("mng", [128, 8]); tng = S.tok()
        S.dma("sp", ng[:], I[f"mlng{l}"], writes=[tng])
        self.conv_next(tng, k=1000)
        mk = lambda nm, dt=F32: [self.sb(nm, [128, 512], dt) for _ in range(4)]
        tk = lambda: [S.tok() for _ in range(4)]
        H0, H1, MO, hs_, sq, rs, sg, ob = mk("H0"), mk("H1"), mk("MO", BF16), mk("hs"), mk("sq"), mk("rs"), mk("sg"), mk("ob", BF16)
        tH, ths, tsq, trs, tsg, tob = tk(), tk(), tk(), tk(), tk(), tk()
        pp = [self.ps("pp", [128, 512]) for _ in range(4)]; tpp = tk()
        blocks = ([(0, 256)] if need_ctx else []) + [(256 + i * 512, 512) for i in range(8)]
        bodies = []
        for (t0, n) in blocks:
            for h in range(8):
                def body(i, t0=t0, n=n, h=h):
                    S.dma("sp", H0[i][:, 0:n], self.HD[0][h * 128:(h + 1) * 128, t0:t0 + n], reads=[self.tk["HD0"]], writes=[tH[i]])
                    S.dma("sp", H1[i][:, 0:n], self.HD[1][h * 128:(h + 1) * 128, t0:t0 + n], reads=[self.tk["HD1"]], writes=[tH[i]])
                    S.dma("sp", MO[i][:, 0:n], self.PFM[1024 + h * 128:1024 + (h + 1) * 128, t0:t0 + n], reads=[self.tk["PFM"]], writes=[tH[i]])
                    S.op("dve", lambda e: e.tensor_tensor(hs_[i][:, 0:n], H0[i][:, 0:n], H1[i][:, 0:n], op=ALU.add), reads=[tH[i]], writes=[ths[i]])
                    S.op("act", lambda e: e.activation(sq[i][:, 0:n], hs_[i][:, 0:n], AF.Square), reads=[ths[i]], writes=[tsq[i]])
                    S.op("pe", lambda e: e.matmul(pp[i][:, 0:n], C["ones_f"][:], sq[i][:, 0:n], start=True, stop=True), reads=[tsq[i], self.tC], writes=[tpp[i]])
                    S.op("dve", lambda e: e.tensor_scalar(rs[i][:, 0:n], pp[i][:, 0:n], 1.0 / 128, EPS, op0=ALU.mult, op1=ALU.add), reads=[tpp[i]], writes=[trs[i]])
                    S.op("act", lambda e: e.activation(rs[i][:, 0:n], rs[i][:, 0:n], AF.Sqrt), reads=[trs[i]], writes=[trs[i]])
                    S.op("dve", lambda e: e.reciprocal(rs[i][:, 0:n], rs[i][:, 0:n]), reads=[trs[i]], writes=[trs[i]])
                    S.op("act", lambda e: e.activation(sg[i][:, 0:n], MO[i][:, 0:n], AF.Sigmoid), reads=[tH[i]], writes=[tsg[i]])
                    S.op("dve", lambda e: e.scalar_tensor_tensor(hs_[i][:, 0:n], hs_[i][:, 0:n], ng[:, h:h + 1], rs[i][:, 0:n], op0=ALU.mult, op1=ALU.mult), reads=[ths[i], trs[i], tng], writes=[ths[i]])
                    S.op("dve", lambda e: e.tensor_tensor(ob[i][:, 0:n], hs_[i][:, 0:n], sg[i][:, 0:n], op=ALU.mult), reads=[ths[i], tsg[i]], writes=[tob[i]])
                    S.dma("sp", self.MT[h * 128:(h + 1) * 128, t0:t0 + n], ob[i][:, 0:n], reads=[tob[i]], writes=[self.tk["MT"]])
                bodies.append(body)
        S.threads(bodies, ways=4)

    def ph_merge_a(self, l, need_ctx, last):
        S, I = self.S, self.I
        Wb = self.sb("Wb", [128, 3, 8, 2048], BF16); tWb = S.tok()
        for br, nm in enumerate(("wba", "wbf", "wbm")):
            for hlf in range(2):
                S.dma("pool", Wb[:, br, hlf * 4:(hlf + 1) * 4, :], I[f"{nm}{l}"].rearrange("(k p) n -> p k n", p=128)[:, hlf * 4:(hlf + 1) * 4, :], writes=[tWb])
        bg = self.sb("bg", [128, 3, 16]); tbg = S.tok()
        S.dma("sp", bg[:], I[f"bgate{l}"], writes=[tbg])
        BR = [self.sb("BR", [128, 3, 8, 512], BF16) for _ in range(1)]; tBR = [S.tok() for _ in range(1)]
        GP = [self.sb("GP", [128, 3, 512], BF16) for _ in range(2)]; tGP = [S.tok() for _ in range(2)]
        gt = [self.sb("gt", [128, 3, 512]) for _ in range(2)]; tgt = [S.tok() for _ in range(2)]
        acc = [self.sb("acc", [128, 512]) for _ in range(2)]; tacc = [S.tok() for _ in range(2)]
        t2 = [self.sb("t2", [128, 512]) for _ in range(2)]; tt2 = [S.tok() for _ in range(2)]
        yo = [self.sb("yo", [128, 512], BF16) for _ in range(2)]; tyo = [S.tok() for _ in range(2)]
        PS = [[self.ps("pb", [128, 512]) for _ in range(3)] for _ in range(2)]; tPS = [[S.tok() for _ in range(3)] for _ in range(2)]
        blocks = ([(0, 256)] if need_ctx else []) + [(256 + i * 512, 512) for i in range(8)]
        for bi, (t0, n) in enumerate(blocks):
            br_, tbr_ = BR[0], tBR[0]
            for b3, (src, tkn) in enumerate(((self.AT, "AT"), (self.FT, "FT"), (self.MT, "MT"))):
                S.dma("sp", br_[:, b3, :, 0:n], src[:, t0:t0 + n].rearrange("(k p) t -> p k t", p=128), reads=[self.tk[tkn]], writes=[tbr_])
            bodies = []
            for nch in range(16):
                def body(i, nch=nch, t0=t0, n=n):
                    S.dma("sp", GP[i][:, :, 0:n], self.PFM[3072:9216, t0:t0 + n].rearrange("(b c p) t -> p b c t", b=3, p=128)[:, :, nch, :],
                          reads=[self.tk["PFM"]], writes=[tGP[i]])
                    for b3 in range(3):
                        for kc in range(8):
                            S.op("pe", lambda e, b3=b3, kc=kc: e.matmul(PS[i][b3][:, 0:n], Wb[:, b3, kc, nch * 128:(nch + 1) * 128], br_[:, b3, kc, 0:n], start=(kc == 0), stop=(kc == 7)),
                                 reads=[tWb, tbr_], writes=[tPS[i][b3]], accum=True)
                        S.op("act", lambda e, b3=b3: e.activation(gt[i][:, b3, 0:n], GP[i][:, b3, 0:n], AF.Sigmoid, bias=bg[:, b3, nch:nch + 1], scale=1.0),
                             reads=[tGP[i], tbg], writes=[tgt[i]])
                    S.op("dve", lambda e: e.tensor_tensor(acc[i][:, 0:n], PS[i][0][:, 0:n], gt[i][:, 0, 0:n], op=ALU.mult), reads=[tPS[i][0], tgt[i]], writes=[tacc[i]])
                    S.op("dve", lambda e: e.tensor_tensor(t2[i][:, 0:n], PS[i][1][:, 0:n], gt[i][:, 1, 0:n], op=ALU.mult), reads=[tPS[i][1], tgt[i]], writes=[tt2[i]])
                    S.op("dve", lambda e: e.tensor_tensor(acc[i][:, 0:n], acc[i][:, 0:n], t2[i][:, 0:n], op=ALU.add), reads=[tacc[i], tt2[i]], writes=[tacc[i]])
                    S.op("dve", lambda e: e.tensor_tensor(t2[i][:, 0:n], PS[i][2][:, 0:n], gt[i][:, 2, 0:n], op=ALU.mult), reads=[tPS[i][2], tgt[i]], writes=[tt2[i]])
                    S.op("dve", lambda e: e.tensor_tensor(yo[i][:, 0:n], acc[i][:, 0:n], t2[i][:, 0:n], op=ALU.add), reads=[tacc[i], tt2[i]], writes=[tyo[i]])
                    S.dma("sp", self.YT[nch * 128:(nch + 1) * 128, t0:t0 + n], yo[i][:, 0:n], reads=[tyo[i]], writes=[self.tk["YT"]])
                bodies.append(body)
            S.threads(bodies)

    def ph_merge_b(self, l, need_ctx, last):
        S, I = self.S, self.I
        Wo = self.sb("Wo", [128, 16, 2048], BF16); tWo = S.tok()
        for q4 in range(4):
            S.dma("pool", Wo[:, q4 * 4:(q4 + 1) * 4, :], I[f"wout{l}"].rearrange("(k p) n -> p k n", p=128)[:, q4 * 4:(q4 + 1) * 4, :], writes=[tWo])
        Y = [self.sb("Y", [128, 16, 512], BF16) for _ in range(1)]; X = [self.sb("X", [128, 16, 512]) for _ in range(1)]; tY = [S.tok() for _ in range(1)]
        xo = [self.sb("xo", [128, 512]) for _ in range(3)]; txo = [S.tok() for _ in range(3)]
        PS = [self.ps("po", [128, 512]) for _ in range(4)]; tPS = [S.tok() for _ in range(4)]
        blocks = ([(0, 256, 1)] if need_ctx else []) + [(256 + i * 512, 512, 0) for i in range(8)]
        k = 0
        for bi, (t0, n, j) in enumerate(blocks):
            y, x, ty = Y[0], X[0], tY[0]
            S.dma("sp", y[:, :, 0:n], self.YT[:, t0:t0 + n].rearrange("(k p) t -> p k t", p=128), reads=[self.tk["YT"]], writes=[ty])
            S.dma("sp", x[:, :, 0:n], self.XT[:, t0:t0 + n].rearrange("(k p) t -> p k t", p=128), reads=[self.tk["XT"]], writes=[ty])
            for nch in range(16):
                pi = k % 4; oi = k % 3; k += 1
                for kc in range(16):
                    S.op("pe", lambda e, kc=kc: e.matmul(PS[pi][:, 0:n], Wo[:, kc, nch * 128:(nch + 1) * 128], y[:, kc, 0:n], start=(kc == 0), stop=(kc == 15)),
                         reads=[tWo, ty], writes=[tPS[pi]], accum=True)
                S.op("dve", lambda e: e.scalar_tensor_tensor(xo[oi][:, 0:n], PS[pi][:, 0:n], self.modT[:, 32 + nch, j:j + 1], x[:, nch, 0:n], op0=ALU.mult, op1=ALU.add),
                     reads=[tPS[pi], ty, self.tmod], writes=[txo[oi]])
                S.dma("sp", self.X1T[nch * 128:(nch + 1) * 128, t0:t0 + n], xo[oi][:, 0:n], reads=[txo[oi]], writes=[self.tk["X1T"]])

    def ph_route(self, l, need_ctx, last):
        S, I, C = self.S, self.I, self.C
        WR = self.sb("WR", [128, 16, 36]); tWR = S.tok()
        S.dma("sp", WR[:], I[f"wr{l}"], writes=[tWR])
        rb = self.sb("rb", [128, 36]); trb = S.tok()
        S.dma("sp", rb[:], I[f"rbias{l}"], writes=[trb])
        io = self.sb("io128", [128, 128]); hp = self.sb("hp", [128, 2]); tio = S.tok()
        S.dma("sp", io[:], I["iota128"], writes=[tio]); S.dma("sp", hp[:], I["hpc"], writes=[tio])
        X = self.sb("X", [128, 16, 512]); SQ = self.sb("SQ", [128, 16, 512]); rs = self.sb("rs", [128, 512]); tmp = self.sb("tmp", [128, 512])
        pss = self.ps("pss", [128, 512])
        ntoks = [S.tok() for _ in range(5)]
        H32 = self.sb("H32", [128, 16, 512]); tH32 = S.tok()
        Hb = self.sb("Hb", [128, 16, 512], BF16); tHb = S.tok()
        Htm = [self.sb("Htm", [128, 2048], BF16) for _ in range(2)]; tHtm = [S.tok() for _ in range(2)]
        pT = [self.ps("pT", [128, 8, 128], BF16) for _ in range(4)]; tpT = [S.tok() for _ in range(4)]
        plc = [self.ps("plc", [128, 128]) for _ in range(2)]; tpl = [S.tok() for _ in range(2)]; tpc = [S.tok() for _ in range(2)]
        two = lambda nm, shp, dt=F32: [self.sb(nm, shp, dt) for _ in range(2)]
        tk2 = lambda: [S.tok() for _ in range(2)]
        lg = two("lg", [128, 36]); tlg = tk2()
        sm = two("sm", [128, 16]); tsm = tk2()
        ohg = two("ohg", [128, 4]); tohg = tk2()
        el = two("el", [128, 32]); tel = tk2()
        ohs = two("ohs", [128, 32], BF16); tohs = tk2()
        t32 = two("t32", [128, 32]); tt32 = tk2()
        OH = self.sb("OH", [128, NB, 2, 32]); tOH = S.tok()
        CNT = self.sb("CNT", [128, NB, 32]); tCNT = S.tok()
        COLS = self.sb("COLS", [128, NB, 32]); tCOLS = S.tok()
        base = self.sb("base", [128, 32]); tbase = S.tok()
        df = self.sb("df", [128, 2]); tdf = S.tok()
        IDX = [self.sb("idx", [128, 1], I32) for _ in range(4)]; tIDX = [S.tok() for _ in range(4)]
        S.op("pool", lambda e: e.memset(base[:], 0.0), writes=[tbase])
        zt = self.sb("zt", [128, 2048], BF16); tzt = S.tok()
        S.op("pool", lambda e: e.memset(zt[:], 0.0), writes=[tzt])
        xsv = self.XS.rearrange("(b p) d -> b p d", p=128)
        for zb in range(NBLK):
            S.dma("sp", xsv[zb], zt[:], reads=[tzt], writes=[self.tk["XS"]])
        blocks = ([(0, 256, 1)] if need_ctx else []) + [(256 + i * 512, 512, 0) for i in range(8)]
        blks = []
        for (t0, n, j) in blocks:
            def outf(kc, tap, bias, ttmp):
                S.op("act", lambda e: e.activation(H32[:, kc, 0:n], tap, AF.Identity, bias=bias, scale=1.0), reads=[ttmp, self.tmod], writes=[tH32])
            self.norm_block(self.X1T, self.tk["X1T"], t0, n, self.A2, 48, j, X, SQ, rs, tmp, pss, ntoks, outf)
            S.op("dve", lambda e: e.tensor_copy(Hb[:, :, 0:n], H32[:, :, 0:n]), reads=[tH32], writes=[tHb])
            bodies = []
            for s0 in range(0, n, 128):
                blk = (t0 + s0) // 128
                blks.append(blk)

                def body(i, s0=s0, blk=blk):
                    pl = plc[i][:, 0:36]; pc = plc[i][:, 64:128]
                    for half in range(2):
                        pt, tpt = pT[2 * i + half], tpT[2 * i + half]
                        for kk in range(8):
                            kc = half * 8 + kk
                            S.op("pe", lambda e, kc=kc, kk=kk, pt=pt: e.transpose(pt[:, kk, :], Hb[:, kc, s0:s0 + 128], C["ident_b"][:]), reads=[tHb, self.tC], writes=[tpt], accum=True)
                        if half == 0:
                            S.op("act", lambda e, pt=pt: e.copy(Htm[i][:, 0:1024], pt[:].rearrange("p a b -> p (a b)")), reads=[tpt], writes=[tHtm[i]])
                        else:
                            S.op("dve", lambda e, pt=pt: e.tensor_copy(Htm[i][:, 1024:2048], pt[:].rearrange("p a b -> p (a b)")), reads=[tpt], writes=[tHtm[i]])
                    S.dma("sp", self.H2[blk * 128:(blk + 1) * 128, :], Htm[i][:], reads=[tHtm[i]], writes=[self.tk["H2"]])
                    for kc in range(16):
                        S.op("pe", lambda e, kc=kc: e.matmul(pl, H32[:, kc, s0:s0 + 128], WR[:, kc, :], start=(kc == 0), stop=(kc == 15)), reads=[tH32, tWR], writes=[tpl[i]], accum=True)
                    S.op("dve", lambda e: e.tensor_tensor(lg[i][:], pl, rb[:], op=ALU.add), reads=[tpl[i], trb], writes=[tlg[i]])
                    S.op("dve", lambda e: e.tensor_reduce(sm[i][:, 0:1], lg[i][:, 0:4], axis=AX.X, op=ALU.max), reads=[tlg[i]], writes=[tsm[i]])
                    S.op("dve", lambda e: e.tensor_tensor(ohg[i][:], lg[i][:, 0:4], sm[i][:, 0:1].broadcast_to([128, 4]), op=ALU.is_equal), reads=[tlg[i], tsm[i]], writes=[tohg[i]])
                    S.op("dve", lambda e: e.tensor_scalar(sm[i][:, 1:2], sm[i][:, 0:1], -1.0, None, op0=ALU.mult), reads=[tsm[i]], writes=[tsm[i]])
                    S.op("act", lambda e: e.activation(t32[i][:, 0:4], lg[i][:, 0:4], AF.Exp, bias=sm[i][:, 1:2], scale=1.0), reads=[tlg[i], tsm[i]], writes=[tt32[i]])
                    S.op("dve", lambda e: e.tensor_reduce(sm[i][:, 2:3], t32[i][:, 0:4], axis=AX.X, op=ALU.add), reads=[tt32[i]], writes=[tsm[i]])
                    S.op("dve", lambda e: e.reciprocal(sm[i][:, 3:4], sm[i][:, 2:3]), reads=[tsm[i]], writes=[tsm[i]])
                    S.op("dve", lambda e: e.tensor_scalar(ohg[i][:], ohg[i][:], -1.0, 1e30, op0=ALU.add, op1=ALU.mult), reads=[tohg[i]], writes=[tohg[i]])
                    S.op("dve", lambda e: e.tensor_tensor(el[i][:].rearrange("p (g x) -> p g x", g=4), lg[i][:, 4:36].rearrange("p (g x) -> p g x", g=4),
                                                          ohg[i][:, :].unsqueeze(2).broadcast_to([128, 4, 8]), op=ALU.add), reads=[tlg[i], tohg[i]], writes=[tel[i]])
                    S.op("dve", lambda e: e.tensor_reduce(sm[i][:, 4:5], el[i][:], axis=AX.X, op=ALU.max), reads=[tel[i]], writes=[tsm[i]])
                    S.op("dve", lambda e: e.tensor_tensor(OH[:, blk, 0, :], el[i][:], sm[i][:, 4:5].broadcast_to([128, 32]), op=ALU.is_equal), reads=[tel[i], tsm[i]], writes=[tOH])
                    S.op("dve", lambda e: e.scalar_tensor_tensor(el[i][:], OH[:, blk, 0, :], -1e30, el[i][:], op0=ALU.mult, op1=ALU.add), reads=[tOH, tel[i]], writes=[tel[i]])
                    S.op("dve", lambda e: e.tensor_reduce(sm[i][:, 5:6], el[i][:], axis=AX.X, op=ALU.max), reads=[tel[i]], writes=[tsm[i]])
                    S.op("dve", lambda e: e.tensor_tensor(OH[:, blk, 1, :], el[i][:], sm[i][:, 5:6].broadcast_to([128, 32]), op=ALU.is_equal), reads=[tel[i], tsm[i]], writes=[tOH])
                    S.op("dve", lambda e: e.tensor_tensor(sm[i][:, 6:7], sm[i][:, 5:6], sm[i][:, 4:5], op=ALU.subtract), reads=[tsm[i]], writes=[tsm[i]])
                    S.op("act", lambda e: e.activation(sm[i][:, 6:7], sm[i][:, 6:7], AF.Exp), reads=[tsm[i]], writes=[tsm[i]])
                    S.op("dve", lambda e: e.tensor_scalar(sm[i][:, 6:7], sm[i][:, 6:7], 1.0, None, op0=ALU.add), reads=[tsm[i]], writes=[tsm[i]])
                    S.op("dve", lambda e: e.reciprocal(sm[i][:, 6:7], sm[i][:, 6:7]), reads=[tsm[i]], writes=[tsm[i]])
                    S.op("dve", lambda e: e.tensor_tensor(self.WT[:, blk, 0:1], sm[i][:, 6:7], sm[i][:, 3:4], op=ALU.mult), reads=[tsm[i]], writes=[self.troute])
                    S.op("dve", lambda e: e.tensor_tensor(self.WT[:, blk, 1:2], sm[i][:, 3:4], self.WT[:, blk, 0:1], op=ALU.subtract), reads=[tsm[i], self.troute], writes=[self.troute])
                    S.op("dve", lambda e: e.tensor_tensor(ohs[i][:], OH[:, blk, 0, :], OH[:, blk, 1, :], op=ALU.add), reads=[tOH], writes=[tohs[i]])
                    S.op("pe", lambda e: e.matmul(pc[:, 0:32], C["Ltri_b"][:], ohs[i][:], start=True, stop=True), reads=[tohs[i], self.tC], writes=[tpc[i]], accum=True)
                    S.op("pe", lambda e: e.matmul(pc[:, 32:64], C["ones_b"][:], ohs[i][:], start=True, stop=True), reads=[tohs[i], self.tC], writes=[tpc[i]], accum=True)
                    S.op("dve", lambda e: e.tensor_copy(CNT[:, blk, :], pc[:, 0:32]), reads=[tpc[i]], writes=[tCNT])
                    S.op("dve", lambda e: e.tensor_copy(COLS[:, blk, :], pc[:, 32:64]), reads=[tpc[i]], writes=[tCOLS])
                bodies.append(body)
            S.threads(bodies)
        for blk in blks:
            S.op("dve", lambda e, blk=blk: e.tensor_tensor(CNT[:, blk, :], CNT[:, blk, :], base[:], op=ALU.add), reads=[tCNT, tbase], writes=[tCNT])
            S.op("dve", lambda e, blk=blk: e.tensor_tensor(base[:], base[:], COLS[:, blk, :], op=ALU.add), reads=[tCOLS, tbase], writes=[tbase])
        big = self.sb("big", [128, 32, 100]); tbig = S.tok()
        thr = self.sb("thr", [128, 100]); tthr = S.tok()
        nbk = self.sb("nbk", [128, 32]); pend = self.sb("pend", [128, 32]); pst = self.sb("pst", [128, 32]); tsc = S.tok()
        S.op("dve", lambda e: e.tensor_scalar(thr[:], io[:, 0:100], float(RB), None, op0=ALU.mult), reads=[tio], writes=[tthr])
        S.op("dve", lambda e: e.tensor_tensor(big[:], base[:, :].unsqueeze(2).broadcast_to([128, 32, 100]), thr[:, :].unsqueeze(1).broadcast_to([128, 32, 100]), op=ALU.is_gt),
             reads=[tbase, tthr], writes=[tbig])
        S.op("dve", lambda e: e.tensor_reduce(nbk[:], big[:], axis=AX.X, op=ALU.add), reads=[tbig], writes=[tsc])
        S.op("dve", lambda e: e.tensor_scalar(nbk[:], nbk[:], float(RB), None, op0=ALU.mult), reads=[tsc], writes=[tsc])
        tri = big[:, :, 0:32]
        S.op("dve", lambda e: e.tensor_tensor(tri, C["iota32"][:, :].unsqueeze(1).broadcast_to([128, 32, 32]), C["iota32"][:, :].unsqueeze(2).broadcast_to([128, 32, 32]), op=ALU.is_le),
             reads=[self.tC, tbig], writes=[tbig])
        S.op("dve", lambda e: e.tensor_tensor(tri, tri, nbk[:, :].unsqueeze(1).broadcast_to([128, 32, 32]), op=ALU.mult), reads=[tbig, tsc], writes=[tbig])
        S.op("dve", lambda e: e.tensor_reduce(pend[:], tri, axis=AX.X, op=ALU.add), reads=[tbig], writes=[tsc])
        S.op("dve", lambda e: e.tensor_tensor(pst[:], pend[:], nbk[:], op=ALU.subtract), reads=[tsc], writes=[tsc])
        big2 = self.sb("big2", [128, 100, 32]); tbig2 = S.tok()
        S.op("dve", lambda e: e.tensor_tensor(big2[:], pend[:, :].unsqueeze(1).broadcast_to([128, 100, 32]), thr[:, :].unsqueeze(2).broadcast_to([128, 100, 32]), op=ALU.is_le),
             reads=[tsc, tthr], writes=[tbig2])
        S.op("dve", lambda e: e.tensor_reduce(self.BE[:], big2[:], axis=AX.X, op=ALU.add), reads=[tbig2], writes=[self.troute])
        S.op("dve", lambda e: e.tensor_scalar(self.BE[:], self.BE[:], 256.0, None, op0=ALU.mult), reads=[self.troute], writes=[self.troute])
        bif = self.sb("bif", [128, 100, 2]); tbif = S.tok()
        S.op("dve", lambda e: e.tensor_tensor(bif[:], self.BE[:, :].unsqueeze(2).broadcast_to([128, 100, 2]), hp[:, :].unsqueeze(1).broadcast_to([128, 100, 2]), op=ALU.add),
             reads=[self.troute, tio], writes=[tbif])
        S.op("dve", lambda e: e.tensor_copy(self.BIDX[:], bif[:]), reads=[tbif], writes=[self.troute])
        df2 = [self.sb("df2", [128, 2]) for _ in range(2)]; tdf2 = [S.tok() for _ in range(2)]
        bodies = []
        for blk in blks:
            def body(i, blk=blk):
                S.dma("sp", Htm[i][:], self.H2[blk * 128:(blk + 1) * 128, :], reads=[self.tk["H2"]], writes=[tHtm[i]])
                S.op("dve", lambda e: e.tensor_tensor(t32[i][:], CNT[:, blk, :], pst[:], op=ALU.add), reads=[tCNT, tsc], writes=[tt32[i]])
                for s_ in range(2):
                    S.op("dve", lambda e, s_=s_: e.tensor_tensor(el[i][:], OH[:, blk, s_, :], t32[i][:], op=ALU.mult), reads=[tOH, tt32[i]], writes=[tel[i]])
                    S.op("dve", lambda e, s_=s_: e.tensor_reduce(df2[i][:, s_:s_ + 1], el[i][:], axis=AX.X, op=ALU.add), reads=[tel[i]], writes=[tdf2[i]])
                S.op("dve", lambda e: e.tensor_copy(self.DEST[:, blk, :], df2[i][:]), reads=[tdf2[i]], writes=[self.troute])
                for s_ in range(2):
                    ix = IDX[2 * i + s_]; tix = tIDX[2 * i + s_]
                    S.op("dve", lambda e, s_=s_, ix=ix: e.tensor_copy(ix[:, :], df2[i][:, s_:s_ + 1]), reads=[tdf2[i]], writes=[tix])
                    S.indirect(self.XS[:, :], bass.IndirectOffsetOnAxis(ap=ix[:, :], axis=0), Htm[i][:, :], None, NBLK * 128 - 1,
                               reads=[tHtm[i], tix], writes=[self.tk["XS"]])
            bodies.append(body)
        S.threads(bodies)

    def ph_moe(self, l, need_ctx, last):
        S, I, C = self.S, self.I, self.C
        NS = 8
        W = [self.sb("We", [128, 16, 512], BF16) for _ in range(NS)]; tW = [S.tok() for _ in range(NS)]
        xe = [self.sb("xe", [128, 2048], BF16) for _ in range(2)]; txe = [S.tok() for _ in range(2)]
        xeT = [self.sb("xeT", [128, 16, 128], BF16) for _ in range(2)]; txeT = [S.tok() for _ in range(2)]
        actT = [self.sb("actT", [128, 8, 128], BF16) for _ in range(2)]; tact = [S.tok() for _ in range(2)]
        sl = [self.sb("sl", [128, 512]) for _ in range(2)]; tsl = [S.tok() for _ in range(2)]
        atm = [self.sb("atm", [128, 1024], BF16) for _ in range(2)]; tatm = [S.tok() for _ in range(2)]
        ye = [self.sb("ye", [128, 512]) for _ in range(3)]; tye = [S.tok() for _ in range(3)]
        pT = [self.ps("pT", [128, 8, 128], BF16) for _ in range(2)]; tpT = [S.tok() for _ in range(2)]
        p1 = [self.ps("p1", [128, 512]) for _ in range(2)]; tp1 = [S.tok() for _ in range(2)]
        p3 = [self.ps("p3", [128, 512]) for _ in range(2)]; tp3 = [S.tok() for _ in range(2)]
        p2 = [self.ps("p2", [128, 512]) for _ in range(2)]; tp2 = [S.tok() for _ in range(2)]
        nblk = (2 * (NT if need_ctx else NLAT)) // RB + NE
        SUB = RB // 128
        srcs = self.WB
        tiles = []
        for j in range(nblk):
            for h in range(2):
                tiles.append(("w1", j, h)); tiles.append(("w3", j, h))
            for h in range(2):
                tiles.append(("w2", j, h))

        def load(i):
            kind, j, h = tiles[i]
            w, tw = W[i % NS], tW[i % NS]
            S.indirect(w[:].rearrange("p a b -> p (a b)"), None, srcs[kind][:, :], bass.IndirectOffsetOnAxis(ap=self.BIDX[:, j, h:h + 1], axis=0), NE * 2 * 128 - 1,
                       reads=[self.troute, self.tkWB[kind]], writes=[tw])
        PF = 6
        for i in range(PF):
            load(i)
        ti = 0
        kk = 0
        units = [(j, r) for j in range(nblk) for r in range(SUB)]

        def xload(u):
            j, r = units[u]
            row = (j * SUB + r) * 128
            S.dma("sp", xe[u % 2][:], self.XS[row:row + 128, :], reads=[self.tk["XS"]], writes=[txe[u % 2]])
        xload(0)
        for j in range(nblk):
            for r in range(SUB):
                u = j * SUB + r
                x, tx = xe[u % 2], txe[u % 2]
                if u + 1 < len(units):
                    xload(u + 1)
                xt, txt = xeT[r], txeT[r]
                for half in range(2):
                    pi = kk % 2; kk += 1
                    for j8 in range(8):
                        kc = half * 8 + j8
                        S.op("pe", lambda e, kc=kc, j8=j8: e.transpose(pT[pi][:, j8, :], x[:, kc * 128:(kc + 1) * 128], C["ident_b"][:]), reads=[tx, self.tC], writes=[tpT[pi]], accum=True)
                    if half == 0:
                        S.op("act", lambda e: e.copy(xt[:, 0:8, :], pT[pi][:]), reads=[tpT[pi]], writes=[txt])
                    else:
                        S.op("dve", lambda e: e.tensor_copy(xt[:, 8:16, :], pT[pi][:]), reads=[tpT[pi]], writes=[txt])
            for fh in range(2):
                w1, tw1 = W[ti % NS], tW[ti % NS]
                w3, tw3 = W[(ti + 1) % NS], tW[(ti + 1) % NS]
                for q in range(2):
                    if ti + PF + q < len(tiles):
                        load(ti + PF + q)
                ti += 2
                for r in range(SUB):
                    xt, txt = xeT[r], txeT[r]
                    for kc in range(16):
                        S.op("pe", lambda e, kc=kc: e.matmul(p1[r][:], xt[:, kc, :], w1[:, kc, :], start=(kc == 0), stop=(kc == 15)),
                             reads=[tw1, txt], writes=[tp1[r]], accum=True)
                    for kc in range(16):
                        S.op("pe", lambda e, kc=kc: e.matmul(p3[r][:], xt[:, kc, :], w3[:, kc, :], start=(kc == 0), stop=(kc == 15)),
                             reads=[tw3, txt], writes=[tp3[r]], accum=True)
                    S.op("act", lambda e: e.activation(sl[r][:], p1[r][:], AF.Silu), reads=[tp1[r]], writes=[tsl[r]])
                    S.op("dve", lambda e: e.tensor_tensor(atm[r][:, fh * 512:(fh + 1) * 512], sl[r][:], p3[r][:], op=ALU.mult), reads=[tsl[r], tp3[r]], writes=[tatm[r]])
            for r in range(SUB):
                pi = kk % 2; kk += 1
                for fc in range(8):
                    S.op("pe", lambda e, fc=fc: e.transpose(pT[pi][:, fc, :], atm[r][:, fc * 128:(fc + 1) * 128], C["ident_b"][:]), reads=[tatm[r], self.tC], writes=[tpT[pi]], accum=True)
                if r % 2:
                    S.op("act", lambda e: e.copy(actT[r][:], pT[pi][:]), reads=[tpT[pi]], writes=[tact[r]])
                else:
                    S.op("dve", lambda e: e.tensor_copy(actT[r][:], pT[pi][:]), reads=[tpT[pi]], writes=[tact[r]])
            for ch in range(2):
                w2, tw2 = W[ti % NS], tW[ti % NS]
                if ti + PF < len(tiles):
                    load(ti + PF)
                ti += 1
                w2v = w2[:].rearrange("p (a b) c -> p a (b c)", a=8)
                for r in range(SUB):
                    for cb in range(2):
                        pi = kk % 2; yi = kk % 3; kk += 1
                        for fc in range(8):
                            S.op("pe", lambda e, fc=fc: e.matmul(p2[pi][:], actT[r][:, fc, :], w2v[:, fc, cb * 512:(cb + 1) * 512], start=(fc == 0), stop=(fc == 7)),
                                 reads=[tact[r], tw2], writes=[tp2[pi]], accum=True)
                        if kk % 2:
                            S.op("act", lambda e: e.copy(ye[yi][:], p2[pi][:]), reads=[tp2[pi]], writes=[tye[yi]])
                        else:
                            S.op("dve", lambda e: e.tensor_copy(ye[yi][:], p2[pi][:]), reads=[tp2[pi]], writes=[tye[yi]])
                        c0 = ch * 1024 + cb * 512
                        row = (j * SUB + r) * 128
                        S.dma("sp", self.YS[row:row + 128, c0:c0 + 512], ye[yi][:], reads=[tye[yi]], writes=[self.tk["YS"]])

    def ph_combine(self, l, need_ctx, last):
        S, I, C = self.S, self.I, self.C
        tk = lambda k=2: [S.tok() for _ in range(k)]
        Y1 = [self.sb("Y1", [128, 2048]) for _ in range(2)]; Y2 = [self.sb("Y2", [128, 2048]) for _ in range(2)]; tY = tk()
        ym = [self.sb("ym", [128, 2048]) for _ in range(2)]; tym = tk()
        x1 = [self.sb("x1", [128, 16, 128]) for _ in range(2)]; tx1 = tk()
        xo = [self.sb("xo", [128, 16, 128]) for _ in range(2)]; txo = tk()
        pT = [self.ps("pT", [128, 4, 128]) for _ in range(4)]; tpT = tk(4)
        IDX = [self.sb("idx", [128, 1], I32) for _ in range(4)]; tIDX = tk(4)
        blocks = ([0, 1] if need_ctx else []) + list(range(2, NB))
        bodies = []
        for blk in blocks:
            def body(i, blk=blk):
                j = 1 if blk < 2 else 0
                ix1, ix2, t1_, t2_ = IDX[2 * i], IDX[2 * i + 1], tIDX[2 * i], tIDX[2 * i + 1]
                S.op("dve", lambda e: e.tensor_copy(ix1[:, :], self.DEST[:, blk, 0:1]), reads=[self.troute], writes=[t1_])
                S.op("dve", lambda e: e.tensor_copy(ix2[:, :], self.DEST[:, blk, 1:2]), reads=[self.troute], writes=[t2_])
                S.indirect(Y1[i][:, :], None, self.YS[:, :], bass.IndirectOffsetOnAxis(ap=ix1[:, :], axis=0), NBLK * 128 - 1,
                           reads=[self.tk["YS"], t1_], writes=[tY[i]])
                S.indirect(Y2[i][:, :], None, self.YS[:, :], bass.IndirectOffsetOnAxis(ap=ix2[:, :], axis=0), NBLK * 128 - 1,
                           reads=[self.tk["YS"], t2_], writes=[tY[i]])
                S.dma("sp", x1[i][:], self.X1T[:, blk * 128:(blk + 1) * 128].rearrange("(k p) t -> p k t", p=128), reads=[self.tk["X1T"]], writes=[tx1[i]])
                S.op("dve", lambda e: e.tensor_scalar(ym[i][:], Y1[i][:], self.WT[:, blk, 0:1], None, op0=ALU.mult), reads=[tY[i], self.troute], writes=[tym[i]])
                S.op("dve", lambda e: e.scalar_tensor_tensor(ym[i][:], Y2[i][:], self.WT[:, blk, 1:2], ym[i][:], op0=ALU.mult, op1=ALU.add), reads=[tY[i], self.troute, tym[i]], writes=[tym[i]])
                for q in range(4):
                    pi = 2 * i + (q % 2)
                    for c in range(4):
                        kc = q * 4 + c
                        S.op("pe", lambda e, kc=kc, c=c, pi=pi: e.transpose(pT[pi][:, c, :], ym[i][:, kc * 128:(kc + 1) * 128], C["ident_f"][:]), reads=[tym[i], self.tC], writes=[tpT[pi]], accum=True)
                    for c in range(4):
                        kc = q * 4 + c
                        S.op("dve", lambda e, kc=kc, c=c, pi=pi: e.scalar_tensor_tensor(xo[i][:, kc, :], pT[pi][:, c, :], self.modT[:, 80 + kc, j:j + 1], x1[i][:, kc, :], op0=ALU.mult, op1=ALU.add),
                             reads=[tpT[pi], tx1[i], self.tmod], writes=[txo[i]])
                if last:
                    S.dma("sp", self.out[:, (blk - 2) * 128:(blk - 1) * 128].rearrange("(k p) t -> p k t", p=128), xo[i][:], reads=[txo[i]], writes=[self.tk["OUT"]])
                else:
                    S.dma("sp", self.XT[:, blk * 128:(blk + 1) * 128].rearrange("(k p) t -> p k t", p=128), xo[i][:], reads=[txo[i]], writes=[self.tk["XT"]])
            bodies.append(body)
        S.threads(bodies)


_CONSTS = None


def _prep_shared(inp, n_layers):
    global _CONSTS
    if _CONSTS is None:
        _CONSTS = _host_consts()
    sh = dict(_CONSTS)
    sp = np.cumsum([0, 1024, 256, 256, 512, 512, 1024, 1024, 32, 1024, 6144])
    q, k, v, mq, mk, mv, mo, mg, fu, gp = [slice(sp[i], sp[i + 1]) for i in range(10)]
    for l in range(n_layers):
        w_in = np.asarray(inp["w_in"][l], np.float32)
        sh[f"w_fm{l}"] = np.ascontiguousarray(np.concatenate([w_in[:, s] for s in (q, k, mq, mk, mo, fu, gp)], axis=1))
        sh[f"w_tm{l}"] = np.ascontiguousarray(np.concatenate([w_in[:, s] for s in (mv, mk, v, mg)] + [np.zeros((D, 224), np.float32)], axis=1))
        sh[f"w_mod{l}"] = np.ascontiguousarray(inp["w_mod"][l], dtype=np.float32)
        sh[f"bmod{l}"] = _fm(inp["b_mod"][l])
        sh[f"ng{l}"] = np.ascontiguousarray(np.stack([_fm(inp["norm1_g"][l]), _fm(inp["norm2_g"][l])], axis=1))
        sh[f"qkg{l}"] = np.ascontiguousarray(np.stack([np.tile(inp["q_norm_g"][l], 2), np.tile(inp["k_norm_g"][l], 2)], axis=1).astype(np.float32))
        sh[f"sink{l}"] = np.ascontiguousarray(np.tile(np.asarray(inp["attn_sink"][l], np.float32)[None, :], (64, 1)))
        sh[f"mlgb{l}"] = np.ascontiguousarray(np.tile(np.asarray(inp["ml_gate_b"][l], np.float32).reshape(1, 32), (128, 1)))
        sh[f"mlng{l}"] = _fm(inp["ml_norm_g"][l])
        sh[f"wba{l}"] = np.ascontiguousarray(inp["w_br_attn"][l], dtype=np.float32)
        sh[f"wbf{l}"] = np.ascontiguousarray(inp["w_br_four"][l], dtype=np.float32)
        sh[f"wbm{l}"] = np.ascontiguousarray(inp["w_br_mlstm"][l], dtype=np.float32)
        sh[f"bgate{l}"] = np.ascontiguousarray(np.stack([_fm(inp["b_gate"][l][b]) for b in range(3)], axis=1))
        sh[f"wout{l}"] = np.ascontiguousarray(inp["w_out"][l], dtype=np.float32)
        wr = np.concatenate([np.asarray(inp["w_grp"][l], np.float32), np.asarray(inp["w_exp_router"][l], np.float32).reshape(D, 32)], axis=1)
        sh[f"wr{l}"] = np.ascontiguousarray(wr.reshape(16, 128, 36).transpose(1, 0, 2))
        rbias = np.concatenate([np.asarray(inp["b_grp"][l], np.float32), np.asarray(inp["b_exp_router"][l], np.float32).reshape(32)])
        sh[f"rbias{l}"] = np.ascontiguousarray(np.tile(rbias[None, :], (128, 1)))
        for nm in ("w1", "w3"):
            w = np.asarray(inp[nm][l], np.float32).reshape(NE, 16, 128, 2, 512)
            sh[f"{nm}r_{l}"] = np.ascontiguousarray(w.transpose(0, 3, 2, 1, 4)).reshape(NE * 2 * 128, 16 * 512)
        w = np.asarray(inp["w2"][l], np.float32).reshape(NE, 8, 128, 2, 1024)
        sh[f"w2r_{l}"] = np.ascontiguousarray(w.transpose(0, 3, 2, 1, 4)).reshape(NE * 2 * 128, 8 * 1024)
    return sh


def _prep_core(inp, b):
    x = np.asarray(inp["x"][b], np.float32)
    ctx = np.asarray(inp["ctx"][b], np.float32)
    xT0 = np.ascontiguousarray(np.concatenate([ctx, x], axis=0).T)
    cT = np.ascontiguousarray(np.stack([_fm(inp["c"][b]), _fm(inp["c_ctx"])], axis=2))
    return {"xT0": xT0, "cT": cT}


def run(inp, batches, n_layers=2, stop_after=None, debug=(), only=None, trace=False):
    sh = _prep_shared(inp, n_layers)
    if only == "NOBIG":
        sh = {k: v for k, v in sh.items() if not k.startswith(("w1r_", "w3r_", "w2r_", "wba", "wbf", "wbm", "wout"))}
    elif only is not None:
        sh = {k: v for k, v in sh.items() if k in only}
    in_maps = []
    for b in batches:
        m = dict(sh)
        m.update(_prep_core(inp, b))
        in_maps.append(m)
    shapes = {k: (v.shape, v.dtype) for k, v in in_maps[0].items()}
    nc = bass.Bass("TRN2", target_bir_lowering=False)
    Prog(nc, shapes, n_layers=n_layers, stop_after=stop_after, debug=debug).build()
    res = run_bass_kernel_spmd(nc, in_maps, core_ids=list(range(len(batches))), **({"trace": True} if trace else {}))
    return res


def kernel(**inputs):
    res = run(inputs, [0, 1, 2, 3])
    out = np.stack([np.ascontiguousarray(r["out"].T) for r in res.results], axis=0)
    return out.astype(np.float32)
```

```python
import contextlib
import numpy as np
import ml_dtypes
import concourse.bass as bass
import concourse.mybir as mybir
from concourse.bass_utils import run_bass_kernel_spmd

F32 = mybir.dt.float32
BF16 = mybir.dt.bfloat16
I32 = mybir.dt.int32
AF = mybir.ActivationFunctionType
ALU = mybir.AluOpType
AX = mybir.AxisListType
NPBF = ml_dtypes.bfloat16

D = 2048
KC = 16
NCTX = 256
NLAT = 4096
NT = NCTX + NLAT
NB = NT // 128
NE = 32
CAP = 384
NBLK = 100
EPS = 1e-6
NFM = 10496
NTM = 1824
BIGNEG = -30000.0


class Tok:
    __slots__ = ("w", "r", "dsem", "persist", "old")

    def __init__(self, persist=False):
        self.w = None
        self.r = []
        self.dsem = None
        self.persist = persist


class DSem:
    __slots__ = ("h", "total", "bg")

    def __init__(self, h):
        self.h = h
        self.total = 0
        self.bg = False


class Sched:
    ENG = ("pe", "act", "dve", "pool", "sp")
    EPOCH = 30000
    DMAX = 60000

    def __init__(self, nc, stack):
        self.nc = nc
        self.stack = stack
        self.e = {"pe": nc.tensor, "act": nc.scalar, "dve": nc.vector, "pool": nc.gpsimd, "sp": nc.sync}
        self.csem, self.ccnt, self.prev = {}, {}, {}
        self.nsem = 0
        for k in self.ENG:
            self.prev[k] = None
            self._new_epoch(k)
        self.seen = {k: {} for k in self.ENG}
        self.free_dsems = []
        self.retired = []
        self.all_dsems = []
        self.phase_toks = []
        self.n_inst = 0
        self.npe = 0

    def _new_epoch(self, k):
        if k in self.csem:
            self.prev[k] = (self.csem[k], self.ccnt[k])
        self.nsem += 1
        self.csem[k] = self.stack.enter_context(self.nc.semaphore(f"cs{self.nsem}"))
        self.ccnt[k] = 0

    def tok(self, persist=False):
        t = Tok(persist)
        if not persist:
            self.phase_toks.append(t)
        return t

    def _dsem(self, tok, q=None):
        if tok.dsem is not None and tok.dsem.total >= self.DMAX and q is not None:
            old = tok.dsem
            self._wait_hv(q, old.h, old.total)
            tok.old = getattr(tok, "old", [])
            tok.dsem = None
            self.retired.append(old)
        if tok.dsem is None:
            while self.free_dsems and self.free_dsems[-1].total >= self.DMAX - 64:
                self.free_dsems.pop()
            if self.free_dsems:
                tok.dsem = self.free_dsems.pop()
            else:
                self.nsem += 1
                tok.dsem = DSem(self.stack.enter_context(self.nc.semaphore(f"ds{self.nsem}")))
                self.all_dsems.append(tok.dsem)
        return tok.dsem

    def _wait_hv(self, eng, h, val):
        if val <= 0:
            return
        sd = self.seen[eng]
        if sd.get(id(h), 0) >= val:
            return
        sd[id(h)] = val
        self.e[eng].wait_ge(h, val)
        self.n_inst += 1

    def _wait(self, eng, dep):
        if dep is None:
            return
        if dep[0] == "c":
            self._wait_hv(eng, dep[2], dep[3])
        else:
            self._wait_hv(eng, dep[1].h, dep[1].total)

    def _deps(self, eng, reads, writes, accum=False):
        for t in reads:
            self._wait(eng, t.w)
        for t in writes:
            if not (accum and eng == "pe" and t.w is not None and t.w[0] == "c" and t.w[1] == "pe"):
                self._wait(eng, t.w)
            for d in t.r:
                self._wait(eng, d)

    def _post(self, dep, reads, writes):
        for t in reads:
            if len(t.r) > 6:
                t.r = t.r[-6:] if False else t.r
            t.r.append(dep)
        for t in writes:
            t.w = dep
            t.r = []

    def threads(self, bodies, ways=2):
        for g0 in range(0, len(bodies), ways):
            grp = bodies[g0:g0 + ways]
            recs = []
            for slot, b in enumerate(grp):
                self.rec = []
                b(slot)
                recs.append(self.rec)
                self.rec = None
            for k in range(max(len(r) for r in recs)):
                for r in recs:
                    if k < len(r):
                        kind, a, kw = r[k]
                        getattr(self, kind)(*a, **kw)

    def op(self, eng, fn, reads=(), writes=(), accum=False):
        if getattr(self, "rec", None) is not None:
            self.rec.append(("op", (eng, fn), dict(reads=list(reads), writes=list(writes), accum=accum)))
            return None
        self._deps(eng, reads, writes, accum)
        ins = fn(self.e[eng])
        if eng == "pe":
            self.npe += 1
        if self.ccnt[eng] >= self.EPOCH:
            self._new_epoch(eng)
        self.ccnt[eng] += 1
        h, val = self.csem[eng], self.ccnt[eng]
        ins.then_inc(h, 1)
        dep = ("c", eng, h, val)
        for t in reads:
            t.r = [d for d in t.r if not (d[0] == "c" and d[1] == eng and d[2] is h)]
        self._post(dep, reads, writes)
        self.n_inst += 1
        return ins

    def dma(self, q, out, in_, reads=(), writes=(), pace=(), **kw):
        if getattr(self, "rec", None) is not None:
            self.rec.append(("dma", (q, out, in_), dict(reads=list(reads), writes=list(writes), pace=list(pace), **kw)))
            return None
        ds = self._dsem(writes[0], q)
        for t in pace:
            self._wait(q, t.w)
        for t in reads:
            self._wait(q, t.w)
        for t in writes:
            if not (t.w is not None and t.w[0] == "d" and t.w[1] is ds):
                self._wait(q, t.w)
            for d in t.r:
                self._wait(q, d)
        ins = self.e[q].dma_start(out=out, in_=in_, **kw)
        ins.then_inc(ds.h, 16)
        ds.total += 16
        dep = ("d", ds)
        for t in reads:
            t.r = [d for d in t.r if not (d[0] == "d" and d[1] is ds)]
        self._post(dep, reads, writes)
        self.n_inst += 1
        return ins

    def indirect(self, out, out_off, in_, in_off, bound, reads=(), writes=()):
        if getattr(self, "rec", None) is not None:
            self.rec.append(("indirect", (out, out_off, in_, in_off, bound), dict(reads=list(reads), writes=list(writes))))
            return None
        ds = self._dsem(writes[0], "pool")
        self._deps("pool", reads, writes)
        if not hasattr(self, "_breg"):
            self._breg = {}
        if bound not in self._breg:
            self._breg[bound] = self.nc.gpsimd.to_reg(bound)
        ins = self.nc.gpsimd.indirect_dma_start(out=out, out_offset=out_off, in_=in_, in_offset=in_off,
                                                bounds_check=self._breg[bound], oob_is_err=False)
        ins.then_inc(ds.h, 16)
        ds.total += 16
        dep = ("d", ds)
        for t in reads:
            t.r = [d for d in t.r if not (d[0] == "d" and d[1] is ds)]
        self._post(dep, reads, writes)
        self.n_inst += 1
        return ins

    def wait_tok(self, eng, tok):
        self._wait(eng, tok.w)
        for d in tok.r:
            self._wait(eng, d)

    def barrier(self):
        for e in self.ENG:
            for o in self.ENG:
                if self.prev[o] is not None:
                    self._wait_hv(e, *self.prev[o])
                self._wait_hv(e, self.csem[o], self.ccnt[o])
            for ds in self.all_dsems:
                if not ds.bg:
                    self._wait_hv(e, ds.h, ds.total)
        for t in self.phase_toks:
            if t.dsem is not None:
                self.free_dsems.append(t.dsem)
                t.dsem = None
            t.w = None
            t.r = []
        self.phase_toks = []


def _host_consts():
    c = {}
    i = np.arange(128)
    c["ident_f"] = np.eye(128, dtype=np.float32)
    c["ones_f"] = np.ones((128, 128), np.float32)
    bo = np.zeros((128, 128), np.float32)
    bo[:64, :64] = 1.0
    bo[64:, 64:] = 1.0
    c["blockones"] = bo
    ps = np.zeros((128, 128), np.float32)
    ps[i ^ 1, i] = 1.0
    c["pswap"] = ps
    c["Mf"] = (i[:, None] <= i[None, :]).astype(np.float32)
    c["Mb"] = (i[:, None] >= i[None, :]).astype(np.float32)
    c["maskf"] = np.where(i[:, None] <= i[None, :], 0.0, BIGNEG).astype(np.float32)
    c["maskb"] = np.where(i[:, None] >= i[None, :], 0.0, BIGNEG).astype(np.float32)
    mL = np.where(i[:, None] >= i[None, :], 0.0, 8 * BIGNEG).astype(np.float32)
    mR = np.where(i[:, None] <= i[None, :], 0.0, 8 * BIGNEG).astype(np.float32)
    c["amaskL"] = np.tile(mL, (1, 4))
    c["amaskR"] = np.tile(mR, (1, 4))
    c["iota32"] = np.tile(np.arange(32, dtype=np.float32)[None, :], (128, 1))
    c["Ltri"] = (i[:, None] < i[None, :]).astype(np.float32)
    c["iota128"] = np.tile(np.arange(128, dtype=np.float32)[None, :], (128, 1))
    c["hpc"] = np.stack([np.arange(128, dtype=np.float32), 128.0 + np.arange(128, dtype=np.float32)], axis=1)
    t = np.arange(NLAT)
    row = (t // 64).astype(np.float64)
    col = (t % 64).astype(np.float64)
    inv = 10000.0 ** (-np.arange(16) / 16.0)
    ang = np.concatenate([row[:, None] * inv, col[:, None] * inv], -1)
    cos = np.cos(ang).astype(np.float32)
    sin = np.sin(ang).astype(np.float32)
    dd = np.arange(128) % 64
    fi = dd // 2
    sgn = np.where(dd % 2 == 0, -1.0, 1.0).astype(np.float32)
    c["ropec"] = np.ascontiguousarray(cos[:, fi].T)
    c["ropes"] = np.ascontiguousarray((sin[:, fi] * sgn[None, :]).T)
    cc = np.arange(256)
    a = 2 * np.pi * np.outer(cc, cc) / 256.0
    c["cosC"] = np.ascontiguousarray(np.cos(a).reshape(2, 128, 256).transpose(1, 0, 2)).astype(NPBF)
    c["sinC"] = np.ascontiguousarray(np.sin(a).reshape(2, 128, 256).transpose(1, 0, 2)).astype(NPBF)
    tt = np.arange(NLAT, dtype=np.int64)
    ph = (np.outer(tt, tt) % NLAT).astype(np.float64) * (2 * np.pi / NLAT)
    ct = (np.cos(ph) / 1024.0).astype(np.float32)
    st = (-np.sin(ph) / 1024.0).astype(np.float32)
    c["cosT"] = np.ascontiguousarray(ct.reshape(32, 128, 16, 256).transpose(2, 1, 0, 3)).astype(NPBF)
    c["sinT"] = np.ascontiguousarray(st.reshape(32, 128, 16, 256).transpose(2, 1, 0, 3)).astype(NPBF)
    tc_ = np.arange(NCTX)
    phc = (np.outer(tc_, tc_) % NCTX).astype(np.float64) * (2 * np.pi / NCTX)
    c["cosTc"] = np.ascontiguousarray((np.cos(phc) / 256.0).reshape(2, 128, 256).transpose(1, 0, 2)).astype(NPBF)
    c["sinTc"] = np.ascontiguousarray((-np.sin(phc) / 256.0).reshape(2, 128, 256).transpose(1, 0, 2)).astype(NPBF)
    return c


def _fm(v):
    v = np.asarray(v, np.float32)
    return np.ascontiguousarray(v.reshape(-1, 128).T)


_DT = {np.dtype(np.float32): F32, np.dtype(NPBF): BF16, np.dtype(np.int32): I32}


class Prog:
    def __init__(self, nc, in_shapes, n_layers=2, stop_after=None, debug=()):
        self.nc = nc
        self.L = n_layers
        self.stop_after = stop_after
        self.debug = set(debug)
        self.I = {}
        for name, (shape, dt) in in_shapes.items():
            self.I[name] = nc.dram_tensor(name, list(shape), _DT[np.dtype(dt)], kind="ExternalInput").ap()
        self.out = nc.dram_tensor("out", [D, NLAT], F32, kind="ExternalOutput").ap()

    def dram(self, name, shape, dt):
        kind = "ExternalOutput" if name in self.debug else "Internal"
        return self.nc.dram_tensor(name, list(shape), dt, kind=kind).ap()

    def sb(self, name, shape, dt=F32):
        self._n += 1
        return self.ph.enter_context(self.nc.sbuf_tensor(f"{name}_{self._n}", list(shape), dt))

    def ps(self, name, shape, dt=F32):
        self._n += 1
        return self.ph.enter_context(self.nc.psum_tensor(f"{name}_{self._n}", list(shape), dt))

    @contextlib.contextmanager
    def phase(self):
        old = getattr(self, "ph", None)
        with contextlib.ExitStack() as st:
            self.ph = st
            yield
            self.S.barrier()
        self.ph = old

    def build(self):
        nc = self.nc
        self._n = 0
        with contextlib.ExitStack() as top:
            self.S = S = Sched(nc, top)
            self.phase_log = []
            self.ph = top
            I = self.I
            self.XT = self.dram("XT", [D, NT], F32)
            self.X1T = self.dram("X1T", [D, NT], F32)
            self.QK32 = self.dram("QK32", [1280, NT], F32)
            self.PFM = self.dram("PFM", [9216, NT], BF16)
            self.PTM = self.dram("PTM", [NT, 1792], BF16)
            self.MG = self.dram("MG", [NT, 32], F32)
            self.QT = self.dram("QT", [1280, NT], BF16)
            self.AT = self.dram("AT", [1024, NT], BF16)
            self.FT = self.dram("FT", [1024, NT], BF16)
            self.MT = self.dram("MT", [1024, NT], BF16)
            self.HD = [self.dram(f"HD{d}", [1024, NT], F32) for d in range(2)]
            self.PP = self.dram("PP", [NT, 1024], BF16)
            self.QQ = self.dram("QQ", [NT, 1024], BF16)
            self.YT = self.dram("YT", [D, NT], BF16)
            self.XS = self.dram("XS", [NBLK * 128, D], BF16)
            self.H2 = self.dram("H2", [NT, D], BF16)
            self.YS = self.dram("YS", [NBLK * 128, D], F32)
            self._try("after_dram")
            self.WB = {k: self.dram(f"{k}B", [NE * 2 * 128, 8192], BF16) for k in ("w1", "w3", "w2")}
            self.tkWB = {k: S.tok(True) for k in ("w1", "w3", "w2")}
            for k in self.tkWB:
                S._dsem(self.tkWB[k]).bg = True
            self.tk = {n: S.tok(True) for n in ("XT", "X1T", "QK32", "PFM", "PTM", "MG", "QT", "AT", "FT", "MT", "HD0", "HD1",
                                                "PP", "QQ", "YT", "XS", "YS", "OUT", "H2")}
            self.C = {}
            self.tC = S.tok(True)
            for n in ("ident_f", "ones_f", "blockones", "pswap", "Mf", "Mb", "maskf", "maskb", "iota32", "Ltri"):
                t = self.sb(n, I[n].shape, F32)
                S.dma("sp", t[:], I[n], writes=[self.tC])
                self.C[n] = t
            for n, src in (("ident_b", "ident_f"), ("ones_b", "ones_f"), ("Ltri_b", "Ltri")):
                t = self.sb(n, [128, 128], BF16)
                S.op("dve", lambda e, t=t, src=src: e.tensor_copy(t[:], self.C[src][:]), reads=[self.tC], writes=[self.tC])
                self.C[n] = t
            self._try("after_consts")
            self.modT = self.sb("modT", [128, 96, 2], F32)
            self.A1 = self.sb("A1", [128, 16, 2], F32)
            self.A2 = self.sb("A2", [128, 16, 2], F32)
            self.tmod = S.tok(True)
            self.DEST = self.sb("DEST", [128, NB, 2], I32)
            self.WT = self.sb("WTS", [128, NB, 2], F32)
            self.troute = S.tok(True)
            self.BE = self.sb("BE", [128, NBLK], F32)
            self.BIDX = self.sb("BIDX", [128, NBLK, 2], I32)
            for r in range(16):
                S.dma("sp", self.XT[r * 128:(r + 1) * 128, :], I["xT0"][r * 128:(r + 1) * 128, :], writes=[self.tk["XT"]])
            self._try("before_barrier0")
            S.barrier()
            self._try("after_barrier0")
            for l in range(self.L):
                need_ctx = l < self.L - 1
                last = l == self.L - 1
                for name, fn in (("mod", self.ph_mod), ("proj", self.ph_proj), ("qk", self.ph_qkprep), ("attn", self.ph_attn),
                                 ("four", self.ph_fourier), ("mlstm", self.ph_mlstm), ("mlout", self.ph_mlout),
                                 ("mergea", self.ph_merge_a), ("mergeb", self.ph_merge_b), ("route", self.ph_route),
                                 ("moe", self.ph_moe), ("comb", self.ph_combine)):
                    import os
                    if "ONLY_PHASES" in os.environ and name not in os.environ["ONLY_PHASES"].split(","):
                        continue
                    with self.nc.named_scope(f"L{l}_{name}"):
                        with self.phase():
                            fn(l, need_ctx, last)
                    self.phase_log.append((l, name, self.S.npe))
                    if self.stop_after == (l, name):
                        break
                else:
                    continue
                break
            if self.stop_after is not None:
                S.dma("sp", self.out[0:128, 0:128], self.XT[0:128, 0:128], reads=[self.tk["XT"]], writes=[self.tk["OUT"]])
            for t in self.tk.values():
                S.wait_tok("sp", t)
            S.barrier()
            print("instructions:", S.n_inst, "sems:", S.nsem)
            import os
            if "PHASELOG" in os.environ:
                print("PHASELOG", self.phase_log)

    def _try(self, tag):
        import os
        if "TRYDBG" not in os.environ:
            return
        if not hasattr(self, "_tix"):
            self._tix = self.nc.alloc_sbuf_tensor("tix", [128, 1], I32) if False else None
        try:
            self._n += 1
            with self.nc.sbuf_tensor(f"tix{self._n}", [128, 1], I32) as ix, self.nc.sbuf_tensor(f"tH{self._n}", [128, 2048], BF16) as H:
                self.nc.gpsimd.indirect_dma_start(out=self.XS[:, :], out_offset=bass.IndirectOffsetOnAxis(ap=ix[:, :], axis=0), in_=H[:, :], in_offset=None, bounds_check=NE * CAP - 1, oob_is_err=False)
            print("TRY ok", tag)
        except Exception as e:
            print("TRY FAIL", tag, e)

    def ph_mod(self, l, need_ctx, last):
        S, I = self.S, self.I
        sT = self.sb("sT", [128, 16, 2]); tsT = S.tok()
        S.dma("sp", sT[:], I["cT"], writes=[tsT])
        S.op("act", lambda e: e.activation(sT[:], sT[:], AF.Silu), reads=[tsT], writes=[tsT])
        bm = self.sb("bm", [128, 96]); tbm = S.tok()
        S.dma("sp", bm[:], I[f"bmod{l}"], writes=[tbm])
        W = [self.sb("Wm", [128, 16, 512]) for _ in range(2)]
        tW = [S.tok() for _ in range(2)]
        P = [self.ps("pm", [128, 4, 2]) for _ in range(2)]
        tP = [S.tok() for _ in range(2)]
        wsrc = I[f"w_mod{l}"].rearrange("(k p) n -> p k n", p=128)

        def load(t):
            S.dma("sp", W[t % 2][:], wsrc[:, :, t * 512:(t + 1) * 512], writes=[tW[t % 2]])
        load(0)
        for t in range(24):
            if t + 1 < 24:
                load(t + 1)
            w, p = W[t % 2], P[t % 2]
            for j in range(4):
                for kc in range(16):
                    S.op("pe", lambda e, j=j, kc=kc: e.matmul(p[:, j, :], w[:, kc, j * 128:(j + 1) * 128], sT[:, kc, :],
                                                              start=(kc == 0), stop=(kc == 15)),
                         reads=[tW[t % 2], tsT], writes=[tP[t % 2]], accum=True)
            S.op("dve", lambda e: e.tensor_tensor(self.modT[:, t * 4:(t + 1) * 4, :], p[:],
                                                  bm[:, t * 4:(t + 1) * 4].unsqueeze(2).broadcast_to([128, 4, 2]), op=ALU.add),
                 reads=[tP[t % 2], tbm], writes=[self.tmod])
        ng = self.sb("ng", [128, 2, 16]); tng = S.tok()
        S.dma("sp", ng[:], I[f"ng{l}"], writes=[tng])
        for A, off, gi in ((self.A1, 16, 0), (self.A2, 64, 1)):
            S.op("dve", lambda e, A=A, off=off: e.tensor_scalar(A[:], self.modT[:, off:off + 16, :], 1.0, None, op0=ALU.add),
                 reads=[self.tmod], writes=[self.tmod])
            S.op("dve", lambda e, A=A, gi=gi: e.tensor_tensor(A[:], A[:], ng[:, gi, :].unsqueeze(2).broadcast_to([128, 16, 2]), op=ALU.mult),
                 reads=[self.tmod, tng], writes=[self.tmod])

    def norm_block(self, src, tsrc, c0, n, A, Bofs, j, X, SQ, rs, tmp, pss, toks, out_fn):
        S = self.S
        tX, tSQ, trs, ttmp, tps = toks
        S.dma("sp", X[:, :, 0:n], src.rearrange("(k p) t -> p k t", p=128)[:, :, c0:c0 + n], reads=[tsrc], writes=[tX])
        S.op("act", lambda e: e.activation(SQ[:, :, 0:n], X[:, :, 0:n], AF.Square), reads=[tX], writes=[tSQ])
        for kc in range(16):
            S.op("pe", lambda e, kc=kc: e.matmul(pss[:, 0:n], self.C["ones_f"][:], SQ[:, kc, 0:n], start=(kc == 0), stop=(kc == 15)),
                 reads=[tSQ, self.tC], writes=[tps], accum=True)
        S.op("dve", lambda e: e.tensor_scalar(rs[:, 0:n], pss[:, 0:n], 1.0 / D, EPS, op0=ALU.mult, op1=ALU.add), reads=[tps], writes=[trs])
        S.op("act", lambda e: e.activation(rs[:, 0:n], rs[:, 0:n], AF.Sqrt), reads=[trs], writes=[trs])
        S.op("dve", lambda e: e.reciprocal(rs[:, 0:n], rs[:, 0:n]), reads=[trs], writes=[trs])
        for kc in range(16):
            S.op("dve", lambda e, kc=kc: e.scalar_tensor_tensor(tmp[:, 0:n], X[:, kc, 0:n], A[:, kc, j:j + 1], rs[:, 0:n],
                                                                op0=ALU.mult, op1=ALU.mult),
                 reads=[tX, trs, self.tmod], writes=[ttmp])
            out_fn(kc, tmp[:, 0:n], self.modT[:, Bofs + kc, j:j + 1], ttmp)

    def ph_proj(self, l, need_ctx, last):
        S, I = self.S, self.I
        hT = self.sb("hT", [128, 16, 1024], BF16); thT = S.tok()
        X = self.sb("X", [128, 16, 512]); SQ = self.sb("SQ", [128, 16, 512]); rs = self.sb("rs", [128, 512]); tmp = self.sb("tmp", [128, 512])
        pss = self.ps("pss", [128, 512])
        ntoks = [S.tok() for _ in range(5)]
        W = [self.sb("W", [128, 16, 512], BF16) for _ in range(2)]
        tW = [S.tok() for _ in range(2)]
        PS = [self.ps("pp", [128, 512]) for _ in range(4)]
        tPS = [S.tok() for _ in range(4)]
        OB = [self.sb("ob", [128, 512], BF16) for _ in range(3)]
        OF = [self.sb("of", [128, 512], F32) for _ in range(3)]
        tOB = [S.tok() for _ in range(3)]
        tOF = [S.tok() for _ in range(3)]
        wfm = I[f"w_fm{l}"].rearrange("(k p) n -> p k n", p=128)
        wtm = I[f"w_tm{l}"].rearrange("(k p) n -> p k n", p=128)
        fm_tiles = [(t * 512, 512) for t in range(20)] + [(10240, 256)]
        tm_tiles = [(0, 512), (512, 512), (1024, 512), (1536, 512)]
        cnt = {"ps": 0, "ob": 0, "of": 0, "w": 0}
        import os
        sbs = [(0, 256, 1)] + [(256 + i * 1024, 1024, 0) for i in range(4)]
        sbs = sbs[:int(os.environ.get("PROJ_SB", "5"))]
        for (t0, nt, j) in sbs:
            for s0 in range(0, nt, 512):
                n = min(512, nt - s0)

                def outf(kc, tap, bias, ttmp, s0=s0, n=n):
                    S.op("act", lambda e: e.activation(hT[:, kc, s0:s0 + n], tap, AF.Identity, bias=bias, scale=1.0),
                         reads=[ttmp, self.tmod], writes=[thT])
                self.norm_block(self.XT, self.tk["XT"], t0 + s0, n, self.A1, 0, j, X, SQ, rs, tmp, pss, ntoks, outf)
            tiles = [("fm", c0, w) for c0, w in fm_tiles] + [("tm", c0, w) for c0, w in tm_tiles]
            if "PROJ_T0" in os.environ:
                tiles = tiles[int(os.environ["PROJ_T0"]):int(os.environ["PROJ_T1"])]

            def load(i):
                kind, c0, w = tiles[i]
                src = wfm if kind == "fm" else wtm
                S.dma("pool", W[i % 2][:, :, 0:w], src[:, :, c0:c0 + w], writes=[tW[i % 2]])
            load(0)
            for i, (kind, c0, w) in enumerate(tiles):
                if i + 1 < len(tiles):
                    load(i + 1)
                wt, twt = W[i % 2], tW[i % 2]
                if kind == "fm":
                    for jc in range(w // 128):
                        ci = (c0 + jc * 128) // 128
                        for s0 in range(0, nt, 512):
                            n = min(512, nt - s0)
                            pi = cnt["ps"] % 4; cnt["ps"] += 1
                            for kc in range(16):
                                S.op("pe", lambda e, kc=kc, jc=jc: e.matmul(PS[pi][:, 0:n], wt[:, kc, jc * 128:(jc + 1) * 128], hT[:, kc, s0:s0 + n],
                                                                          start=(kc == 0), stop=(kc == 15)),
                                     reads=[twt, thT], writes=[tPS[pi]], accum=True)
                            if ci < 10:
                                oi = cnt["of"] % 3; cnt["of"] += 1
                                S.op("act", lambda e: e.copy(OF[oi][:, 0:n], PS[pi][:, 0:n]), reads=[tPS[pi]], writes=[tOF[oi]])
                                S.dma("sp", self.QK32[ci * 128:(ci + 1) * 128, t0 + s0:t0 + s0 + n], OF[oi][:, 0:n], reads=[tOF[oi]], writes=[self.tk["QK32"]])
                            else:
                                oi = cnt["ob"] % 3; cnt["ob"] += 1
                                eng = "act" if (cnt["ob"] % 2) else "dve"
                                if 10 <= ci < 14:
                                    S.op("act", lambda e: e.mul(OB[oi][:, 0:n], PS[pi][:, 0:n], 0.125), reads=[tPS[pi]], writes=[tOB[oi]])
                                elif eng == "act":
                                    S.op("act", lambda e: e.copy(OB[oi][:, 0:n], PS[pi][:, 0:n]), reads=[tPS[pi]], writes=[tOB[oi]])
                                else:
                                    S.op("dve", lambda e: e.tensor_copy(OB[oi][:, 0:n], PS[pi][:, 0:n]), reads=[tPS[pi]], writes=[tOB[oi]])
                                r0 = (ci - 10) * 128
                                S.dma("sp", self.PFM[r0:r0 + 128, t0 + s0:t0 + s0 + n], OB[oi][:, 0:n], reads=[tOB[oi]], writes=[self.tk["PFM"]])
                else:
                    for b0 in range(0, nt, 128):
                        pi = cnt["ps"] % 4; cnt["ps"] += 1
                        for kc in range(16):
                            S.op("pe", lambda e, kc=kc: e.matmul(PS[pi][:, 0:w], hT[:, kc, b0:b0 + 128], wt[:, kc, 0:w], start=(kc == 0), stop=(kc == 15)),
                                 reads=[twt, thT], writes=[tPS[pi]], accum=True)
                        oi = cnt["ob"] % 3; cnt["ob"] += 1
                        wv = min(w, 512) if c0 < 1536 else 256
                        S.op("dve", lambda e: e.tensor_copy(OB[oi][:, 0:wv], PS[pi][:, 0:wv]), reads=[tPS[pi]], writes=[tOB[oi]])
                        S.dma("sp", self.PTM[t0 + b0:t0 + b0 + 128, c0:c0 + wv], OB[oi][:, 0:wv], reads=[tOB[oi]], writes=[self.tk["PTM"]])
                        if c0 == 1536:
                            oj = cnt["of"] % 3; cnt["of"] += 1
                            S.op("dve", lambda e: e.tensor_copy(OF[oj][:, 0:32], PS[pi][:, 256:288]), reads=[tPS[pi]], writes=[tOF[oj]])
                            S.dma("sp", self.MG[t0 + b0:t0 + b0 + 128, :], OF[oj][:, 0:32], reads=[tOF[oj]], writes=[self.tk["MG"]])

    def ph_qkprep(self, l, need_ctx, last):
        S, I = self.S, self.I
        g = self.sb("qkg", [128, 2]); tg = S.tok()
        S.dma("sp", g[:], I[f"qkg{l}"], writes=[tg])
        RC = [self.sb("rc", [128, 512]) for _ in range(2)]; RSn = [self.sb("rsn", [128, 512]) for _ in range(2)]; tR = [S.tok() for _ in range(2)]
        mk = lambda nm, dt=F32: [self.sb(nm, [128, 512], dt) for _ in range(4)]
        X, SQ, rs, xn, t1, t2, ob = mk("x"), mk("sq"), mk("rs"), mk("xn"), mk("t1"), mk("t2"), mk("ob", BF16)
        tk = lambda: [S.tok() for _ in range(4)]
        tX, tSQ, trs, txn, tt1, tt2, tob = tk(), tk(), tk(), tk(), tk(), tk(), tk()
        p1 = [self.ps("p1", [128, 512]) for _ in range(4)]; tp1 = tk()
        p2 = [self.ps("p2", [128, 512]) for _ in range(4)]; tp2 = tk()
        bodies = []
        for bidx, (t0, n) in enumerate([(0, 256)] + [(256 + i * 512, 512) for i in range(8)]):
            lat = t0 >= NCTX
            first = True
            for ci in range(10):
                if (not lat) and (not need_ctx) and ci < 8:
                    continue

                def body(sl, t0=t0, n=n, lat=lat, ci=ci, first=first, rp=bidx % 2):
                    x, tx, o, to = X[sl], tX[sl], ob[sl], tob[sl]
                    gi = 0 if ci < 8 else 1
                    if lat and first:
                        S.dma("sp", RC[rp][:, 0:n], I["ropec"][:, t0 - NCTX:t0 - NCTX + n], writes=[tR[rp]])
                        S.dma("sp", RSn[rp][:, 0:n], I["ropes"][:, t0 - NCTX:t0 - NCTX + n], writes=[tR[rp]])
                    S.dma("sp", x[:, 0:n], self.QK32[ci * 128:(ci + 1) * 128, t0:t0 + n], reads=[self.tk["QK32"]], writes=[tx])
                    S.op("act", lambda e: e.activation(SQ[sl][:, 0:n], x[:, 0:n], AF.Square), reads=[tx], writes=[tSQ[sl]])
                    S.op("pe", lambda e: e.matmul(p1[sl][:, 0:n], self.C["blockones"][:], SQ[sl][:, 0:n], start=True, stop=True), reads=[tSQ[sl], self.tC], writes=[tp1[sl]])
                    S.op("dve", lambda e: e.tensor_scalar(rs[sl][:, 0:n], p1[sl][:, 0:n], 1.0 / 64, EPS, op0=ALU.mult, op1=ALU.add), reads=[tp1[sl]], writes=[trs[sl]])
                    S.op("act", lambda e: e.activation(rs[sl][:, 0:n], rs[sl][:, 0:n], AF.Sqrt), reads=[trs[sl]], writes=[trs[sl]])
                    S.op("dve", lambda e: e.reciprocal(rs[sl][:, 0:n], rs[sl][:, 0:n]), reads=[trs[sl]], writes=[trs[sl]])
                    if lat:
                        S.op("dve", lambda e: e.scalar_tensor_tensor(xn[sl][:, 0:n], x[:, 0:n], g[:, gi:gi + 1], rs[sl][:, 0:n], op0=ALU.mult, op1=ALU.mult),
                             reads=[tx, trs[sl], tg], writes=[txn[sl]])
                        S.op("pe", lambda e: e.matmul(p2[sl][:, 0:n], self.C["pswap"][:], xn[sl][:, 0:n], start=True, stop=True), reads=[txn[sl], self.tC], writes=[tp2[sl]])
                        S.op("dve", lambda e: e.tensor_tensor(t1[sl][:, 0:n], xn[sl][:, 0:n], RC[rp][:, 0:n], op=ALU.mult), reads=[txn[sl], tR[rp]], writes=[tt1[sl]])
                        S.op("dve", lambda e: e.tensor_tensor(t2[sl][:, 0:n], p2[sl][:, 0:n], RSn[rp][:, 0:n], op=ALU.mult), reads=[tp2[sl], tR[rp]], writes=[tt2[sl]])
                        S.op("dve", lambda e: e.tensor_tensor(o[:, 0:n], t1[sl][:, 0:n], t2[sl][:, 0:n], op=ALU.add), reads=[tt1[sl], tt2[sl]], writes=[to])
                    else:
                        S.op("dve", lambda e: e.scalar_tensor_tensor(o[:, 0:n], x[:, 0:n], g[:, gi:gi + 1], rs[sl][:, 0:n], op0=ALU.mult, op1=ALU.mult),
                             reads=[tx, trs[sl], tg], writes=[to])
                    S.dma("sp", self.QT[ci * 128:(ci + 1) * 128, t0:t0 + n], o[:, 0:n], reads=[to], writes=[self.tk["QT"]])
                bodies.append(body)
                first = False
        S.threads(bodies, ways=4)

    def conv_next(self, pace_tok, k=1):
        for _ in range(k):
            if not getattr(self, "conv_q", None):
                return
            kind, r = self.conv_q.pop(0)
            self.S.dma("pool", self.WB[kind][r * 64:(r + 1) * 64, :], self.I[f"{kind}r_{self.conv_l}"][r * 64:(r + 1) * 64, :],
                       pace=[pace_tok], writes=[self.tkWB[kind]])

    def ph_attn(self, l, need_ctx, last):
        S, I = self.S, self.I
        self.conv_q = [(k, r) for r in range(128) for k in ("w1", "w3", "w2")]
        self.conv_l = l
        es = self.sb("es", [64, 16]); tes = S.tok()
        S.dma("sp", es[:], I[f"sink{l}"], writes=[tes])
        S.op("act", lambda e: e.activation(es[:], es[:], AF.Exp), reads=[tes], writes=[tes])
        mL = self.sb("mL", [128, 512]); mR = self.sb("mR", [128, 512]); tm = S.tok()
        S.dma("sp", mL[:], I["amaskL"], writes=[tm]); S.dma("sp", mR[:], I["amaskR"], writes=[tm])
        kT = self.sb("kT", [64, NT], BF16); tkT = S.tok()
        Q4 = self.sb("Q4", [64, 4, NT], BF16); tQ4 = S.tok()
        Vt = self.sb("Vt", [128, NB, 64], BF16); tVt = S.tok()
        AO = [self.sb("AO", [64, 4, 128], BF16) for _ in range(2)]; tAO = [S.tok() for _ in range(2)]
        ST = [self.ps("st", [128, 512]) for _ in range(4)]; tST = [S.tok() for _ in range(4)]
        OT = [self.ps("ot", [64, 512]) for _ in range(2)]; tOT = [S.tok() for _ in range(2)]
        DN = [self.ps("dn", [64, 512]) for _ in range(2)]; tDN = [S.tok() for _ in range(2)]
        E = [self.sb("E", [128, 512], BF16) for _ in range(3)]; tE = [S.tok() for _ in range(3)]
        tmpm = [self.sb("tmpm", [128, 512]) for _ in range(2)]; ttm = [S.tok() for _ in range(2)]
        dsum = self.sb("dsum", [64, 4, 128]); tds = S.tok()
        ke = 0; ks = 0; kq = 0
        qblocks = ([0, 1] if need_ctx else []) + list(range(2, NB))
        for hk in range(4):
            S.dma("sp", kT[:], self.QT[1024 + hk * 64:1024 + (hk + 1) * 64, :], reads=[self.tk["QT"]], writes=[tkT])
            S.dma("sp", Q4[:], self.QT[hk * 256:(hk + 1) * 256, :].rearrange("(h d) t -> d h t", d=64), reads=[self.tk["QT"]], writes=[tQ4])
            S.dma("sp", Vt[:], self.PTM[:, 1536 + hk * 64:1536 + (hk + 1) * 64].rearrange("(b p) d -> p b d", p=128),
                  reads=[self.tk["PTM"]], writes=[tVt])
            items = []
            for qi, n in enumerate(qblocks):
                if n < 2:
                    kbs = [(0, None), (1, None)]
                else:
                    kbs = []
                    if n - 1 >= 2:
                        kbs.append((n - 1, mL))
                    kbs.append((n, None))
                    if n + 1 < NB:
                        kbs.append((n + 1, mR))
                    kbs += [(0, None), (1, None)]
                for bi, (kb, msk) in enumerate(kbs):
                    items.append((qi, n, bi, kb, msk, len(kbs)))

            def emit_st(it, si):
                qi, n, bi, kb, msk, nk = it
                S.op("pe", lambda e: e.matmul(ST[si][:], kT[:, kb * 128:(kb + 1) * 128], Q4[:, :, n * 128:(n + 1) * 128], start=True, stop=True),
                     reads=[tkT, tQ4], writes=[tST[si]])
            emit_st(items[0], ks % 4)
            emit_st(items[1], (ks + 1) % 4)
            for ii, it in enumerate(items):
                qi, n, bi, kb, msk, nk = it
                si = ks % 4; ks += 1
                ei = ke % 3; ke += 1
                if ii + 2 < len(items):
                    emit_st(items[ii + 2], (ks + 1) % 4)
                if bi == 0:
                    oi = kq % 2; kq += 1
                if msk is not None:
                    S.op("dve", lambda e: e.tensor_tensor(tmpm[si % 2][:], ST[si][:], msk[:], op=ALU.add), reads=[tST[si], tm], writes=[ttm[si % 2]])
                    S.op("act", lambda e: e.activation(E[ei][:], tmpm[si % 2][:], AF.Exp, scale=0.125), reads=[ttm[si % 2]], writes=[tE[ei]])
                else:
                    S.op("act", lambda e: e.activation(E[ei][:], ST[si][:], AF.Exp, scale=0.125), reads=[tST[si]], writes=[tE[ei]])
                S.op("pe", lambda e: e.matmul(OT[oi][:], Vt[:, kb, :], E[ei][:], start=(bi == 0), stop=(bi == nk - 1)),
                     reads=[tVt, tE[ei]], writes=[tOT[oi]], accum=True)
                S.op("pe", lambda e: e.matmul(DN[oi][:], self.C["ones_b"][:, 0:64], E[ei][:], start=(bi == 0), stop=(bi == nk - 1)),
                     reads=[self.tC, tE[ei]], writes=[tDN[oi]], accum=True)
                if bi == nk - 1:
                    S.op("dve", lambda e: e.tensor_tensor(dsum[:], DN[oi][:].rearrange("p (h t) -> p h t", h=4),
                                                          es[:, hk * 4:(hk + 1) * 4].unsqueeze(2).broadcast_to([64, 4, 128]), op=ALU.add),
                         reads=[tDN[oi], tes], writes=[tds])
                    S.op("dve", lambda e: e.reciprocal(dsum[:], dsum[:]), reads=[tds], writes=[tds])
                    ao = AO[qi % 2]
                    S.op("dve", lambda e: e.tensor_tensor(ao[:], OT[oi][:].rearrange("p (h t) -> p h t", h=4), dsum[:], op=ALU.mult),
                         reads=[tOT[oi], tds], writes=[tAO[qi % 2]])
                    S.dma("sp", self.AT[hk * 256:(hk + 1) * 256, n * 128:(n + 1) * 128].rearrange("(h d) t -> d h t", d=64), ao[:], reads=[tAO[qi % 2]], writes=[self.tk["AT"]])
                    self.conv_next(tAO[qi % 2])

    def ph_fourier(self, l, need_ctx, last):
        S, I = self.S, self.I
        with contextlib.ExitStack() as st1:
            old = self.ph; self.ph = st1
            cC = self.sb("cC", [128, 2, 256], BF16); sC = self.sb("sC", [128, 2, 256], BF16); tcs = S.tok()
            S.dma("sp", cC[:], I["cosC"], writes=[tcs]); S.dma("sp", sC[:], I["sinC"], writes=[tcs])
            U = [self.sb("U", [128, 8, 128], BF16) for _ in range(2)]; tU = [S.tok() for _ in range(2)]
            PPs = [[self.ps("ppp", [128, 4, 256]) for _ in range(2)] for _ in range(2)]; tPP = [[S.tok() for _ in range(2)] for _ in range(2)]
            ob = [[self.sb("ob", [128, 1024], BF16) for _ in range(2)] for _ in range(2)]; tob = [[S.tok() for _ in range(2)] for _ in range(2)]
            blocks = ([0, 1] if need_ctx else []) + list(range(2, NB))
            bodies = []
            for b_ in blocks:
                def body(sl, b=b_):
                    u, tu = U[sl], tU[sl]
                    S.dma("sp", u[:], self.PFM[2048:3072, b * 128:(b + 1) * 128].rearrange("(k p) t -> p k t", p=128), reads=[self.tk["PFM"]], writes=[tu])
                    for wi, (tab, dst, tkn) in enumerate(((cC, self.PP, "PP"), (sC, self.QQ, "QQ"))):
                        pp, tpp = PPs[sl][wi], tPP[sl][wi]
                        for g in range(4):
                            for cc in range(2):
                                S.op("pe", lambda e, g=g, cc=cc, pp=pp, tab=tab: e.matmul(pp[:, g, :], u[:, 2 * g + cc, :], tab[:, cc, :], start=(cc == 0), stop=(cc == 1)),
                                     reads=[tu, tcs], writes=[tpp], accum=True)
                        o, to = ob[sl][wi], tob[sl][wi]
                        if wi == 0:
                            S.op("act", lambda e, o=o, pp=pp: e.copy(o[:], pp[:].rearrange("p g c -> p (g c)")), reads=[tpp], writes=[to])
                        else:
                            S.op("dve", lambda e, o=o, pp=pp: e.tensor_copy(o[:], pp[:].rearrange("p g c -> p (g c)")), reads=[tpp], writes=[to])
                        S.dma("sp", dst[b * 128:(b + 1) * 128, :], o[:], reads=[to], writes=[self.tk[tkn]])
                bodies.append(body)
            S.threads(bodies)
            S.barrier()
            self.ph = old
        PS = [self.ps("f2", [128, 256]) for _ in range(4)]; tPS = [S.tok() for _ in range(4)]
        FO = [self.sb("fo", [128, 256], BF16) for _ in range(3)]; tFO = [S.tok() for _ in range(3)]
        kp = 0; ko = 0
        if need_ctx:
            Pc = self.sb("Pc", [128, 2, 1024], BF16); Qc = self.sb("Qc", [128, 2, 1024], BF16); tpc = S.tok()
            S.dma("sp", Pc[:], self.PP[0:256, :].rearrange("(b p) c -> p b c", p=128), reads=[self.tk["PP"]], writes=[tpc])
            S.dma("sp", Qc[:], self.QQ[0:256, :].rearrange("(b p) c -> p b c", p=128), reads=[self.tk["QQ"]], writes=[tpc])
            cTc = self.sb("cTc", [128, 2, 256], BF16); sTc = self.sb("sTc", [128, 2, 256], BF16); ttc = S.tok()
            S.dma("sp", cTc[:], I["cosTc"], writes=[ttc]); S.dma("sp", sTc[:], I["sinTc"], writes=[ttc])
            for ch in range(8):
                pi = kp % 4; kp += 1
                oi = ko % 3; ko += 1
                k = 0
                for tb in range(2):
                    for (Pm, Tm) in ((Pc, cTc), (Qc, sTc)):
                        S.op("pe", lambda e, Pm=Pm, Tm=Tm, tb=tb, k=k: e.matmul(PS[pi][:], Pm[:, tb, ch * 128:(ch + 1) * 128], Tm[:, tb, :], start=(k == 0), stop=(k == 3)),
                             reads=[tpc, ttc], writes=[tPS[pi]], accum=True)
                        k += 1
                S.op("act", lambda e: e.copy(FO[oi][:], PS[pi][:]), reads=[tPS[pi]], writes=[tFO[oi]])
                S.dma("sp", self.FT[ch * 128:(ch + 1) * 128, 0:256], FO[oi][:], reads=[tFO[oi]], writes=[self.tk["FT"]])
        Ph = self.sb("Ph", [128, 32, 512], BF16); Qh = self.sb("Qh", [128, 32, 512], BF16); tph = S.tok()
        CT = [self.sb("CT", [128, 32, 256], BF16) for _ in range(2)]; STn = [self.sb("STn", [128, 32, 256], BF16) for _ in range(2)]
        tCT = [S.tok() for _ in range(2)]
        for half in range(2):
            S.dma("sp", Ph[:], self.PP[256:NT, half * 512:(half + 1) * 512].rearrange("(b p) c -> p b c", p=128), reads=[self.tk["PP"]], writes=[tph])
            S.dma("sp", Qh[:], self.QQ[256:NT, half * 512:(half + 1) * 512].rearrange("(b p) c -> p b c", p=128), reads=[self.tk["QQ"]], writes=[tph])

            def load(tq):
                S.dma("sp", CT[tq % 2][:], I["cosT"][tq], writes=[tCT[tq % 2]])
                S.dma("sp", STn[tq % 2][:], I["sinT"][tq], writes=[tCT[tq % 2]])
            load(0)
            for tq in range(16):
                if tq + 1 < 16:
                    load(tq + 1)
                for ch in range(4):
                    pi = kp % 4; kp += 1
                    oi = ko % 3; ko += 1
                    for tb in range(32):
                        S.op("pe", lambda e, tb=tb: e.matmul(PS[pi][:], Ph[:, tb, ch * 128:(ch + 1) * 128], CT[tq % 2][:, tb, :], start=(tb == 0), stop=False),
                             reads=[tph, tCT[tq % 2]], writes=[tPS[pi]], accum=True)
                    for tb in range(32):
                        S.op("pe", lambda e, tb=tb: e.matmul(PS[pi][:], Qh[:, tb, ch * 128:(ch + 1) * 128], STn[tq % 2][:, tb, :], start=False, stop=(tb == 31)),
                             reads=[tph, tCT[tq % 2]], writes=[tPS[pi]], accum=True)
                    if ch % 2:
                        S.op("act", lambda e: e.copy(FO[oi][:], PS[pi][:]), reads=[tPS[pi]], writes=[tFO[oi]])
                    else:
                        S.op("dve", lambda e: e.tensor_copy(FO[oi][:], PS[pi][:]), reads=[tPS[pi]], writes=[tFO[oi]])
                    r0 = half * 512 + ch * 128
                    S.dma("sp", self.FT[r0:r0 + 128, 256 + tq * 256:256 + (tq + 1) * 256], FO[oi][:], reads=[tFO[oi]], writes=[self.tk["FT"]])
                    self.conv_next(tFO[oi])

    def ph_mlstm(self, l, need_ctx, last):
        S, I, C = self.S, self.I, self.C
        gb = self.sb("gb", [128, 32]); tgb = S.tok()
        S.dma("sp", gb[:], I[f"mlgb{l}"], writes=[tgb])
        G = self.sb("G", [128, NB, 32]); tG = S.tok()
        S.dma("sp", G[:], self.MG.rearrange("(b p) c -> p b c", p=128), reads=[self.tk["MG"]], writes=[tG])
        S.op("dve", lambda e: e.tensor_tensor(G[:], G[:], gb[:, :].unsqueeze(1).broadcast_to([128, NB, 32]), op=ALU.add), reads=[tG, tgb], writes=[tG])
        Gv = G[:].rearrange("p b (d i h) -> p b d i h", d=2, i=2)
        LF = self.sb("LF", [128, NB, 2, 8]); tLF = S.tok()
        S.op("act", lambda e: e.activation(LF[:], Gv[:, :, :, 1, :], AF.Exp, scale=-1.0), reads=[tG], writes=[tLF])
        S.op("act", lambda e: e.activation(LF[:], LF[:], AF.Ln, bias=1.0), reads=[tLF], writes=[tLF])
        S.op("dve", lambda e: e.tensor_scalar(LF[:], LF[:], -1.0, None, op0=ALU.mult), reads=[tLF], writes=[tLF])
        two = lambda nm, shp, dt=F32: [self.sb(nm, shp, dt) for _ in range(2)]
        tk = lambda: [S.tok() for _ in range(2)]
        Cst = two("Cst", [64, 4, 129]); tCst = tk()
        Cbf = two("Cbf", [64, 4, 128], BF16); nrep = two("nrep", [64, 4, 128], BF16); tCb = tk()
        qT4 = two("qT4", [64, 4, 128], BF16); kT4 = two("kT4", [64, 4, 128], BF16)
        ktm = two("ktm", [128, 4, 64], BF16); vtm = two("vtm", [128, 4, 128], BF16); tin = tk()
        R = two("R", [128, 4, 128]); tR = tk()
        a4 = two("a4", [128, 4]); ta4 = tk()
        brs = two("brs", [128, 4, 128]); tbrs = tk()
        E1 = two("E1", [128, 4, 128]); tE1 = tk()
        Dm = two("Dm", [128, 4, 128]); tDm = tk()
        eb = two("eb", [128, 4, 128]); teb = tk()
        AT4 = two("AT4", [128, 4, 128], BF16); tAT4 = tk()
        qTs = two("qTs", [64, 4, 128], BF16); tqTs = tk()
        ad = two("ad", [128, 4, 128]); tad = tk()
        ho = two("ho", [128, 4, 128]); tho = tk()
        lw = two("lw", [128, 4]); tlw = tk()
        wk = two("wk", [128, 4, 64], BF16); twk = tk()
        psm_all = self.ps("psm", [128, 16]); tpsm = tk()
        pA = [self.ps("pA", [128, 4, 128]) for _ in range(2)]; tpA = tk()
        pB = [self.ps("pB", [128, 4, 128]) for _ in range(2)]; tpB = tk()
        pC = [self.ps("pC", [128, 4, 128]) for _ in range(2)]; tpC = tk()
        for d in range(2):
            M = C["Mf"] if d == 0 else C["Mb"]
            msk = C["maskf"] if d == 0 else C["maskb"]
            tl = 127 if d == 0 else 0
            order = [0, 1] + list(range(2, NB)) if d == 0 else [1, 0] + list(range(NB - 1, 1, -1))
            for hh in range(2):
                S.op("pool", lambda e, hh=hh: e.memset(Cst[hh][:], 0.0), writes=[tCst[hh]])
                S.op("pool", lambda e, hh=hh: e.memset(Cbf[hh][:], 0.0), writes=[tCb[hh]])
                S.op("pool", lambda e, hh=hh: e.memset(nrep[hh][:], 0.0), writes=[tCb[hh]])
            bodies = []
            for blk in order:
                for hh in range(2):
                    def body(sl, blk=blk, hh=hh, d=d, M=M, msk=msk, tl=tl):
                        assert sl == hh
                        c0 = blk * 128
                        hs = slice(hh * 4, hh * 4 + 4)
                        q4, k4, kt, vt, ti = qT4[sl], kT4[sl], ktm[sl], vtm[sl], tin[sl]
                        psm = psm_all[:, sl * 8:(sl + 1) * 8]
                        S.dma("sp", q4[:], self.PFM[hh * 256:(hh + 1) * 256, c0:c0 + 128].rearrange("(h d) t -> d h t", d=64), reads=[self.tk["PFM"]], writes=[ti])
                        S.dma("sp", k4[:], self.PFM[512 + hh * 256:512 + (hh + 1) * 256, c0:c0 + 128].rearrange("(h d) t -> d h t", d=64), reads=[self.tk["PFM"]], writes=[ti])
                        S.dma("sp", kt[:], self.PTM[c0:c0 + 128, 1024 + hh * 256:1024 + (hh + 1) * 256].rearrange("s (h d) -> s h d", d=64), reads=[self.tk["PTM"]], writes=[ti])
                        S.dma("sp", vt[:], self.PTM[c0:c0 + 128, hh * 512:(hh + 1) * 512].rearrange("s (h d) -> s h d", d=128), reads=[self.tk["PTM"]], writes=[ti])
                        lf4 = LF[:, blk, d, hs]
                        ig4 = Gv[:, blk, d, 0, hs]
                        S.op("pe", lambda e: e.matmul(psm[:, 0:4], M[:], lf4, start=True, stop=True), reads=[tLF, self.tC], writes=[tpsm[sl]])
                        S.op("dve", lambda e: e.tensor_tensor(R[sl][:], lf4.unsqueeze(2).broadcast_to([128, 4, 128]), M[:, :].unsqueeze(1).broadcast_to([128, 4, 128]), op=ALU.mult),
                             reads=[tLF, self.tC], writes=[tR[sl]])
                        S.op("pe", lambda e: e.matmul(pA[sl][:], C["ones_f"][:], R[sl][:], start=True, stop=True), reads=[tR[sl], self.tC], writes=[tpA[sl]])
                        S.op("dve", lambda e: e.tensor_tensor(a4[sl][:], ig4, psm[:, 0:4], op=ALU.subtract), reads=[tG, tpsm[sl]], writes=[ta4[sl]])
                        S.op("dve", lambda e: e.tensor_copy(brs[sl][:], pA[sl][:]), reads=[tpA[sl]], writes=[tbrs[sl]])
                        S.op("dve", lambda e: e.tensor_tensor(E1[sl][:], brs[sl][:], a4[sl][:, :].unsqueeze(2).broadcast_to([128, 4, 128]), op=ALU.add), reads=[tbrs[sl], ta4[sl]], writes=[tE1[sl]])
                        S.op("dve", lambda e: e.tensor_tensor(E1[sl][:], E1[sl][:], msk[:, :].unsqueeze(1).broadcast_to([128, 4, 128]), op=ALU.add), reads=[tE1[sl], self.tC], writes=[tE1[sl]])
                        S.op("act", lambda e: e.activation(Dm[sl][:], E1[sl][:], AF.Exp), reads=[tE1[sl]], writes=[tDm[sl]])
                        S.op("act", lambda e: e.activation(eb[sl][:], brs[sl][:], AF.Exp), reads=[tbrs[sl]], writes=[teb[sl]])
                        for h in range(4):
                            S.op("pe", lambda e, h=h: e.matmul(pA[sl][:, h, :], k4[:, h, :], q4[:, h, :], start=True, stop=True), reads=[ti], writes=[tpA[sl]], accum=(h > 0))
                        S.op("dve", lambda e: e.tensor_tensor(AT4[sl][:], pA[sl][:], Dm[sl][:], op=ALU.mult), reads=[tpA[sl], tDm[sl]], writes=[tAT4[sl]])
                        S.op("dve", lambda e: e.tensor_tensor(qTs[sl][:], q4[:], eb[sl][0:64], op=ALU.mult), reads=[ti, teb[sl]], writes=[tqTs[sl]])
                        for h in range(4):
                            S.op("pe", lambda e, h=h: e.matmul(pB[sl][:, h, :], vt[:, h, :], AT4[sl][:, h, :], start=True, stop=False), reads=[ti, tAT4[sl]], writes=[tpB[sl]], accum=(h > 0))
                            S.op("pe", lambda e, h=h: e.matmul(pB[sl][:, h, :], Cbf[sl][:, h, :], qTs[sl][:, h, :], start=False, stop=True), reads=[tCb[sl], tqTs[sl]], writes=[tpB[sl]], accum=True)
                        S.op("pe", lambda e: e.matmul(pC[sl][:], C["ones_b"][:], AT4[sl][:], start=True, stop=False), reads=[self.tC, tAT4[sl]], writes=[tpC[sl]])
                        for h in range(4):
                            S.op("pe", lambda e, h=h: e.matmul(pC[sl][:, h, :], nrep[sl][:, h, :], qTs[sl][:, h, :], start=False, stop=True, skip_group_check=True),
                                 reads=[tCb[sl], tqTs[sl]], writes=[tpC[sl]], accum=True)
                        S.op("act", lambda e: e.activation(ad[sl][:], pC[sl][:], AF.Abs), reads=[tpC[sl]], writes=[tad[sl]])
                        S.op("dve", lambda e: e.tensor_scalar(ad[sl][:], ad[sl][:], 1.0, None, op0=ALU.max), reads=[tad[sl]], writes=[tad[sl]])
                        S.op("dve", lambda e: e.reciprocal(ad[sl][:], ad[sl][:]), reads=[tad[sl]], writes=[tad[sl]])
                        S.op("dve", lambda e: e.tensor_tensor(ho[sl][:], pB[sl][:], ad[sl][:], op=ALU.mult), reads=[tpB[sl], tad[sl]], writes=[tho[sl]])
                        S.dma("sp", self.HD[d][hh * 512:(hh + 1) * 512, c0:c0 + 128].rearrange("(h p) t -> p h t", p=128), ho[sl][:], reads=[tho[sl]], writes=[self.tk[f"HD{d}"]])
                        self.conv_next(tho[sl])
                        S.op("dve", lambda e: e.tensor_tensor(lw[sl][:], a4[sl][:], brs[sl][:, :, tl], op=ALU.add), reads=[ta4[sl], tbrs[sl]], writes=[tlw[sl]])
                        S.op("act", lambda e: e.activation(lw[sl][:], lw[sl][:], AF.Exp), reads=[tlw[sl]], writes=[tlw[sl]])
                        S.op("dve", lambda e: e.tensor_tensor(wk[sl][:], kt[:], lw[sl][:, :].unsqueeze(2).broadcast_to([128, 4, 64]), op=ALU.mult), reads=[ti, tlw[sl]], writes=[twk[sl]])
                        for h in range(4):
                            S.op("pe", lambda e, h=h: e.matmul(pC[sl][0:64, h, :], wk[sl][:, h, :], vt[:, h, :], start=True, stop=True), reads=[twk[sl], ti], writes=[tpC[sl]], accum=(h > 0))
                            S.op("pe", lambda e, h=h: e.matmul(psm[0:64, 4 + h:5 + h], wk[sl][:, h, :], C["ones_b"][:, 0:1], start=True, stop=True), reads=[twk[sl], self.tC], writes=[tpsm[sl]], accum=(h > 0))
                        S.op("dve", lambda e: e.tensor_tensor(Cst[sl][:], Cst[sl][:], eb[sl][0:64, :, tl:tl + 1].broadcast_to([64, 4, 129]), op=ALU.mult), reads=[tCst[sl], teb[sl]], writes=[tCst[sl]])
                        S.op("dve", lambda e: e.tensor_tensor(Cst[sl][:, :, 0:128], Cst[sl][:, :, 0:128], pC[sl][0:64], op=ALU.add), reads=[tCst[sl], tpC[sl]], writes=[tCst[sl]])
                        S.op("dve", lambda e: e.tensor_tensor(Cst[sl][:, :, 128], Cst[sl][:, :, 128], psm[0:64, 4:8], op=ALU.add), reads=[tCst[sl], tpsm[sl]], writes=[tCst[sl]])
                        S.op("act", lambda e: e.copy(Cbf[sl][:], Cst[sl][:, :, 0:128]), reads=[tCst[sl]], writes=[tCb[sl]])
                        S.op("dve", lambda e: e.tensor_copy(nrep[sl][:], Cst[sl][:, :, 128:129].broadcast_to([64, 4, 128])), reads=[tCst[sl]], writes=[tCb[sl]])
                    bodies.append(body)
            S.threads(bodies)

    def ph_mlout(self, l, need_ctx, last):
        S, I, C = self.S, self.I, self.C
        ng = self.sb("mng", [128, 8]); tng = S.tok()
        S.dma("sp", ng[:], I[f"mlng{l}"], writes=[tng])
        self.conv_next(tng, k=1000)
        mk = lambda nm, dt=F32: [self.sb(nm, [128, 512], dt) for _ in range(4)]
        tk = lambda: [S.tok() for _ in range(4)]
        H0, H1, MO, hs_, sq, rs, sg, ob = mk("H0"), mk("H1"), mk("MO", BF16), mk("hs"), mk("sq"), mk("rs"), mk("sg"), mk("ob", BF16)
        tH, ths, tsq, trs, tsg, tob = tk(), tk(), tk(), tk(), tk(), tk()
        pp = [self.ps("pp", [128, 512]) for _ in range(4)]; tpp = tk()
        blocks = ([(0, 256)] if need_ctx else []) + [(256 + i * 512, 512) for i in range(8)]
        bodies = []
        for (t0, n) in blocks:
            for h in range(8):
                def body(i, t0=t0, n=n, h=h):
                    S.dma("sp", H0[i][:, 0:n], self.HD[0][h * 128:(h + 1) * 128, t0:t0 + n], reads=[self.tk["HD0"]], writes=[tH[i]])
                    S.dma("sp", H1[i][:, 0:n], self.HD[1][h * 128:(h + 1) * 128, t0:t0 + n], reads=[self.tk["HD1"]], writes=[tH[i]])
                    S.dma("sp", MO[i][:, 0:n], self.PFM[1024 + h * 128:1024 + (h + 1) * 128, t0:t0 + n], reads=[self.tk["PFM"]], writes=[tH[i]])
                    S.op("dve", lambda e: e.tensor_tensor(hs_[i][:, 0:n], H0[i][:, 0:n], H1[i][:, 0:n], op=ALU.add), reads=[tH[i]], writes=[ths[i]])
                    S.op("act", lambda e: e.activation(sq[i][:, 0:n], hs_[i][:, 0:n], AF.Square), reads=[ths[i]], writes=[tsq[i]])
                    S.op("pe", lambda e: e.matmul(pp[i][:, 0:n], C["ones_f"][:], sq[i][:, 0:n], start=True, stop=True), reads=[tsq[i], self.tC], writes=[tpp[i]])
                    S.op("dve", lambda e: e.tensor_scalar(rs[i][:, 0:n], pp[i][:, 0:n], 1.0 / 128, EPS, op0=ALU.mult, op1=ALU.add), reads=[tpp[i]], writes=[trs[i]])
                    S.op("act", lambda e: e.activation(rs[i][:, 0:n], rs[i][:, 0:n], AF.Sqrt), reads=[trs[i]], writes=[trs[i]])
                    S.op("dve", lambda e: e.reciprocal(rs[i][:, 0:n], rs[i][:, 0:n]), reads=[trs[i]], writes=[trs[i]])
                    S.op("act", lambda e: e.activation(sg[i][:, 0:n], MO[i][:, 0:n], AF.Sigmoid), reads=[tH[i]], writes=[tsg[i]])
                    S.op("dve", lambda e: e.scalar_tensor_tensor(hs_[i][:, 0:n], hs_[i][:, 0:n], ng[:, h:h + 1], rs[i][:, 0:n], op0=ALU.mult, op1=ALU.mult), reads=[ths[i], trs[i], tng], writes=[ths[i]])
                    S.op("dve", lambda e: e.tensor_tensor(ob[i][:, 0:n], hs_[i][:, 0:n], sg[i][:, 0:n], op=ALU.mult), reads=[ths[i], tsg[i]], writes=[tob[i]])
                    S.dma("sp", self.MT[h * 128:(h + 1) * 128, t0:t0 + n], ob[i][:, 0:n], reads=[tob[i]], writes=[self.tk["MT"]])
                bodies.append(body)
        S.threads(bodies, ways=4)

    def ph_merge_a(self, l, need_ctx, last):
        S, I = self.S, self.I
        Wb = self.sb("Wb", [128, 3, 8, 2048], BF16); tWb = S.tok()
        for br, nm in enumerate(("wba", "wbf", "wbm")):
            for hlf in range(2):
                S.dma("pool", Wb[:, br, hlf * 4:(hlf + 1) * 4, :], I[f"{nm}{l}"].rearrange("(k p) n -> p k n", p=128)[:, hlf * 4:(hlf + 1) * 4, :], writes=[tWb])
        bg = self.sb("bg", [128, 3, 16]); tbg = S.tok()
        S.dma("sp", bg[:], I[f"bgate{l}"], writes=[tbg])
        BR = [self.sb("BR", [128, 3, 8, 512], BF16) for _ in range(1)]; tBR = [S.tok() for _ in range(1)]
        GP = [self.sb("GP", [128, 3, 512], BF16) for _ in range(2)]; tGP = [S.tok() for _ in range(2)]
        gt = [self.sb("gt", [128, 3, 512]) for _ in range(2)]; tgt = [S.tok() for _ in range(2)]
        acc = [self.sb("acc", [128, 512]) for _ in range(2)]; tacc = [S.tok() for _ in range(2)]
        t2 = [self.sb("t2", [128, 512]) for _ in range(2)]; tt2 = [S.tok() for _ in range(2)]
        yo = [self.sb("yo", [128, 512], BF16) for _ in range(2)]; tyo = [S.tok() for _ in range(2)]
        PS = [[self.ps("pb", [128, 512]) for _ in range(3)] for _ in range(2)]; tPS = [[S.tok() for _ in range(3)] for _ in range(2)]
        blocks = ([(0, 256)] if need_ctx else []) + [(256 + i * 512, 512) for i in range(8)]
        for bi, (t0, n) in enumerate(blocks):
            br_, tbr_ = BR[0], tBR[0]
            for b3, (src, tkn) in enumerate(((self.AT, "AT"), (self.FT, "FT"), (self.MT, "MT"))):
                S.dma("sp", br_[:, b3, :, 0:n], src[:, t0:t0 + n].rearrange("(k p) t -> p k t", p=128), reads=[self.tk[tkn]], writes=[tbr_])
            bodies = []
            for nch in range(16):
                def body(i, nch=nch, t0=t0, n=n):
                    S.dma("sp", GP[i][:, :, 0:n], self.PFM[3072:9216, t0:t0 + n].rearrange("(b c p) t -> p b c t", b=3, p=128)[:, :, nch, :],
                          reads=[self.tk["PFM"]], writes=[tGP[i]])
                    for b3 in range(3):
                        for kc in range(8):
                            S.op("pe", lambda e, b3=b3, kc=kc: e.matmul(PS[i][b3][:, 0:n], Wb[:, b3, kc, nch * 128:(nch + 1) * 128], br_[:, b3, kc, 0:n], start=(kc == 0), stop=(kc == 7)),
                                 reads=[tWb, tbr_], writes=[tPS[i][b3]], accum=True)
                        S.op("act", lambda e, b3=b3: e.activation(gt[i][:, b3, 0:n], GP[i][:, b3, 0:n], AF.Sigmoid, bias=bg[:, b3, nch:nch + 1], scale=1.0),
                             reads=[tGP[i], tbg], writes=[tgt[i]])
                    S.op("dve", lambda e: e.tensor_tensor(acc[i][:, 0:n], PS[i][0][:, 0:n], gt[i][:, 0, 0:n], op=ALU.mult), reads=[tPS[i][0], tgt[i]], writes=[tacc[i]])
                    S.op("dve", lambda e: e.tensor_tensor(t2[i][:, 0:n], PS[i][1][:, 0:n], gt[i][:, 1, 0:n], op=ALU.mult), reads=[tPS[i][1], tgt[i]], writes=[tt2[i]])
                    S.op("dve", lambda e: e.tensor_tensor(acc[i][:, 0:n], acc[i][:, 0:n], t2[i][:, 0:n], op=ALU.add), reads=[tacc[i], tt2[i]], writes=[tacc[i]])
                    S.op("dve", lambda e: e.tensor_tensor(t2[i][:, 0:n], PS[i][2][:, 0:n], gt[i][:, 2, 0:n], op=ALU.mult), reads=[tPS[i][2], tgt[i]], writes=[tt2[i]])
                    S.op("dve", lambda e: e.tensor_tensor(yo[i][:, 0:n], acc[i][:, 0:n], t2[i][:, 0:n], op=ALU.add), reads=[tacc[i], tt2[i]], writes=[tyo[i]])
                    S.dma("sp", self.YT[nch * 128:(nch + 1) * 128, t0:t0 + n], yo[i][:, 0:n], reads=[tyo[i]], writes=[self.tk["YT"]])
                bodies.append(body)
            S.threads(bodies)

    def ph_merge_b(self, l, need_ctx, last):
        S, I = self.S, self.I
        Wo = self.sb("Wo", [128, 16, 2048], BF16); tWo = S.tok()
        for q4 in range(4):
            S.dma("pool", Wo[:, q4 * 4:(q4 + 1) * 4, :], I[f"wout{l}"].rearrange("(k p) n -> p k n", p=128)[:, q4 * 4:(q4 + 1) * 4, :], writes=[tWo])
        Y = [self.sb("Y", [128, 16, 512], BF16) for _ in range(1)]; X = [self.sb("X", [128, 16, 512]) for _ in range(1)]; tY = [S.tok() for _ in range(1)]
        xo = [self.sb("xo", [128, 512]) for _ in range(3)]; txo = [S.tok() for _ in range(3)]
        PS = [self.ps("po", [128, 512]) for _ in range(4)]; tPS = [S.tok() for _ in range(4)]
        blocks = ([(0, 256, 1)] if need_ctx else []) + [(256 + i * 512, 512, 0) for i in range(8)]
        k = 0
        for bi, (t0, n, j) in enumerate(blocks):
            y, x, ty = Y[0], X[0], tY[0]
            S.dma("sp", y[:, :, 0:n], self.YT[:, t0:t0 + n].rearrange("(k p) t -> p k t", p=128), reads=[self.tk["YT"]], writes=[ty])
            S.dma("sp", x[:, :, 0:n], self.XT[:, t0:t0 + n].rearrange("(k p) t -> p k t", p=128), reads=[self.tk["XT"]], writes=[ty])
            for nch in range(16):
                pi = k % 4; oi = k % 3; k += 1
                for kc in range(16):
                    S.op("pe", lambda e, kc=kc: e.matmul(PS[pi][:, 0:n], Wo[:, kc, nch * 128:(nch + 1) * 128], y[:, kc, 0:n], start=(kc == 0), stop=(kc == 15)),
                         reads=[tWo, ty], writes=[tPS[pi]], accum=True)
                S.op("dve", lambda e: e.scalar_tensor_tensor(xo[oi][:, 0:n], PS[pi][:, 0:n], self.modT[:, 32 + nch, j:j + 1], x[:, nch, 0:n], op0=ALU.mult, op1=ALU.add),
                     reads=[tPS[pi], ty, self.tmod], writes=[txo[oi]])
                S.dma("sp", self.X1T[nch * 128:(nch + 1) * 128, t0:t0 + n], xo[oi][:, 0:n], reads=[txo[oi]], writes=[self.tk["X1T"]])

    def ph_route(self, l, need_ctx, last):
        S, I, C = self.S, self.I, self.C
        WR = self.sb("WR", [128, 16, 36]); tWR = S.tok()
        S.dma("sp", WR[:], I[f"wr{l}"], writes=[tWR])
        rb = self.sb("rb", [128, 36]); trb = S.tok()
        S.dma("sp", rb[:], I[f"rbias{l}"], writes=[trb])
        io = self.sb("io128", [128, 128]); hp = self.sb("hp", [128, 2]); tio = S.tok()
        S.dma("sp", io[:], I["iota128"], writes=[tio]); S.dma("sp", hp[:], I["hpc"], writes=[tio])
        X = self.sb("X", [128, 16, 512]); SQ = self.sb("SQ", [128, 16, 512]); rs = self.sb("rs", [128, 512]); tmp = self.sb("tmp", [128, 512])
        pss = self.ps("pss", [128, 512])
        ntoks = [S.tok() for _ in range(5)]
        H32 = self.sb("H32", [128, 16, 512]); tH32 = S.tok()
        Hb = self.sb("Hb", [128, 16, 512], BF16); tHb = S.tok()
        Htm = [self.sb("Htm", [128, 2048], BF16) for _ in range(2)]; tHtm = [S.tok() for _ in range(2)]
        pT = [self.ps("pT", [128, 8, 128], BF16) for _ in range(4)]; tpT = [S.tok() for _ in range(4)]
        plc = [self.ps("plc", [128, 128]) for _ in range(2)]; tpl = [S.tok() for _ in range(2)]; tpc = [S.tok() for _ in range(2)]
        two = lambda nm, shp, dt=F32: [self.sb(nm, shp, dt) for _ in range(2)]
        tk2 = lambda: [S.tok() for _ in range(2)]
        lg = two("lg", [128, 36]); tlg = tk2()
        sm = two("sm", [128, 16]); tsm = tk2()
        ohg = two("ohg", [128, 4]); tohg = tk2()
        el = two("el", [128, 32]); tel = tk2()
        ohs = two("ohs", [128, 32], BF16); tohs = tk2()
        t32 = two("t32", [128, 32]); tt32 = tk2()
        OH = self.sb("OH", [128, NB, 2, 32]); tOH = S.tok()
        CNT = self.sb("CNT", [128, NB, 32]); tCNT = S.tok()
        COLS = self.sb("COLS", [128, NB, 32]); tCOLS = S.tok()
        base = self.sb("base", [128, 32]); tbase = S.tok()
        df = self.sb("df", [128, 2]); tdf = S.tok()
        IDX = [self.sb("idx", [128, 1], I32) for _ in range(4)]; tIDX = [S.tok() for _ in range(4)]
        S.op("pool", lambda e: e.memset(base[:], 0.0), writes=[tbase])
        zt = self.sb("zt", [128, 2048], BF16); tzt = S.tok()
        S.op("pool", lambda e: e.memset(zt[:], 0.0), writes=[tzt])
        xsv = self.XS.rearrange("(b p) d -> b p d", p=128)
        for zb in range(NBLK if l == 0 else 0):
            S.dma("sp", xsv[zb], zt[:], reads=[tzt], writes=[self.tk["XS"]])
        blocks = ([(0, 256, 1)] if need_ctx else []) + [(256 + i * 512, 512, 0) for i in range(8)]
        blks = []
        for (t0, n, j) in blocks:
            def outf(kc, tap, bias, ttmp):
                S.op("act", lambda e: e.activation(H32[:, kc, 0:n], tap, AF.Identity, bias=bias, scale=1.0), reads=[ttmp, self.tmod], writes=[tH32])
            self.norm_block(self.X1T, self.tk["X1T"], t0, n, self.A2, 48, j, X, SQ, rs, tmp, pss, ntoks, outf)
            S.op("dve", lambda e: e.tensor_copy(Hb[:, :, 0:n], H32[:, :, 0:n]), reads=[tH32], writes=[tHb])
            bodies = []
            for s0 in range(0, n, 128):
                blk = (t0 + s0) // 128
                blks.append(blk)

                def body(i, s0=s0, blk=blk):
                    pl = plc[i][:, 0:36]; pc = plc[i][:, 64:128]
                    for half in range(2):
                        pt, tpt = pT[2 * i + half], tpT[2 * i + half]
                        for kk in range(8):
                            kc = half * 8 + kk
                            S.op("pe", lambda e, kc=kc, kk=kk, pt=pt: e.transpose(pt[:, kk, :], Hb[:, kc, s0:s0 + 128], C["ident_b"][:]), reads=[tHb, self.tC], writes=[tpt], accum=True)
                        if half == 0:
                            S.op("act", lambda e, pt=pt: e.copy(Htm[i][:, 0:1024], pt[:].rearrange("p a b -> p (a b)")), reads=[tpt], writes=[tHtm[i]])
                        else:
                            S.op("dve", lambda e, pt=pt: e.tensor_copy(Htm[i][:, 1024:2048], pt[:].rearrange("p a b -> p (a b)")), reads=[tpt], writes=[tHtm[i]])
                    S.dma("sp", self.H2[blk * 128:(blk + 1) * 128, :], Htm[i][:], reads=[tHtm[i]], writes=[self.tk["H2"]])
                    for kc in range(16):
                        S.op("pe", lambda e, kc=kc: e.matmul(pl, H32[:, kc, s0:s0 + 128], WR[:, kc, :], start=(kc == 0), stop=(kc == 15)), reads=[tH32, tWR], writes=[tpl[i]], accum=True)
                    S.op("dve", lambda e: e.tensor_tensor(lg[i][:], pl, rb[:], op=ALU.add), reads=[tpl[i], trb], writes=[tlg[i]])
                    S.op("dve", lambda e: e.tensor_reduce(sm[i][:, 0:1], lg[i][:, 0:4], axis=AX.X, op=ALU.max), reads=[tlg[i]], writes=[tsm[i]])
                    S.op("dve", lambda e: e.tensor_tensor(ohg[i][:], lg[i][:, 0:4], sm[i][:, 0:1].broadcast_to([128, 4]), op=ALU.is_equal), reads=[tlg[i], tsm[i]], writes=[tohg[i]])
                    S.op("dve", lambda e: e.tensor_scalar(sm[i][:, 1:2], sm[i][:, 0:1], -1.0, None, op0=ALU.mult), reads=[tsm[i]], writes=[tsm[i]])
                    S.op("act", lambda e: e.activation(t32[i][:, 0:4], lg[i][:, 0:4], AF.Exp, bias=sm[i][:, 1:2], scale=1.0), reads=[tlg[i], tsm[i]], writes=[tt32[i]])
                    S.op("dve", lambda e: e.tensor_reduce(sm[i][:, 2:3], t32[i][:, 0:4], axis=AX.X, op=ALU.add), reads=[tt32[i]], writes=[tsm[i]])
                    S.op("dve", lambda e: e.reciprocal(sm[i][:, 3:4], sm[i][:, 2:3]), reads=[tsm[i]], writes=[tsm[i]])
                    S.op("dve", lambda e: e.tensor_scalar(ohg[i][:], ohg[i][:], -1.0, 1e30, op0=ALU.add, op1=ALU.mult), reads=[tohg[i]], writes=[tohg[i]])
                    S.op("dve", lambda e: e.tensor_tensor(el[i][:].rearrange("p (g x) -> p g x", g=4), lg[i][:, 4:36].rearrange("p (g x) -> p g x", g=4),
                                                          ohg[i][:, :].unsqueeze(2).broadcast_to([128, 4, 8]), op=ALU.add), reads=[tlg[i], tohg[i]], writes=[tel[i]])
                    S.op("dve", lambda e: e.tensor_reduce(sm[i][:, 4:5], el[i][:], axis=AX.X, op=ALU.max), reads=[tel[i]], writes=[tsm[i]])
                    S.op("dve", lambda e: e.tensor_tensor(OH[:, blk, 0, :], el[i][:], sm[i][:, 4:5].broadcast_to([128, 32]), op=ALU.is_equal), reads=[tel[i], tsm[i]], writes=[tOH])
                    S.op("dve", lambda e: e.scalar_tensor_tensor(el[i][:], OH[:, blk, 0, :], -1e30, el[i][:], op0=ALU.mult, op1=ALU.add), reads=[tOH, tel[i]], writes=[tel[i]])
                    S.op("dve", lambda e: e.tensor_reduce(sm[i][:, 5:6], el[i][:], axis=AX.X, op=ALU.max), reads=[tel[i]], writes=[tsm[i]])
                    S.op("dve", lambda e: e.tensor_tensor(OH[:, blk, 1, :], el[i][:], sm[i][:, 5:6].broadcast_to([128, 32]), op=ALU.is_equal), reads=[tel[i], tsm[i]], writes=[tOH])
                    S.op("dve", lambda e: e.tensor_tensor(sm[i][:, 6:7], sm[i][:, 5:6], sm[i][:, 4:5], op=ALU.subtract), reads=[tsm[i]], writes=[tsm[i]])
                    S.op("act", lambda e: e.activation(sm[i][:, 6:7], sm[i][:, 6:7], AF.Exp), reads=[tsm[i]], writes=[tsm[i]])
                    S.op("dve", lambda e: e.tensor_scalar(sm[i][:, 6:7], sm[i][:, 6:7], 1.0, None, op0=ALU.add), reads=[tsm[i]], writes=[tsm[i]])
                    S.op("dve", lambda e: e.reciprocal(sm[i][:, 6:7], sm[i][:, 6:7]), reads=[tsm[i]], writes=[tsm[i]])
                    S.op("dve", lambda e: e.tensor_tensor(self.WT[:, blk, 0:1], sm[i][:, 6:7], sm[i][:, 3:4], op=ALU.mult), reads=[tsm[i]], writes=[self.troute])
                    S.op("dve", lambda e: e.tensor_tensor(self.WT[:, blk, 1:2], sm[i][:, 3:4], self.WT[:, blk, 0:1], op=ALU.subtract), reads=[tsm[i], self.troute], writes=[self.troute])
                    S.op("dve", lambda e: e.tensor_tensor(ohs[i][:], OH[:, blk, 0, :], OH[:, blk, 1, :], op=ALU.add), reads=[tOH], writes=[tohs[i]])
                    S.op("pe", lambda e: e.matmul(pc[:, 0:32], C["Ltri_b"][:], ohs[i][:], start=True, stop=True), reads=[tohs[i], self.tC], writes=[tpc[i]], accum=True)
                    S.op("pe", lambda e: e.matmul(pc[:, 32:64], C["ones_b"][:], ohs[i][:], start=True, stop=True), reads=[tohs[i], self.tC], writes=[tpc[i]], accum=True)
                    S.op("dve", lambda e: e.tensor_copy(CNT[:, blk, :], pc[:, 0:32]), reads=[tpc[i]], writes=[tCNT])
                    S.op("dve", lambda e: e.tensor_copy(COLS[:, blk, :], pc[:, 32:64]), reads=[tpc[i]], writes=[tCOLS])
                bodies.append(body)
            S.threads(bodies)
        for blk in blks:
            S.op("dve", lambda e, blk=blk: e.tensor_tensor(CNT[:, blk, :], CNT[:, blk, :], base[:], op=ALU.add), reads=[tCNT, tbase], writes=[tCNT])
            S.op("dve", lambda e, blk=blk: e.tensor_tensor(base[:], base[:], COLS[:, blk, :], op=ALU.add), reads=[tCOLS, tbase], writes=[tbase])
        big = self.sb("big", [128, 32, 100]); tbig = S.tok()
        thr = self.sb("thr", [128, 100]); tthr = S.tok()
        nbk = self.sb("nbk", [128, 32]); pend = self.sb("pend", [128, 32]); pst = self.sb("pst", [128, 32]); tsc = S.tok()
        S.op("dve", lambda e: e.tensor_scalar(thr[:], io[:, 0:100], 128.0, None, op0=ALU.mult), reads=[tio], writes=[tthr])
        S.op("dve", lambda e: e.tensor_tensor(big[:], base[:, :].unsqueeze(2).broadcast_to([128, 32, 100]), thr[:, :].unsqueeze(1).broadcast_to([128, 32, 100]), op=ALU.is_gt),
             reads=[tbase, tthr], writes=[tbig])
        S.op("dve", lambda e: e.tensor_reduce(nbk[:], big[:], axis=AX.X, op=ALU.add), reads=[tbig], writes=[tsc])
        S.op("dve", lambda e: e.tensor_scalar(nbk[:], nbk[:], 128.0, None, op0=ALU.mult), reads=[tsc], writes=[tsc])
        tri = big[:, :, 0:32]
        S.op("dve", lambda e: e.tensor_tensor(tri, C["iota32"][:, :].unsqueeze(1).broadcast_to([128, 32, 32]), C["iota32"][:, :].unsqueeze(2).broadcast_to([128, 32, 32]), op=ALU.is_le),
             reads=[self.tC, tbig], writes=[tbig])
        S.op("dve", lambda e: e.tensor_tensor(tri, tri, nbk[:, :].unsqueeze(1).broadcast_to([128, 32, 32]), op=ALU.mult), reads=[tbig, tsc], writes=[tbig])
        S.op("dve", lambda e: e.tensor_reduce(pend[:], tri, axis=AX.X, op=ALU.add), reads=[tbig], writes=[tsc])
        S.op("dve", lambda e: e.tensor_tensor(pst[:], pend[:], nbk[:], op=ALU.subtract), reads=[tsc], writes=[tsc])
        big2 = self.sb("big2", [128, 100, 32]); tbig2 = S.tok()
        S.op("dve", lambda e: e.tensor_tensor(big2[:], pend[:, :].unsqueeze(1).broadcast_to([128, 100, 32]), thr[:, :].unsqueeze(2).broadcast_to([128, 100, 32]), op=ALU.is_le),
             reads=[tsc, tthr], writes=[tbig2])
        S.op("dve", lambda e: e.tensor_reduce(self.BE[:], big2[:], axis=AX.X, op=ALU.add), reads=[tbig2], writes=[self.troute])
        S.op("dve", lambda e: e.tensor_scalar(self.BE[:], self.BE[:], 256.0, None, op0=ALU.mult), reads=[self.troute], writes=[self.troute])
        bif = self.sb("bif", [128, 100, 2]); tbif = S.tok()
        S.op("dve", lambda e: e.tensor_tensor(bif[:], self.BE[:, :].unsqueeze(2).broadcast_to([128, 100, 2]), hp[:, :].unsqueeze(1).broadcast_to([128, 100, 2]), op=ALU.add),
             reads=[self.troute, tio], writes=[tbif])
        S.op("dve", lambda e: e.tensor_copy(self.BIDX[:], bif[:]), reads=[tbif], writes=[self.troute])
        df2 = [self.sb("df2", [128, 2]) for _ in range(2)]; tdf2 = [S.tok() for _ in range(2)]
        bodies = []
        for blk in blks:
            def body(i, blk=blk):
                S.dma("sp", Htm[i][:], self.H2[blk * 128:(blk + 1) * 128, :], reads=[self.tk["H2"]], writes=[tHtm[i]])
                S.op("dve", lambda e: e.tensor_tensor(t32[i][:], CNT[:, blk, :], pst[:], op=ALU.add), reads=[tCNT, tsc], writes=[tt32[i]])
                for s_ in range(2):
                    S.op("dve", lambda e, s_=s_: e.tensor_tensor(el[i][:], OH[:, blk, s_, :], t32[i][:], op=ALU.mult), reads=[tOH, tt32[i]], writes=[tel[i]])
                    S.op("dve", lambda e, s_=s_: e.tensor_reduce(df2[i][:, s_:s_ + 1], el[i][:], axis=AX.X, op=ALU.add), reads=[tel[i]], writes=[tdf2[i]])
                S.op("dve", lambda e: e.tensor_copy(self.DEST[:, blk, :], df2[i][:]), reads=[tdf2[i]], writes=[self.troute])
                for s_ in range(2):
                    ix = IDX[2 * i + s_]; tix = tIDX[2 * i + s_]
                    S.op("dve", lambda e, s_=s_, ix=ix: e.tensor_copy(ix[:, :], df2[i][:, s_:s_ + 1]), reads=[tdf2[i]], writes=[tix])
                    S.indirect(self.XS[:, :], bass.IndirectOffsetOnAxis(ap=ix[:, :], axis=0), Htm[i][:, :], None, NBLK * 128 - 1,
                               reads=[tHtm[i], tix], writes=[self.tk["XS"]])
            bodies.append(body)
        S.threads(bodies)

    def ph_moe(self, l, need_ctx, last):
        S, I, C = self.S, self.I, self.C
        NS = 10
        W = [self.sb("We", [128, 16, 512], BF16) for _ in range(NS)]; tW = [S.tok() for _ in range(NS)]
        xe = [self.sb("xe", [128, 2048], BF16) for _ in range(2)]; txe = [S.tok() for _ in range(2)]
        xeT = [self.sb("xeT", [128, 16, 128], BF16) for _ in range(2)]; txeT = [S.tok() for _ in range(2)]
        actT = self.sb("actT", [128, 8, 128], BF16); tact = S.tok()
        sl = [self.sb("sl", [128, 128]) for _ in range(2)]; tsl = [S.tok() for _ in range(2)]
        ye = [self.sb("ye", [128, 512]) for _ in range(3)]; tye = [S.tok() for _ in range(3)]
        pT = [self.ps("pT", [128, 8, 128], BF16) for _ in range(2)]; tpT = [S.tok() for _ in range(2)]
        p1 = [self.ps("p1", [128, 128]) for _ in range(2)]; tp1 = [S.tok() for _ in range(2)]
        p3 = [self.ps("p3", [128, 128]) for _ in range(2)]; tp3 = [S.tok() for _ in range(2)]
        p2 = [self.ps("p2", [128, 512]) for _ in range(2)]; tp2 = [S.tok() for _ in range(2)]
        nblk = (2 * (NT if need_ctx else NLAT)) // 128 + NE
        srcs = self.WB
        tiles = []
        for j in range(nblk):
            for h in range(2):
                tiles.append(("w1", j, h)); tiles.append(("w3", j, h))
            for h in range(2):
                tiles.append(("w2", j, h))

        def load(i):
            kind, j, h = tiles[i]
            w, tw = W[i % NS], tW[i % NS]
            S.indirect(w[:].rearrange("p a b -> p (a b)"), None, srcs[kind][:, :], bass.IndirectOffsetOnAxis(ap=self.BIDX[:, j, h:h + 1], axis=0), NE * 2 * 128 - 1,
                       reads=[self.troute, self.tkWB[kind]], writes=[tw])
        PF = 8
        for i in range(PF):
            load(i)
        ti = 0
        kk = 0
        S.dma("sp", xe[0][:], self.XS[0:128, :], reads=[self.tk["XS"]], writes=[txe[0]])
        for j in range(nblk):
            x, tx = xe[j % 2], txe[j % 2]
            xt, txt = xeT[j % 2], txeT[j % 2]
            if j + 1 < nblk:
                S.dma("sp", xe[(j + 1) % 2][:], self.XS[(j + 1) * 128:(j + 2) * 128, :], reads=[self.tk["XS"]], writes=[txe[(j + 1) % 2]])
            for half in range(2):
                pi = kk % 2; kk += 1
                for j8 in range(8):
                    kc = half * 8 + j8
                    S.op("pe", lambda e, kc=kc, j8=j8: e.transpose(pT[pi][:, j8, :], x[:, kc * 128:(kc + 1) * 128], C["ident_b"][:]), reads=[tx, self.tC], writes=[tpT[pi]], accum=True)
                if half == 0:
                    S.op("act", lambda e: e.copy(xt[:, 0:8, :], pT[pi][:]), reads=[tpT[pi]], writes=[txt])
                else:
                    S.op("dve", lambda e: e.tensor_copy(xt[:, 8:16, :], pT[pi][:]), reads=[tpT[pi]], writes=[txt])
            for fh in range(2):
                w1, tw1 = W[ti % NS], tW[ti % NS]
                w3, tw3 = W[(ti + 1) % NS], tW[(ti + 1) % NS]
                for q in range(2):
                    if ti + PF + q < len(tiles):
                        load(ti + PF + q)
                ti += 2
                for fc in range(4):
                    i2 = fc % 2
                    for kc in range(16):
                        S.op("pe", lambda e, kc=kc: e.matmul(p1[i2][:], w1[:, kc, fc * 128:(fc + 1) * 128], xt[:, kc, :], start=(kc == 0), stop=(kc == 15)),
                             reads=[tw1, txt], writes=[tp1[i2]], accum=True)
                    for kc in range(16):
                        S.op("pe", lambda e, kc=kc: e.matmul(p3[i2][:], w3[:, kc, fc * 128:(fc + 1) * 128], xt[:, kc, :], start=(kc == 0), stop=(kc == 15)),
                             reads=[tw3, txt], writes=[tp3[i2]], accum=True)
                    S.op("act", lambda e: e.activation(sl[i2][:], p1[i2][:], AF.Silu), reads=[tp1[i2]], writes=[tsl[i2]])
                    S.op("dve", lambda e: e.tensor_tensor(actT[:, fh * 4 + fc, :], sl[i2][:], p3[i2][:], op=ALU.mult), reads=[tsl[i2], tp3[i2]], writes=[tact])
            for ch in range(2):
                w2, tw2 = W[ti % NS], tW[ti % NS]
                if ti + PF < len(tiles):
                    load(ti + PF)
                ti += 1
                w2v = w2[:].rearrange("p (a b) c -> p a (b c)", a=8)
                for cb in range(2):
                    pi = kk % 2; yi = kk % 3; kk += 1
                    for fc in range(8):
                        S.op("pe", lambda e, fc=fc: e.matmul(p2[pi][:], actT[:, fc, :], w2v[:, fc, cb * 512:(cb + 1) * 512], start=(fc == 0), stop=(fc == 7)),
                             reads=[tact, tw2], writes=[tp2[pi]], accum=True)
                    if kk % 2:
                        S.op("act", lambda e: e.copy(ye[yi][:], p2[pi][:]), reads=[tp2[pi]], writes=[tye[yi]])
                    else:
                        S.op("dve", lambda e: e.tensor_copy(ye[yi][:], p2[pi][:]), reads=[tp2[pi]], writes=[tye[yi]])
                    c0 = ch * 1024 + cb * 512
                    S.dma("sp", self.YS[j * 128:(j + 1) * 128, c0:c0 + 512], ye[yi][:], reads=[tye[yi]], writes=[self.tk["YS"]])

    def ph_combine(self, l, need_ctx, last):
        S, I, C = self.S, self.I, self.C
        tk = lambda k=2: [S.tok() for _ in range(k)]
        Y1 = [self.sb("Y1", [128, 2048]) for _ in range(2)]; Y2 = [self.sb("Y2", [128, 2048]) for _ in range(2)]; tY = tk()
        ym = [self.sb("ym", [128, 2048]) for _ in range(2)]; tym = tk()
        x1 = [self.sb("x1", [128, 16, 128]) for _ in range(2)]; tx1 = tk()
        xo = [self.sb("xo", [128, 16, 128]) for _ in range(2)]; txo = tk()
        pT = [self.ps("pT", [128, 4, 128]) for _ in range(4)]; tpT = tk(4)
        IDX = [self.sb("idx", [128, 1], I32) for _ in range(4)]; tIDX = tk(4)
        blocks = ([0, 1] if need_ctx else []) + list(range(2, NB))
        bodies = []
        for blk in blocks:
            def body(i, blk=blk):
                j = 1 if blk < 2 else 0
                ix1, ix2, t1_, t2_ = IDX[2 * i], IDX[2 * i + 1], tIDX[2 * i], tIDX[2 * i + 1]
                S.op("dve", lambda e: e.tensor_copy(ix1[:, :], self.DEST[:, blk, 0:1]), reads=[self.troute], writes=[t1_])
                S.op("dve", lambda e: e.tensor_copy(ix2[:, :], self.DEST[:, blk, 1:2]), reads=[self.troute], writes=[t2_])
                S.indirect(Y1[i][:, :], None, self.YS[:, :], bass.IndirectOffsetOnAxis(ap=ix1[:, :], axis=0), NBLK * 128 - 1,
                           reads=[self.tk["YS"], t1_], writes=[tY[i]])
                S.indirect(Y2[i][:, :], None, self.YS[:, :], bass.IndirectOffsetOnAxis(ap=ix2[:, :], axis=0), NBLK * 128 - 1,
                           reads=[self.tk["YS"], t2_], writes=[tY[i]])
                S.dma("sp", x1[i][:], self.X1T[:, blk * 128:(blk + 1) * 128].rearrange("(k p) t -> p k t", p=128), reads=[self.tk["X1T"]], writes=[tx1[i]])
                S.op("dve", lambda e: e.tensor_scalar(ym[i][:], Y1[i][:], self.WT[:, blk, 0:1], None, op0=ALU.mult), reads=[tY[i], self.troute], writes=[tym[i]])
                S.op("dve", lambda e: e.scalar_tensor_tensor(ym[i][:], Y2[i][:], self.WT[:, blk, 1:2], ym[i][:], op0=ALU.mult, op1=ALU.add), reads=[tY[i], self.troute, tym[i]], writes=[tym[i]])
                for q in range(4):
                    pi = 2 * i + (q % 2)
                    for c in range(4):
                        kc = q * 4 + c
                        S.op("pe", lambda e, kc=kc, c=c, pi=pi: e.transpose(pT[pi][:, c, :], ym[i][:, kc * 128:(kc + 1) * 128], C["ident_f"][:]), reads=[tym[i], self.tC], writes=[tpT[pi]], accum=True)
                    for c in range(4):
                        kc = q * 4 + c
                        S.op("dve", lambda e, kc=kc, c=c, pi=pi: e.scalar_tensor_tensor(xo[i][:, kc, :], pT[pi][:, c, :], self.modT[:, 80 + kc, j:j + 1], x1[i][:, kc, :], op0=ALU.mult, op1=ALU.add),
                             reads=[tpT[pi], tx1[i], self.tmod], writes=[txo[i]])
                if last:
                    S.dma("sp", self.out[:, (blk - 2) * 128:(blk - 1) * 128].rearrange("(k p) t -> p k t", p=128), xo[i][:], reads=[txo[i]], writes=[self.tk["OUT"]])
                else:
                    S.dma("sp", self.XT[:, blk * 128:(blk + 1) * 128].rearrange("(k p) t -> p k t", p=128), xo[i][:], reads=[txo[i]], writes=[self.tk["XT"]])
            bodies.append(body)
        S.threads(bodies)


_CONSTS = None


def _prep_shared(inp, n_layers):
    global _CONSTS
    if _CONSTS is None:
        _CONSTS = _host_consts()
    sh = dict(_CONSTS)
    sp = np.cumsum([0, 1024, 256, 256, 512, 512, 1024, 1024, 32, 1024, 6144])
    q, k, v, mq, mk, mv, mo, mg, fu, gp = [slice(sp[i], sp[i + 1]) for i in range(10)]
    for l in range(n_layers):
        w_in = np.asarray(inp["w_in"][l], np.float32)
        sh[f"w_fm{l}"] = np.ascontiguousarray(np.concatenate([w_in[:, s] for s in (q, k, mq, mk, mo, fu, gp)], axis=1))
        sh[f"w_tm{l}"] = np.ascontiguousarray(np.concatenate([w_in[:, s] for s in (mv, mk, v, mg)] + [np.zeros((D, 224), np.float32)], axis=1))
        sh[f"w_mod{l}"] = np.ascontiguousarray(inp["w_mod"][l], dtype=np.float32)
        sh[f"bmod{l}"] = _fm(inp["b_mod"][l])
        sh[f"ng{l}"] = np.ascontiguousarray(np.stack([_fm(inp["norm1_g"][l]), _fm(inp["norm2_g"][l])], axis=1))
        sh[f"qkg{l}"] = np.ascontiguousarray(np.stack([np.tile(inp["q_norm_g"][l], 2), np.tile(inp["k_norm_g"][l], 2)], axis=1).astype(np.float32))
        sh[f"sink{l}"] = np.ascontiguousarray(np.tile(np.asarray(inp["attn_sink"][l], np.float32)[None, :], (64, 1)))
        sh[f"mlgb{l}"] = np.ascontiguousarray(np.tile(np.asarray(inp["ml_gate_b"][l], np.float32).reshape(1, 32), (128, 1)))
        sh[f"mlng{l}"] = _fm(inp["ml_norm_g"][l])
        sh[f"wba{l}"] = np.ascontiguousarray(inp["w_br_attn"][l], dtype=np.float32)
        sh[f"wbf{l}"] = np.ascontiguousarray(inp["w_br_four"][l], dtype=np.float32)
        sh[f"wbm{l}"] = np.ascontiguousarray(inp["w_br_mlstm"][l], dtype=np.float32)
        sh[f"bgate{l}"] = np.ascontiguousarray(np.stack([_fm(inp["b_gate"][l][b]) for b in range(3)], axis=1))
        sh[f"wout{l}"] = np.ascontiguousarray(inp["w_out"][l], dtype=np.float32)
        wr = np.concatenate([np.asarray(inp["w_grp"][l], np.float32), np.asarray(inp["w_exp_router"][l], np.float32).reshape(D, 32)], axis=1)
        sh[f"wr{l}"] = np.ascontiguousarray(wr.reshape(16, 128, 36).transpose(1, 0, 2))
        rbias = np.concatenate([np.asarray(inp["b_grp"][l], np.float32), np.asarray(inp["b_exp_router"][l], np.float32).reshape(32)])
        sh[f"rbias{l}"] = np.ascontiguousarray(np.tile(rbias[None, :], (128, 1)))
        for nm in ("w1", "w3"):
            w = np.asarray(inp[nm][l], np.float32).reshape(NE, 16, 128, 2, 512)
            sh[f"{nm}r_{l}"] = np.ascontiguousarray(w.transpose(0, 3, 2, 1, 4)).reshape(NE * 2 * 128, 16 * 512)
        w = np.asarray(inp["w2"][l], np.float32).reshape(NE, 8, 128, 2, 1024)
        sh[f"w2r_{l}"] = np.ascontiguousarray(w.transpose(0, 3, 2, 1, 4)).reshape(NE * 2 * 128, 8 * 1024)
    return sh


def _prep_core(inp, b):
    x = np.asarray(inp["x"][b], np.float32)
    ctx = np.asarray(inp["ctx"][b], np.float32)
    xT0 = np.ascontiguousarray(np.concatenate([ctx, x], axis=0).T)
    cT = np.ascontiguousarray(np.stack([_fm(inp["c"][b]), _fm(inp["c_ctx"])], axis=2))
    return {"xT0": xT0, "cT": cT}


def run(inp, batches, n_layers=2, stop_after=None, debug=(), only=None, trace=False):
    sh = _prep_shared(inp, n_layers)
    if only == "NOBIG":
        sh = {k: v for k, v in sh.items() if not k.startswith(("w1r_", "w3r_", "w2r_", "wba", "wbf", "wbm", "wout"))}
    elif only is not None:
        sh = {k: v for k, v in sh.items() if k in only}
    in_maps = []
    for b in batches:
        m = dict(sh)
        m.update(_prep_core(inp, b))
        in_maps.append(m)
    shapes = {k: (v.shape, v.dtype) for k, v in in_maps[0].items()}
    nc = bass.Bass("TRN2", target_bir_lowering=False)
    Prog(nc, shapes, n_layers=n_layers, stop_after=stop_after, debug=debug).build()
    res = run_bass_kernel_spmd(nc, in_maps, core_ids=list(range(len(batches))), **({"trace": True} if trace else {}))
    return res


def kernel(**inputs):
    res = run(inputs, [0, 1, 2, 3])
    out = np.stack([np.ascontiguousarray(r["out"].T) for r in res.results], axis=0)
    return out.astype(np.float32)
```
